# Optimizing a Trainium2 kernel written in Bass

```python
import math
import jax, jax.numpy as jnp
from jax import lax
import numpy as np

D_MODEL = 1024
BATCH = 2
SEQ = 16384
DEPTH = 2
DEC_BATCH = 16
DEC_SEQ = 16
PAST_LEN = 4096

CHUNK = 64
Q_BLOCK = 128
N_A = (DEPTH + 1) // 2
N_B = DEPTH // 2
DA_HEADS = 8
DA_HEAD_DIM = 64
DA_V_DIM = 2 * DA_HEAD_DIM
DA_WIDTH = DA_HEADS * DA_V_DIM
MLA_HEADS = 16
MLA_Q_LORA = 768
MLA_KV_LORA = 256
MLA_NOPE = 64
MLA_ROPE = 32
MLA_V = 64
ROPE_THETA = 10000.0
FFN_DENSE = 2816
N_EXPERTS = 8
TOP_K = 2
FFN_EXPERT = 3584
NORM_EPS = 1e-6
NEG_INF = -1e30

kernel_name = "hybrid_diffattn_mla_stream_step"


def _rms_norm(x, g):
    xf = x.astype(jnp.float32)
    y = xf * lax.rsqrt(jnp.mean(xf * xf, axis=-1, keepdims=True) + NORM_EPS)
    return (y * g.astype(jnp.float32)).astype(x.dtype)


def _rope(x, pos):
    half = x.shape[-1] // 2
    freqs = jnp.power(ROPE_THETA, -jnp.arange(half, dtype=jnp.float32) * 2.0 / x.shape[-1])
    ang = pos.astype(jnp.float32)[:, None] * freqs[None, :]
    cos = jnp.cos(ang)[:, None, :]
    sin = jnp.sin(ang)[:, None, :]
    xf = x.astype(jnp.float32)
    x1, x2 = xf[..., :half], xf[..., half:]
    return jnp.concatenate([x1 * cos - x2 * sin, x2 * cos + x1 * sin], axis=-1).astype(x.dtype)


def _chunk_mask(q_pos, k_pos):
    return (k_pos[None, :] // CHUNK) <= (q_pos[:, None] // CHUNK)


def _over_query_blocks(fn, q, q_pos):
    B, T = q.shape[0], q.shape[1]
    if T <= Q_BLOCK:
        return fn(q, q_pos)
    nb = T // Q_BLOCK
    qb = jnp.moveaxis(q.reshape((B, nb, Q_BLOCK) + q.shape[2:]), 1, 0)
    pb = q_pos.reshape(nb, Q_BLOCK)
    out = lax.map(lambda a: fn(a[0], a[1]), (qb, pb))
    out = jnp.moveaxis(out, 0, 1)
    return out.reshape((B, T) + out.shape[3:])


def _diff_mixer(h, pos0, past_k, past_v, w_qkv, lam, subln, w_o, lam_init):
    B, T, _ = h.shape
    qkv = h @ w_qkv
    q = qkv[..., :DA_WIDTH].reshape(B, T, DA_HEADS, DA_V_DIM)
    k = qkv[..., DA_WIDTH:2 * DA_WIDTH].reshape(B, T, DA_HEADS, DA_V_DIM)
    v = qkv[..., 2 * DA_WIDTH:].reshape(B, T, DA_HEADS, DA_V_DIM)
    if past_k is None:
        k_all, v_all = k, v
    else:
        k_all = jnp.concatenate([past_k.astype(k.dtype), k], axis=1)
        v_all = jnp.concatenate([past_v.astype(v.dtype), v], axis=1)
    n_keys = k_all.shape[1]
    q_pos = pos0 + jnp.arange(T, dtype=jnp.int32)
    k_pos = jnp.arange(n_keys, dtype=jnp.int32)
    lf = lam.astype(jnp.float32)
    lam_full = jnp.exp(jnp.sum(lf[0] * lf[1])) - jnp.exp(jnp.sum(lf[2] * lf[3])) + lam_init
    slopes = jnp.exp2(-8.0 * jnp.arange(1, DA_HEADS + 1, dtype=jnp.float32) / DA_HEADS)
    k1, k2 = k_all[..., :DA_HEAD_DIM], k_all[..., DA_HEAD_DIM:]
    scale = DA_HEAD_DIM ** -0.5

    def block(qb, pb):
        s1 = jnp.einsum('bqhd,bkhd->bhqk', qb[..., :DA_HEAD_DIM], k1, preferred_element_type=jnp.float32)
        s2 = jnp.einsum('bqhd,bkhd->bhqk', qb[..., DA_HEAD_DIM:], k2, preferred_element_type=jnp.float32)
        dist = jnp.abs(pb[:, None] - k_pos[None, :]).astype(jnp.float32)
        bias = jnp.where(_chunk_mask(pb, k_pos)[None], -slopes[:, None, None] * dist[None], NEG_INF)
        p1 = jax.nn.softmax(s1 * scale + bias[None], axis=-1)
        p2 = jax.nn.softmax(s2 * scale + bias[None], axis=-1)
        a = (p1 - lam_full * p2).astype(v_all.dtype)
        return jnp.einsum('bhqk,bkhe->bqhe', a, v_all)

    o = _over_query_blocks(block, q, q_pos)
    o = _rms_norm(o, subln) * (1.0 - lam_init)
    y = o.reshape(B, T, DA_WIDTH) @ w_o
    return y, k, v


def _mla_mixer(h, pos0, past_c, past_pe, w_a, g_q, g_kv, w_uq, w_ukv, w_o):
    B, T, _ = h.shape
    a = h @ w_a
    c_q = _rms_norm(a[..., :MLA_Q_LORA], g_q)
    c_kv = _rms_norm(a[..., MLA_Q_LORA:MLA_Q_LORA + MLA_KV_LORA], g_kv)
    pe_raw = a[..., MLA_Q_LORA + MLA_KV_LORA:]
    q_pos = pos0 + jnp.arange(T, dtype=jnp.int32)
    q = (c_q @ w_uq).reshape(B, T, MLA_HEADS, MLA_NOPE + MLA_ROPE)
    q = jnp.concatenate([q[..., :MLA_NOPE], _rope(q[..., MLA_NOPE:], q_pos)], axis=-1)
    k_pe = _rope(pe_raw[:, :, None, :], q_pos)[:, :, 0, :]
    if past_c is None:
        c_all, pe_all = c_kv, k_pe
    else:
        c_all = jnp.concatenate([past_c.astype(c_kv.dtype), c_kv], axis=1)
        pe_all = jnp.concatenate([past_pe.astype(k_pe.dtype), k_pe], axis=1)
    n_keys = c_all.shape[1]
    k_pos = jnp.arange(n_keys, dtype=jnp.int32)
    kv = (c_all @ w_ukv).reshape(B, n_keys, MLA_HEADS, MLA_NOPE + MLA_V)
    k_nope, v = kv[..., :MLA_NOPE], kv[..., MLA_NOPE:]
    scale = (MLA_NOPE + MLA_ROPE) ** -0.5

    def block(qb, pb):
        s = jnp.einsum('bqhd,bkhd->bhqk', qb[..., :MLA_NOPE], k_nope, preferred_element_type=jnp.float32)
        s = s + jnp.einsum('bqhr,bkr->bhqk', qb[..., MLA_NOPE:], pe_all, preferred_element_type=jnp.float32)
        s = jnp.where(_chunk_mask(pb, k_pos)[None, None], s * scale, NEG_INF)
        p = jax.nn.softmax(s, axis=-1).astype(v.dtype)
        return jnp.einsum('bhqk,bkhe->bqhe', p, v)

    o = _over_query_blocks(block, q, q_pos)
    y = o.reshape(B, T, MLA_HEADS * MLA_V) @ w_o
    return y, c_kv, k_pe


def _swiglu(h, w_gu, w_down):
    gu = h @ w_gu
    f = w_down.shape[0]
    return (jax.nn.silu(gu[..., :f]) * gu[..., f:]) @ w_down


def _moe(h, router, w_gu, w_down):
    logits = jnp.einsum('btd,de->bte', h, router, preferred_element_type=jnp.float32)
    top_v, top_i = lax.top_k(logits, TOP_K)
    gates = jax.nn.softmax(top_v, axis=-1)
    comb = jnp.sum(jax.nn.one_hot(top_i, N_EXPERTS, dtype=jnp.float32) * gates[..., None], axis=-2)
    comb = comb.astype(h.dtype)
    y = jnp.zeros_like(h)
    for e in range(N_EXPERTS):
        y = y + comb[..., e:e + 1] * _swiglu(h, w_gu[e], w_down[e])
    return y


def _trunk(x, pos0, cache_diff_k, cache_diff_v, cache_mla_ckv, cache_mla_kpe,
           norm_mix, norm_ffn, norm_final, diff_w_qkv, diff_lambda, diff_subln, diff_w_o,
           mla_w_a, mla_norm_q, mla_norm_kv, mla_w_uq, mla_w_ukv, mla_w_o,
           ffn_w_gu, ffn_w_down, moe_router, moe_w_gu, moe_w_down):
    dk, dv, mc, mp = [], [], [], []
    for i in range(DEPTH):
        j = i // 2
        h = _rms_norm(x, norm_mix[i])
        if i % 2 == 0:
            pk = None if cache_diff_k is None else cache_diff_k[j]
            pv = None if cache_diff_v is None else cache_diff_v[j]
            lam_init = 0.8 - 0.6 * math.exp(-0.3 * i)
            out, nk, nv = _diff_mixer(h, pos0, pk, pv, diff_w_qkv[j], diff_lambda[j], diff_subln[j],
                                      diff_w_o[j], lam_init)
            dk.append(nk)
            dv.append(nv)
        else:
            pc = None if cache_mla_ckv is None else cache_mla_ckv[j]
            pp = None if cache_mla_kpe is None else cache_mla_kpe[j]
            out, nc, npe = _mla_mixer(h, pos0, pc, pp, mla_w_a[j], mla_norm_q[j], mla_norm_kv[j],
                                      mla_w_uq[j], mla_w_ukv[j], mla_w_o[j])
            mc.append(nc)
            mp.append(npe)
        x = x + out
        h = _rms_norm(x, norm_ffn[i])
        if i % 2 == 0:
            x = x + _swiglu(h, ffn_w_gu[j], ffn_w_down[j])
        else:
            x = x + _moe(h, moe_router[j], moe_w_gu[j], moe_w_down[j])
    y = _rms_norm(x, norm_final)
    return y, jnp.stack(dk, 0), jnp.stack(dv, 0), jnp.stack(mc, 0), jnp.stack(mp, 0)


def setup_inputs(seed: int = 0) -> dict:
    key = jax.random.key(seed)
    ks = jax.random.split(key, 32)
    f32 = jnp.float32

    def w(k, shape, fan_in):
        return jax.random.normal(k, shape, f32) * (fan_in ** -0.5)

    def gain(k, shape):
        return 1.0 + 0.01 * jax.random.normal(k, shape, f32)

    return {
        "x_prompt": jax.random.normal(ks[0], (BATCH, SEQ, D_MODEL), f32),
        "x_sample": jax.random.normal(ks[1], (DEC_BATCH, DEC_SEQ, D_MODEL), f32),
        "cache_diff_k": jax.random.normal(ks[2], (N_A, DEC_BATCH, PAST_LEN, DA_HEADS, 2 * DA_HEAD_DIM), f32),
        "cache_diff_v": jax.random.normal(ks[3], (N_A, DEC_BATCH, PAST_LEN, DA_HEADS, DA_V_DIM), f32),
        "cache_mla_ckv": jax.random.normal(ks[4], (N_B, DEC_BATCH, PAST_LEN, MLA_KV_LORA), f32),
        "cache_mla_kpe": jax.random.normal(ks[5], (N_B, DEC_BATCH, PAST_LEN, MLA_ROPE), f32),
        "norm_mix": gain(ks[6], (DEPTH, D_MODEL)),
        "norm_ffn": gain(ks[7], (DEPTH, D_MODEL)),
        "norm_final": gain(ks[8], (D_MODEL,)),
        "diff_w_qkv": w(ks[9], (N_A, D_MODEL, 3 * DA_WIDTH), D_MODEL),
        "diff_lambda": 0.1 * jax.random.normal(ks[10], (N_A, 4, DA_HEAD_DIM), f32),
        "diff_subln": gain(ks[11], (N_A, DA_V_DIM)),
        "diff_w_o": w(ks[12], (N_A, DA_WIDTH, D_MODEL), DA_WIDTH),
        "mla_w_a": w(ks[13], (N_B, D_MODEL, MLA_Q_LORA + MLA_KV_LORA + MLA_ROPE), D_MODEL),
        "mla_norm_q": gain(ks[14], (N_B, MLA_Q_LORA)),
        "mla_norm_kv": gain(ks[15], (N_B, MLA_KV_LORA)),
        "mla_w_uq": w(ks[16], (N_B, MLA_Q_LORA, MLA_HEADS * (MLA_NOPE + MLA_ROPE)), MLA_Q_LORA),
        "mla_w_ukv": w(ks[17], (N_B, MLA_KV_LORA, MLA_HEADS * (MLA_NOPE + MLA_V)), MLA_KV_LORA),
        "mla_w_o": w(ks[18], (N_B, MLA_HEADS * MLA_V, D_MODEL), MLA_HEADS * MLA_V),
        "ffn_w_gu": w(ks[19], (N_A, D_MODEL, 2 * FFN_DENSE), D_MODEL),
        "ffn_w_down": w(ks[20], (N_A, FFN_DENSE, D_MODEL), FFN_DENSE),
        "moe_router": w(ks[21], (N_B, D_MODEL, N_EXPERTS), D_MODEL),
        "moe_w_gu": w(ks[22], (N_B, N_EXPERTS, D_MODEL, 2 * FFN_EXPERT), D_MODEL),
        "moe_w_down": w(ks[23], (N_B, N_EXPERTS, FFN_EXPERT, D_MODEL), FFN_EXPERT),
    }


def reference(x_prompt, x_sample, cache_diff_k, cache_diff_v, cache_mla_ckv, cache_mla_kpe,
              norm_mix, norm_ffn, norm_final, diff_w_qkv, diff_lambda, diff_subln, diff_w_o,
              mla_w_a, mla_norm_q, mla_norm_kv, mla_w_uq, mla_w_ukv, mla_w_o,
              ffn_w_gu, ffn_w_down, moe_router, moe_w_gu, moe_w_down):
    y_prompt, diff_k_prompt, diff_v_prompt, mla_ckv_prompt, mla_kpe_prompt = _trunk(
        x_prompt, 0, None, None, None, None,
        norm_mix, norm_ffn, norm_final, diff_w_qkv, diff_lambda, diff_subln, diff_w_o,
        mla_w_a, mla_norm_q, mla_norm_kv, mla_w_uq, mla_w_ukv, mla_w_o,
        ffn_w_gu, ffn_w_down, moe_router, moe_w_gu, moe_w_down)
    past_len = cache_diff_k.shape[2]
    y_sample, diff_k_sample, diff_v_sample, mla_ckv_sample, mla_kpe_sample = _trunk(
        x_sample, past_len, cache_diff_k, cache_diff_v, cache_mla_ckv, cache_mla_kpe,
        norm_mix, norm_ffn, norm_final, diff_w_qkv, diff_lambda, diff_subln, diff_w_o,
        mla_w_a, mla_norm_q, mla_norm_kv, mla_w_uq, mla_w_ukv, mla_w_o,
        ffn_w_gu, ffn_w_down, moe_router, moe_w_gu, moe_w_down)
    return (y_prompt, y_sample, diff_k_prompt, diff_v_prompt, mla_ckv_prompt, mla_kpe_prompt,
            diff_k_sample, diff_v_sample, mla_ckv_sample, mla_kpe_sample)
```

```python
import contextlib
import math
import numpy as np
import concourse.bass as bass
import concourse.mybir as mybir
from concourse.bass_utils import run_bass_kernel_spmd

F32 = mybir.dt.float32
BF16 = mybir.dt.bfloat16
ALU = mybir.AluOpType
AF = mybir.ActivationFunctionType
AX = mybir.AxisListType
ENGS = ("pe", "act", "dve", "pool", "sp")


class Op:
    __slots__ = ("eng", "fn", "reads", "writes", "is_dma", "semkey", "waits", "sig", "signal")

    def __init__(self, eng, fn, reads, writes, is_dma, semkey):
        self.eng, self.fn, self.reads, self.writes = eng, fn, reads, writes
        self.is_dma, self.semkey = is_dma, semkey
        self.waits = {}
        self.sig = None
        self.signal = False


class Prog:
    def __init__(self, nc):
        self.nc = nc
        self.ops = []
        self.last_writer = {}
        self.readers = {}
        self.dma_count = {}
        self.final_dma = {}
        self.pending = {}
        self.last_op = {}
        self.sem_of = {}
        self.phys_count = []

    def barrier(self):
        lasts = [o for o in self.last_op.values()]
        for o in lasts:
            o.signal = True
        dm = {i: v for i, v in enumerate(self.phys_count)}
        for e in ENGS:
            self.pending[e] = (list(lasts), dict(dm))
        self.last_writer = {}
        self.readers = {}
        self.sem_of = {}

    def _add(self, op):
        deps = []
        for r in op.reads:
            w = self.last_writer.get(r)
            if w is not None:
                deps.append(w)
        for r in op.writes:
            w = self.last_writer.get(r)
            if w is not None:
                deps.append(w)
            deps.extend(self.readers.get(r, ()))
        pend = self.pending.pop(op.eng, None)
        if pend is not None:
            for d in pend[0]:
                if d.eng != op.eng:
                    op.waits.setdefault("_dep", []).append(d)
            for k, v in pend[1].items():
                kk = "dma:%s" % (k,)
                op.waits[kk] = max(op.waits.get(kk, 0), v)
        for d in deps:
            if d is op:
                continue
            if d.eng == "pe" and op.eng == "pe" and not d.is_dma and not op.is_dma:
                continue
            d.signal = True
            if d.is_dma:
                key = "dma:%s" % (d.semkey,)
                op.waits[key] = max(op.waits.get(key, 0), self.phys_count[d.semkey])
            else:
                op.waits.setdefault("_dep", []).append(d)
        for r in op.reads:
            self.readers.setdefault(r, []).append(op)
        for r in op.writes:
            self.last_writer[r] = op
            self.readers[r] = []
        self.ops.append(op)
        if not op.is_dma:
            self.last_op[op.eng] = op
        return op

    def op(self, eng, fn, reads=(), writes=()):
        return self._add(Op(eng, fn, tuple(reads), tuple(writes), False, None))

    def dma(self, eng, out, in_, reads=(), writes=(), semkey=None, final=False, **kw):
        if semkey is None:
            semkey = writes[0] if writes else reads[0]
        if semkey not in self.sem_of:
            self.sem_of[semkey] = len(self.sem_of)
            if len(self.phys_count) < len(self.sem_of):
                self.phys_count.append(0)
        phys = self.sem_of[semkey]
        o = Op(eng, (lambda e, out=out, in_=in_, kw=kw: e.dma_start(out=out, in_=in_, **kw)),
               tuple(reads), tuple(writes), True, phys)
        r = self._add(o)
        self.phys_count[phys] += 16
        o.sig = ("dma:%s" % (phys,), self.phys_count[phys])
        return r

    def emit(self, final_engine="sp"):
        nc = self.nc
        cnt = {e: 0 for e in ENGS}
        for o in self.ops:
            if not o.is_dma and o.signal:
                cnt[o.eng] += 1
                o.sig = ("eng:%s" % o.eng, cnt[o.eng])
        for o in self.ops:
            for d in o.waits.pop("_dep", []):
                k, v = d.sig
                o.waits[k] = max(o.waits.get(k, 0), v)
        semnames = set()
        for o in self.ops:
            if o.sig is not None:
                semnames.add(o.sig[0])
            semnames.update(o.waits.keys())
        semnames = sorted(semnames)
        self.n_sems = len(semnames)
        with contextlib.ExitStack() as st:
            sems = {n: st.enter_context(nc.semaphore("s%d" % i)) for i, n in enumerate(semnames)}
            block = st.enter_context(nc.Block())
            per = {e: [o for o in self.ops if o.eng == e] for e in ENGS}
            final_dma = {i: v for i, v in enumerate(self.phys_count)}

            def run(engname, e):
                waited = {}
                for o in per[engname]:
                    for k, v in o.waits.items():
                        if waited.get(k, 0) >= v:
                            continue
                        e.wait_ge(sems[k], v)
                        waited[k] = v
                    ins = o.fn(e)
                    if o.sig is not None and (o.signal or o.is_dma):
                        ins.then_inc(sems[o.sig[0]], 16 if o.is_dma else 1)
                if engname == final_engine:
                    for key, v in final_dma.items():
                        k = "dma:%s" % (key,)
                        if waited.get(k, 0) < v:
                            e.wait_ge(sems[k], v)

            block.tensor(lambda e: run("pe", e))
            block.scalar(lambda e: run("act", e))
            block.vector(lambda e: run("dve", e))
            block.gpsimd(lambda e: run("pool", e))
            block.sync(lambda e: run("sp", e))


D = 1024
EPS = 1e-6
SLOPES = [2.0 ** (-(h + 1)) for h in range(8)]
LAM_INIT0 = 0.8 - 0.6 * math.exp(-0.3 * 0)
FD = 2816
FE = 3584
NE = 8


class Builder:
    def __init__(self, T, PAST, dbg=()):
        self.T, self.PAST = T, PAST
        self.NB = T // 2048
        self.NP = self.NB * 512
        self.NBLKP = self.NB * 4
        self.NBLK = self.NBLKP + 1
        self.NTOK = self.NBLK * 128
        self.PB = PAST // 128
        self.NKB = 16 * self.NB
        self.dbg = set(dbg)
        self.nc = bass.Bass("TRN2", target_bir_lowering=False)
        self.P = Prog(self.nc)
        self.tiles = [(j * 512, 512) for j in range(self.NB)] + [(self.NP, 128)]
        self.uid = 0

    def din(self, name, shape, dt=F32):
        return self.nc.dram_tensor(name, list(shape), dt, kind="ExternalInput").ap()

    def dout(self, name, shape, dt=F32):
        return self.nc.dram_tensor(name, list(shape), dt, kind="ExternalOutput").ap()

    def dscr(self, name, shape, dt=BF16):
        if name in self.dbg:
            return self.nc.dram_tensor(name, list(shape), dt, kind="ExternalOutput").ap()
        return self.nc.dram_tensor(name, list(shape), dt).ap()

    def mm(self, out, lhsT, rhs, start, stop, reads, writes):
        self.P.op("pe", lambda e: e.matmul(out, lhsT, rhs, start=start, stop=stop), reads, writes)

    def tr(self, out, in_, ident, reads, writes):
        self.P.op("pe", lambda e: e.transpose(out, in_, ident), reads, writes)

    def act(self, out, in_, func, reads, writes, **kw):
        self.P.op("act", lambda e: e.activation(out, in_, func, **kw), reads, writes)

    def copy(self, eng, out, in_, reads, writes):
        if eng == "act":
            self.P.op("act", lambda e: e.copy(out, in_), reads, writes)
        else:
            self.P.op(eng, lambda e: e.tensor_copy(out, in_), reads, writes)

    def tt(self, eng, out, a, b, op, reads, writes):
        self.P.op(eng, lambda e: e.tensor_tensor(out, a, b, op), reads, writes)

    def ts(self, eng, out, a, s1, s2, op0, op1, reads, writes):
        if s2 is None:
            self.P.op(eng, lambda e: e.tensor_scalar(out, a, s1, None, op0), reads, writes)
        else:
            self.P.op(eng, lambda e: e.tensor_scalar(out, a, s1, s2, op0, op1), reads, writes)

    def stt(self, eng, out, a, s, b, op0, op1, reads, writes):
        self.P.op(eng, lambda e: e.scalar_tensor_tensor(out, a, s, b, op0, op1), reads, writes)

    def memset(self, eng, ap, v, writes):
        self.P.op(eng, lambda e: e.memset(ap, v), (), writes)

    def ld(self, out, in_, reads, writes, eng="sp", **kw):
        self.P.dma(eng, out, in_, reads=reads, writes=writes, **kw)

    def st(self, out, in_, reads, writes, eng="sp", final=False, semkey=None):
        self.P.dma(eng, out, in_, reads=reads, writes=writes, final=final, semkey=semkey)

    def rstd(self, x, width, sq, ss, rs, xkey, tag):
        n = x.shape[0]
        self.memset("pool", ss, 0.0, [tag + "ss"])
        self.act(sq, x, AF.Square, [xkey, tag + "ss"], [tag + "sq", tag + "ss"], accum_out=ss)
        self.act(rs, ss, AF.Ln, [tag + "ss"], [tag + "rs"], scale=1.0 / width, bias=EPS)
        self.act(rs, rs, AF.Exp, [tag + "rs"], [tag + "rs"], scale=-0.5)

    def build(self):
        nc, P = self.nc, self.P
        NB, NP, NBLKP, NBLK, NTOK, PB, NKB, PAST = self.NB, self.NP, self.NBLKP, self.NBLK, self.NTOK, self.PB, self.NKB, self.PAST
        I = {}
        I["xin"] = self.din("xin", [NTOK, D])
        for n, s in [("g_mix0", [128, D]), ("g_mix1", [128, D]), ("g_ffn0", [128, D]), ("g_ffn1", [128, D]),
                     ("g_fin", [128, D]), ("lam", [128, 256]), ("gsub_col", [128, 1]), ("gsub_row", [128, 128]),
                     ("g_q", [128, 768]), ("g_kv", [128, 256]),
                     ("w_qkv", [D, 3072]), ("w_o0", [D, D]), ("w_gu0", [D, 2 * FD]), ("w_dn0", [FD, D]),
                     ("w_a", [D, 1056]), ("w_uq", [768, 1536]), ("w_uqs", [768, 1536]), ("w_ukv", [256, 2048]),
                     ("w_o1", [D, D]), ("w_r", [D, 8]), ("w_gu1", [NE, D, 2 * FE]), ("w_dn1", [NE, FE, D]),
                     ("cdk", [2, PAST, D]), ("cdv", [2, PAST, D]), ("cckv", [2, PAST, 256]), ("ckpe", [2, PAST, 32]),
                     ("ident", [128, 128]), ("T0", [128, 512]), ("Dneg", [128, 4, 512]), ("Dmask", [128, 4, 512]),
                     ("ctab0", [128, 8 * NB * NKB]), ("mtab", [128, NB * NKB]),
                     ("cs_tm", [NTOK, 32]), ("cs_fm", [2, 32, NTOK]),
                     ("Dnegs", [128, PB * 16]), ("Dnegn", [16, 16])]:
            I[n] = self.din(n, s)
        O = {}
        O["y"] = self.dout("y", [NTOK, D])
        O["dk"] = self.dout("dk", [NTOK, D])
        O["dv"] = self.dout("dv", [NTOK, D])
        O["ckv"] = self.dout("ckv", [NTOK, 256])
        O["kpe"] = self.dout("kpe", [NTOK, 32])
        S = {}
        S["qT"] = self.dscr("qT", [8, 128, NTOK])
        CW = min(NP, 1024); NCW = NP // CW
        self.CW, self.NCW = CW, NCW
        S["kTl"] = [[self.dscr("kTl%d_%d" % (h, c), [128, CW]) for c in range(NCW)] for h in range(8)]
        S["vl"] = [self.dscr("vl%d" % j, [128, D]) for j in range(NBLKP)]
        S["kTg"] = [[self.dscr("kTg%d_%d" % (h, c), [4 * 128, CW]) for c in range(NCW)] for h in range(8)]
        S["vg"] = [self.dscr("vg%d" % j, [4 * 128, D]) for j in range(NBLKP)]
        S["ksT"] = self.dscr("ksT", [8, 128, 128])
        S["vs"] = self.dscr("vs", [128, D])
        S["oT"] = self.dscr("oT", [8, 128, NTOK])
        S["x1"] = self.dscr("x1", [NTOK, D], F32)
        S["actT"] = self.dscr("actT", [22, 128, NTOK])
        S["x2"] = self.dscr("x2", [NTOK, D], F32)
        S["qaT"] = self.dscr("qaT", [16, 96, NTOK])
        S["cpl"] = [[self.dscr("cpl%d_%d" % (j, c), [128, CW]) for c in range(NCW)] for j in range(3)]
        S["cpg"] = [[self.dscr("cpg%d_%d" % (j, c), [4 * 128, CW]) for c in range(NCW)] for j in range(3)]
        S["cps"] = self.dscr("cps", [3, 128, 128])
        S["kaT"] = self.dscr("kaT", [16, 96, 4 * NP])
        S["v1"] = self.dscr("v1", [4 * NP, D])
        S["kaTl"] = self.dscr("kaTl", [16, 96, NP])
        S["v1l"] = self.dscr("v1l", [NP, D])
        S["kaTs"] = self.dscr("kaTs", [16, 96, 128])
        S["v1s"] = self.dscr("v1s", [128, D])
        S["oT1"] = self.dscr("oT1", [16, 64, NTOK])
        S["x3"] = self.dscr("x3", [NTOK, D], F32)
        S["hT"] = self.dscr("hT", [8, 128, NTOK])
        S["comb"] = self.dscr("comb", [NTOK, 8], F32)
        self.I, self.O, self.S = I, O, S

        with contextlib.ExitStack() as g:
            self.g = g
            sb = lambda n, s, d=F32: g.enter_context(nc.sbuf_tensor(n, list(s), d))
            self.ident_f = sb("ident_f", [128, 128])
            self.ident = sb("ident_b", [128, 128], BF16)
            self.ones_f = sb("ones_f", [128, 128])
            self.ones_b = sb("ones_b", [128, 128], BF16)
            self.nlam = sb("nlam", [128, 1])
            self.ld(self.ident_f[:], I["ident"], [], ["ident_f"])
            self.copy("dve", self.ident[:], self.ident_f[:], ["ident_f"], ["ident"])
            self.memset("pool", self.ones_f[:], 1.0, ["ones_f"])
            self.memset("pool", self.ones_b[:], 1.0, ["ones_b"])
            import os
            upto = int(os.environ.get("K_UPTO", "99"))
            steps = [self.phase_lam, self.phase_A,
                     lambda: ([self.gather(S["kTl"][h][c], S["kTg"][h][c], "kT") for h in range(8) for c in range(NCW)], [self.gather(S["vl"][j], S["vg"][j], "v") for j in range(NBLKP)]),
                     lambda: self.phase_attn(0), lambda: self.phase_sample_attn(0), self.phase_C1, self.phase_C2,
                     lambda: [self.gather(S["cpl"][j][c], S["cpg"][j][c], "cp") for j in range(3) for c in range(NCW)], self.phase_D0,
                     lambda: self.phase_attn(1), lambda: self.phase_sample_attn(1), self.phase_E1, self.phase_E2]
            for i, stp in enumerate(steps):
                if i >= upto:
                    break
                stp()
                P.barrier()
            P.emit()
        return nc

    def gather(self, src, dst, key):
        import os
        if key in os.environ.get("K_FAKEG", "").split(","):
            n = src.shape[0]
            for r in range(4):
                self.P.dma("sp", dst[r * n:(r + 1) * n, :], src, reads=["d_" + key + "l"], writes=["d_" + key + "g"], semkey="fakeg")
            return
        self.P.op("pool", lambda e: e.collective_compute(
            "AllGather", ALU.bypass, replica_groups=[[0, 1, 2, 3], [4, 5, 6, 7]],
            ins=[src.opt()], outs=[dst.opt()]), reads=["d_" + key + "l", "cc_chain"], writes=["d_" + key + "g", "cc_chain"])

    def phase_lam(self):
        nc, I = self.nc, self.I
        with contextlib.ExitStack() as st:
            sb = lambda n, s, d=F32: st.enter_context(nc.sbuf_tensor(n, list(s), d))
            lt = sb("lam_t", [128, 256]); pr = sb("lam_p", [128, 128]); s2 = sb("lam_s", [128, 2])
            self.ld(lt[:], I["lam"], [], ["lam_t"])
            self.tt("dve", pr[:, 0:64], lt[:, 0:64], lt[:, 64:128], ALU.mult, ["lam_t"], ["lam_p"])
            self.tt("dve", pr[:, 64:128], lt[:, 128:192], lt[:, 192:256], ALU.mult, ["lam_p", "lam_t"], ["lam_p"])
            self.P.op("dve", lambda e: e.reduce_sum(s2[:, 0:1], pr[:, 0:64], AX.X), ["lam_p"], ["lam_s"])
            self.P.op("dve", lambda e: e.reduce_sum(s2[:, 1:2], pr[:, 64:128], AX.X), ["lam_s", "lam_p"], ["lam_s"])
            self.act(s2[:], s2[:], AF.Exp, ["lam_s"], ["lam_s"])
            self.tt("dve", self.nlam[:], s2[:, 1:2], s2[:, 0:1], ALU.subtract, ["lam_s"], ["nlam"])
            self.ts("dve", self.nlam[:], self.nlam[:], -LAM_INIT0, None, ALU.add, None, ["nlam"], ["nlam"])

    def norm_T(self, st_tiles, x, xkey, gb, width, hT_dst, dstkey, tag, ev="act"):
        t = st_tiles
        self.rstd(x, width, t["sq"][:, 0:width], t["ss"][:, 0:1], t["rs"][:, 0:1], xkey, tag)
        self.stt("dve", t["hb"][:, 0:width], x, t["rs"][:, 0:1], gb, ALU.mult, ALU.mult,
                 [xkey, tag + "rs", "gains"], [tag + "hb"])
        nch = (width + 127) // 128
        for kc in range(nch):
            w = min(128, width - kc * 128)
            self.tr(t["pst"][0:w, kc * 128:(kc + 1) * 128], t["hb"][:, kc * 128:kc * 128 + w], self.ident[:],
                    [tag + "hb", "ident"], [tag + "pst"])
        return nch

    def phase_A(self):
        nc, P, I, O, S = self.nc, self.P, self.I, self.O, self.S
        NP = self.NP
        with contextlib.ExitStack() as st:
            sb = lambda n, s, d=F32: st.enter_context(nc.sbuf_tensor(n, list(s), d))
            psf = lambda n: st.enter_context(nc.psum_tensor(n, [128, 512], F32))
            w = sb("A_w", [128, 8, 3072], BF16)
            gb = sb("A_gb", [128, D])
            xt = [sb("A_xt%d" % i, [128, D]) for i in range(2)]
            T = {"sq": sb("A_sq", [128, D], BF16), "ss": sb("A_ss", [128, 1]), "rs": sb("A_rs", [128, 1]),
                 "hb": sb("A_hb", [128, D], BF16),
                 "pst": st.enter_context(nc.psum_tensor("A_pst", [128, 1024], BF16))}
            hT = [sb("A_hT%d" % i, [128, 8, 512], BF16) for i in range(2)]
            qTt = [sb("A_qT%d" % i, [128, 8, 512], BF16) for i in range(2)]
            kTt = [sb("A_kT%d" % i, [128, 8, 512], BF16) for i in range(2)]
            kvf = [sb("A_kvf%d" % i, [128, 2048]) for i in range(2)]
            vb = [sb("A_vb%d" % i, [128, D], BF16) for i in range(2)]
            ps = [psf("A_ps%d" % i) for i in range(6)]
            pi = 0
            self.ld(w[:], I["w_qkv"].rearrange("(kc p) n -> p kc n", p=128), [], ["A_w"], eng="pool")
            self.ld(gb[:], I["g_mix0"], [], ["gains"])
            bi = 0
            for ti, (t0, nt) in enumerate(self.tiles):
                hs = ti % 2
                nblk = nt // 128
                for b in range(nblk):
                    xs = bi % 2
                    r0 = t0 + b * 128
                    self.ld(xt[xs][:], I["xin"][r0:r0 + 128, :], [], ["A_xt%d" % xs])
                    self.norm_T(T, xt[xs][:], "A_xt%d" % xs, gb[:], D, None, None, "A_")
                    self.copy("act", hT[hs][:, :, b * 128:(b + 1) * 128],
                              T["pst"][:].rearrange("p (k c) -> p k c", k=8), ["A_pst"], ["A_hT%d" % hs])
                    bi += 1
                for (dst, dkey, coff, scl) in ((qTt[hs], "A_qT%d" % hs, 0, 0.125), (kTt[hs], "A_kT%d" % hs, 1024, 1.0)):
                    for h in range(8):
                        p_ = ps[pi % 6]; pk = "A_ps%d" % (pi % 6); pi += 1
                        for kc in range(8):
                            self.mm(p_[:, 0:nt], w[:, kc, coff + h * 128:coff + (h + 1) * 128], hT[hs][:, kc, 0:nt],
                                    kc == 0, kc == 7, ["A_w", "A_hT%d" % hs], [pk])
                        self.act(dst[:, h, 0:nt], p_[:, 0:nt], AF.Copy, [pk], [dkey], scale=scl)
                self.st(S["qT"][:, :, t0:t0 + nt].rearrange("h p t -> p h t"), qTt[hs][:, :, 0:nt],
                        ["A_qT%d" % hs], ["d_qT"], semkey="A_stq%d" % hs)
                if t0 < NP:
                    for h in range(8):
                        self.st(S["kTl"][h][t0 // self.CW][:, t0 % self.CW:t0 % self.CW + nt], kTt[hs][:, h, 0:nt],
                                ["A_kT%d" % hs], ["d_kTl"], semkey="A_stk%d" % hs)
                else:
                    self.st(S["ksT"].rearrange("h p t -> p h t"), kTt[hs][:, :, 0:nt],
                            ["A_kT%d" % hs], ["d_ksT"], semkey="A_stk%d" % hs)
                for b in range(nblk):
                    r0 = t0 + b * 128
                    ks = (bi + b) % 2
                    for n in range(4):
                        p_ = ps[pi % 6]; pk = "A_ps%d" % (pi % 6); pi += 1
                        for kc in range(8):
                            self.mm(p_[:], hT[hs][:, kc, b * 128:(b + 1) * 128], w[:, kc, 1024 + n * 512:1024 + (n + 1) * 512],
                                    kc == 0, kc == 7, ["A_w", "A_hT%d" % hs], [pk])
                        self.copy("dve" if n % 2 else "act", kvf[ks][:, n * 512:(n + 1) * 512], p_[:], [pk], ["A_kvf%d" % ks])
                    self.copy("pool", vb[ks][:], kvf[ks][:, 1024:2048], ["A_kvf%d" % ks], ["A_vb%d" % ks])
                    self.st(O["dk"][r0:r0 + 128, :], kvf[ks][:, 0:1024], ["A_kvf%d" % ks], [], final=True, semkey="A_sto%d" % ks)
                    self.st(O["dv"][r0:r0 + 128, :], kvf[ks][:, 1024:2048], ["A_kvf%d" % ks], [], final=True, semkey="A_sto%d" % ks)
                    if t0 < NP:
                        self.st(S["vl"][r0 // 128], vb[ks][:], ["A_vb%d" % ks], ["d_vl"], semkey="A_stv%d" % ks)
                    else:
                        self.st(S["vs"], vb[ks][:], ["A_vb%d" % ks], ["d_vs"], semkey="A_stv%d" % ks)

    def phase_attn(self, layer):
        nc, P, I, O, S = self.nc, self.P, self.I, self.O, self.S
        NB, NP, NBLKP, NKB = self.NB, self.NP, self.NBLKP, self.NKB
        diff = layer == 0
        NH = 8 if diff else 16
        KR = 128 if diff else 96
        VW = 128 if diff else 65
        nS = 2 if diff else 1
        scale1 = (64 + 32) ** -0.5
        kT_g, v_g = (S["kTg"], S["vg"]) if diff else (S["kaT"], S["v1"])
        kT_l, v_l = (S["kTl"], S["vl"]) if diff else (S["kaTl"], S["v1l"])
        qT_d = S["qT"] if diff else S["qaT"]
        oT_d = S["oT"] if diff else S["oT1"]
        pre = "B%d_" % layer
        with contextlib.ExitStack() as st:
            sb = lambda n, s, d=F32: st.enter_context(nc.sbuf_tensor(pre + n, list(s), d))
            psf = lambda n: st.enter_context(nc.psum_tensor(pre + n, [128, 512], F32))
            KT = sb("KT", [KR, 4, NP], BF16)
            V = sb("V", [128, 4 * NBLKP, VW], BF16)
            QT = [sb("QT%d" % i, [KR, 512], BF16) for i in range(2)]
            KTd = [sb("KTd%d" % i, [KR, 512], BF16) for i in range(2)]
            Vd = [sb("Vd%d" % i, [128, 4, VW], BF16) for i in range(2)]
            ctj = [sb("ct%d" % i, [128, NKB]) for i in range(2)]
            Dn = sb("Dn", [128, 4, 512])
            tmp = [sb("tmp%d" % i, [128, 512]) for i in range(4)]
            Pt = [sb("P%d" % i, [128, 512], BF16) for i in range(4)]
            Lsb = sb("Lsb", [128, 512]); Bs = [sb("Bs%d" % i, [128, 512]) for i in range(2)]
            Pacc = [sb("Pacc%d" % i, [128, 512]) for i in range(2)] if diff else None
            oa = sb("oa", [128, 512]); ob = sb("ob", [128, 512]); osq = sb("osq", [128, 512]); rsd = sb("rsd", [128, 512])
            oTt = [sb("oTt%d" % i, [128, 512], BF16) for i in range(2)]
            Sps = [psf("S%d" % i) for i in range(4)]
            Ops = [psf("O%d" % i) for i in range(2)]
            Lps = psf("L")
            if diff:
                T0 = sb("T0", [128, 512]); Th = sb("Th", [128, 512]); gs = sb("gs", [128, 1])
                self.ld(T0[:], I["T0"], [], [pre + "T0"])
                self.ld(Dn[:], I["Dneg"], [], [pre + "Dn"])
                self.ld(gs[:], I["gsub_col"], [], [pre + "gs"])
                self.ts("dve", gs[:], gs[:], 1.0 - LAM_INIT0, None, ALU.mult, None, [pre + "gs"], [pre + "gs"])
            else:
                self.ld(Dn[:], I["Dmask"], [], [pre + "Dn"])
                self.memset("pool", V[:, :, 64:65], 1.0, [pre + "V"])
                for i in range(2):
                    self.memset("pool", Vd[i][:, :, 64:65], 1.0, [pre + "Vd%d" % i])
            si = 0
            it = 0
            for h in range(NH):
                for rk in range(4):
                    if diff:
                        for c in range(self.NCW):
                            self.ld(KT[:, rk, c * self.CW:(c + 1) * self.CW], kT_g[h][c][rk * 128:(rk + 1) * 128, :], ["d_kTg"], [pre + "KT"], semkey=pre + "ldK")
                        for blk in range(NBLKP):
                            self.ld(V[:, rk * NBLKP + blk, :], v_g[blk][rk * 128:(rk + 1) * 128, h * 128:(h + 1) * 128],
                                    ["d_vg"], [pre + "V"], semkey=pre + "ldV")
                    else:
                        self.ld(KT[:, rk, :], kT_g[h, :, rk * NP:(rk + 1) * NP], ["d_kaT"], [pre + "KT"], semkey=pre + "ldK")
                        self.ld(V[:, rk * NBLKP:(rk + 1) * NBLKP, 0:64],
                                v_g[rk * NP:(rk + 1) * NP, h * 64:(h + 1) * 64].rearrange("(b p) e -> p b e", p=128),
                                ["d_v1"], [pre + "V"], semkey=pre + "ldV")
                if diff:
                    self.ts("pool", Th[:], T0[:], SLOPES[h], None, ALU.mult, None, [pre + "T0"], [pre + "Th"])
                for J in range(NB):
                    js = it % 2; it += 1
                    q0 = J * 512
                    qk, kdk, vdk, ck = pre + "QT%d" % js, pre + "KTd%d" % js, pre + "Vd%d" % js, pre + "ct%d" % js
                    self.ld(QT[js][:], qT_d[h, 0:KR, q0:q0 + 512], ["d_qT"], [qk])
                    if diff:
                        self.ld(KTd[js][:], kT_l[h][q0 // self.CW][:, q0 % self.CW:q0 % self.CW + 512], ["d_kTl"], [kdk])
                        for i_ in range(4):
                            self.ld(Vd[js][:, i_, :], v_l[J * 4 + i_][:, h * 128:(h + 1) * 128], ["d_vl"], [vdk])
                        self.ld(ctj[js][:], I["ctab0"][:, (h * NB + J) * NKB:(h * NB + J + 1) * NKB], [], [ck])
                    else:
                        self.ld(KTd[js][:], kT_l[h, 0:KR, q0:q0 + 512], ["d_kTl"], [kdk])
                        self.ld(Vd[js][:, :, 0:64], v_l[q0:q0 + 512, h * 64:(h + 1) * 64].rearrange("(b p) e -> p b e", p=128),
                                ["d_vl"], [vdk])
                        self.ld(ctj[js][:], I["mtab"][:, J * NKB:(J + 1) * NKB], [], [ck])
                    blocks = []
                    for rk in range(4):
                        for Jk in range(J + 1):
                            for i in range(4):
                                blocks.append(("far", rk, Jk, i))
                    for i in range(4):
                        blocks.append(("diag", 0, 0, i))
                    nblocks = len(blocks)
                    units = [(bi_, blk_, s_) for bi_, blk_ in enumerate(blocks) for s_ in range(nS)]
                    si_base = si
                    si += len(units)

                    def unit_info(u):
                        bi_, (kind, rk, Jk, i), s = units[u]
                        sl = (si_base + u) % 4
                        if kind == "far":
                            kbi = rk * NBLKP + Jk * 4 + i
                            kcol = Jk * 512 + i * 128
                            kfn = lambda lo, hi: KT[lo:hi, rk, kcol:kcol + 128]
                            vap = V[:, kbi, :]
                            krd, vrd = pre + "KT", pre + "V"
                        else:
                            kbi = 0
                            kfn = lambda lo, hi: KTd[js][lo:hi, i * 128:(i + 1) * 128]
                            vap = Vd[js][:, i, :]
                            krd, vrd = kdk, vdk
                        if diff:
                            lo, hi = s * 64, (s + 1) * 64
                        else:
                            lo, hi = 0, 96
                        return bi_, kind, i, s, sl, kbi, kfn, vap, krd, vrd, lo, hi

                    def stage1(u):
                        bi_, kind, i, s, sl, kbi, kfn, vap, krd, vrd, lo, hi = unit_info(u)
                        self.mm(Sps[sl][:], kfn(lo, hi), QT[js][lo:hi, :], True, True, [krd, qk], [pre + "S%d" % sl])

                    def stage2(u):
                        bi_, kind, i, s, sl, kbi, kfn, vap, krd, vrd, lo, hi = unit_info(u)
                        first, last = bi_ == 0, bi_ == nblocks - 1
                        sp_, sk = Sps[sl], pre + "S%d" % sl
                        tp_, tk = tmp[sl], pre + "tmp%d" % sl
                        pp_, pk = Pt[sl], pre + "P%d" % sl
                        if diff:
                            if kind == "far":
                                self.stt("dve", tp_[:], sp_[:], ctj[js][:, kbi:kbi + 1], Th[:], ALU.add, ALU.add,
                                         [sk, ck, pre + "Th"], [tk])
                            else:
                                self.stt("dve", tp_[:], Dn[:, i, :], SLOPES[h], sp_[:], ALU.mult, ALU.add,
                                         [sk, pre + "Dn"], [tk])
                            self.act(pp_[:], tp_[:], AF.Exp, [tk], [pk])
                        else:
                            if kind == "far":
                                self.act(pp_[:], sp_[:], AF.Exp, [sk, ck], [pk], bias=ctj[js][:, kbi:kbi + 1], scale=scale1)
                            else:
                                self.stt("dve", tp_[:], sp_[:], scale1, Dn[:, i, :], ALU.mult, ALU.add,
                                         [sk, pre + "Dn"], [tk])
                                self.act(pp_[:], tp_[:], AF.Exp, [tk], [pk])
                        self.mm(Ops[s][0:VW, :], vap, pp_[:], first, last, [vrd, pk], [pre + "O%d" % s])
                        if diff:
                            leng = "pool" if s == 0 else "dve"
                            if first:
                                self.copy(leng, Pacc[s][:], pp_[:], [pk], [pre + "Pacc%d" % s])
                            else:
                                self.tt(leng, Pacc[s][:], Pacc[s][:], pp_[:], ALU.add, [pk, pre + "Pacc%d" % s], [pre + "Pacc%d" % s])

                    LA = 2
                    for u in range(len(units) + LA):
                        if u < len(units):
                            stage1(u)
                        if u - LA >= 0:
                            stage2(u - LA)
                    ot = oTt[js]; otk = pre + "oTt%d" % js
                    if diff:
                        for s_ in range(2):
                            self.mm(Lps[32 * s_:32 * s_ + 1, :], self.ones_f[:, 0:1], Pacc[s_][:], True, True,
                                    ["ones_f", pre + "Pacc%d" % s_], [pre + "L"])
                        self.copy("dve", Lsb[0:64, :], Lps[0:64, :], [pre + "L"], [pre + "Lsb"])
                        self.P.op("dve", lambda e: e.reciprocal(Lsb[0:1, :], Lsb[0:1, :]), [pre + "Lsb"], [pre + "Lsb"])
                        self.P.op("dve", lambda e: e.reciprocal(Lsb[32:33, :], Lsb[32:33, :]), [pre + "Lsb"], [pre + "Lsb"])
                        self.ts("dve", Lsb[32:33, :], Lsb[32:33, :], self.nlam[32:33, 0:1], None, ALU.mult, None,
                                [pre + "Lsb", "nlam"], [pre + "Lsb"])
                        for s in range(2):
                            self.mm(Sps[s][:], self.ones_f[32 * s:32 * s + 1, :], Lsb[32 * s:32 * s + 1, :], True, True,
                                    ["ones_f", pre + "Lsb"], [pre + "S%d" % s])
                            self.copy("act", Bs[s][:], Sps[s][:], [pre + "S%d" % s], [pre + "Bs%d" % s])
                        self.tt("dve", oa[:], Ops[0][:], Bs[0][:], ALU.mult, [pre + "O0", pre + "Bs0"], [pre + "oa"])
                        self.tt("dve", ob[:], Ops[1][:], Bs[1][:], ALU.mult, [pre + "O1", pre + "Bs1"], [pre + "ob"])
                        self.tt("pool", oa[:], oa[:], ob[:], ALU.add, [pre + "oa", pre + "ob"], [pre + "oa"])
                        self.act(osq[:], oa[:], AF.Square, [pre + "oa"], [pre + "osq"])
                        self.mm(Sps[2][:], self.ones_f[:], osq[:], True, True, ["ones_f", pre + "osq"], [pre + "S2"])
                        self.act(rsd[:], Sps[2][:], AF.Ln, [pre + "S2"], [pre + "rsd"], scale=1.0 / 128, bias=EPS)
                        self.act(rsd[:], rsd[:], AF.Exp, [pre + "rsd"], [pre + "rsd"], scale=-0.5)
                        self.stt("dve", ot[:], oa[:], gs[:, 0:1], rsd[:], ALU.mult, ALU.mult,
                                 [pre + "oa", pre + "gs", pre + "rsd"], [otk])
                        self.st(oT_d[h, :, q0:q0 + 512], ot[:], [otk], ["d_oT"], semkey=pre + "sto%d" % js)
                    else:
                        self.copy("dve", Lsb[64:65, :], Ops[0][64:65, :], [pre + "O0"], [pre + "Lsb"])
                        self.P.op("dve", lambda e: e.reciprocal(Lsb[64:65, :], Lsb[64:65, :]), [pre + "Lsb"], [pre + "Lsb"])
                        self.mm(Sps[0][0:64, :], self.ones_f[64:65, 0:64], Lsb[64:65, :], True, True,
                                ["ones_f", pre + "Lsb"], [pre + "S0"])
                        self.copy("act", Bs[0][0:64, :], Sps[0][0:64, :], [pre + "S0"], [pre + "Bs0"])
                        self.tt("dve", ot[0:64, :], Ops[0][0:64, :], Bs[0][0:64, :], ALU.mult, [pre + "O0", pre + "Bs0"], [otk])
                        self.st(oT_d[h, :, q0:q0 + 512], ot[0:64, :], [otk], ["d_oT1"], semkey=pre + "sto%d" % js)

    def phase_sample_attn(self, layer):
        nc, P, I, O, S = self.nc, self.P, self.I, self.O, self.S
        NP, PB, PAST = self.NP, self.PB, self.PAST
        diff = layer == 0
        pre = "Bs%d_" % layer
        NH = 8 if diff else 16
        scale1 = (64 + 32) ** -0.5
        with contextlib.ExitStack() as st:
            sb = lambda n, s, d=F32: st.enter_context(nc.sbuf_tensor(pre + n, list(s), d))
            psf = lambda n: st.enter_context(nc.psum_tensor(pre + n, [128, 512], F32))
            pst = st.enter_context(nc.psum_tensor(pre + "pst", [128, 1024], BF16))
            Sp = [psf("S%d" % i) for i in range(2)]
            Sn = psf("Sn"); Op_ = [psf("O%d" % i) for i in range(2)]
            zt = sb("zt", [128, 128], BF16)
            self.memset("pool", zt[:], 0.0, [pre + "zt"])
            if diff:
                for h in range(8):
                    self.st(S["oT"][h, :, NP:NP + 128], zt[:], [pre + "zt"], ["d_oT"], semkey=pre + "z")
                Kc = sb("Kc", [128, PB, 128]); Kcb = sb("Kcb", [128, PB, 128], BF16); KcT = sb("KcT", [128, PAST], BF16)
                Vc = sb("Vc", [128, PB, 128]); Vcb = sb("Vcb", [128, PB, 128], BF16)
                qsT = sb("qsT", [128, 8, 128], BF16); ksT = sb("ksT", [128, 8, 128], BF16)
                vsl = [sb("vs%d" % i, [16, D], BF16) for i in range(2)]
                Dns = sb("Dns", [128, PB * 16]); Dnn = sb("Dnn", [16, 16])
                tm = [sb("tm%d" % i, [128, 512]) for i in range(2)]; Ps = [sb("Ps%d" % i, [128, 512], BF16) for i in range(2)]
                tn = [sb("tn%d" % i, [16, 16]) for i in range(2)]; Pn = [sb("Pn%d" % i, [16, 16], BF16) for i in range(2)]
                r = sb("r", [16, 2]); o = sb("o", [16, 128]); sq = sb("sq", [16, 128]); ss = sb("ss", [16, 1]); rs = sb("rs", [16, 1])
                gsr = sb("gsr", [128, 128]); ob = sb("ob", [16, 128], BF16); oTs = sb("oTs", [128, 16], BF16)
                self.ld(qsT[:], S["qT"][:, :, NP:NP + 128].rearrange("h p t -> p h t"), ["d_qT"], [pre + "qsT"])
                self.ld(ksT[:], S["ksT"].rearrange("h p t -> p h t"), ["d_ksT"], [pre + "ksT"])
                for i in range(2):
                    self.ld(vsl[i][:], S["vs"][i * 32:i * 32 + 16, :], ["d_vs"], [pre + "vs"])
                self.ld(Dns[:], I["Dnegs"], [], [pre + "Dns"])
                self.ld(Dnn[:], I["Dnegn"], [], [pre + "Dnn"])
                self.ld(gsr[:], I["gsub_row"], [], [pre + "gsr"])
                for s in range(2):
                    c0 = s * 32
                    for h in range(8):
                        self.ld(Kc[:], I["cdk"][s, :, h * 128:(h + 1) * 128].rearrange("(b p) e -> p b e", p=128), [], [pre + "Kc"])
                        self.ld(Vc[:], I["cdv"][s, :, h * 128:(h + 1) * 128].rearrange("(b p) e -> p b e", p=128), [], [pre + "Vc"])
                        self.copy("pool", Kcb[:], Kc[:], [pre + "Kc"], [pre + "Kcb"])
                        self.copy("pool", Vcb[:], Vc[:], [pre + "Vc"], [pre + "Vcb"])
                        for g8 in range(PB // 8):
                            for j in range(8):
                                kb = g8 * 8 + j
                                self.tr(pst[:, j * 128:(j + 1) * 128], Kcb[:, kb, :], self.ident[:], [pre + "Kcb", "ident"], [pre + "pst"])
                            self.copy("act", KcT[:, g8 * 1024:(g8 + 1) * 1024], pst[:], [pre + "pst"], [pre + "KcT"])
                        for sidx in range(2):
                            lo, hi = sidx * 64, (sidx + 1) * 64
                            for kb in range(PB):
                                self.mm(Sp[sidx][:, kb * 16:(kb + 1) * 16], KcT[lo:hi, kb * 128:(kb + 1) * 128],
                                        qsT[lo:hi, h, c0:c0 + 16], True, True, [pre + "KcT", pre + "qsT"], [pre + "S%d" % sidx])
                            self.stt("dve", tm[sidx][:, 0:PB * 16], Dns[:], SLOPES[h], Sp[sidx][:, 0:PB * 16], ALU.mult, ALU.add,
                                     [pre + "Dns", pre + "S%d" % sidx], [pre + "tm%d" % sidx])
                            self.act(Ps[sidx][:, 0:PB * 16], tm[sidx][:, 0:PB * 16], AF.Exp, [pre + "tm%d" % sidx], [pre + "Ps%d" % sidx])
                            self.mm(Sn[0:16, sidx * 16:(sidx + 1) * 16], ksT[lo:hi, h, c0:c0 + 16], qsT[lo:hi, h, c0:c0 + 16],
                                    True, True, [pre + "ksT", pre + "qsT"], [pre + "Sn"])
                            self.stt("dve", tn[sidx][:], Dnn[:], SLOPES[h], Sn[0:16, sidx * 16:(sidx + 1) * 16], ALU.mult, ALU.add,
                                     [pre + "Dnn", pre + "Sn"], [pre + "tn%d" % sidx])
                            self.act(Pn[sidx][:], tn[sidx][:], AF.Exp, [pre + "tn%d" % sidx], [pre + "Pn%d" % sidx])
                            ok = pre + "O%d" % sidx
                            for kb in range(PB):
                                self.mm(Op_[sidx][0:16, 0:128], Ps[sidx][:, kb * 16:(kb + 1) * 16], Vcb[:, kb, :], kb == 0, False,
                                        [pre + "Ps%d" % sidx, pre + "Vcb"], [ok])
                            self.mm(Op_[sidx][0:16, 0:128], Pn[sidx][:], vsl[s][:, h * 128:(h + 1) * 128], False, True,
                                    [pre + "Pn%d" % sidx, pre + "vs"], [ok])
                            for kb in range(PB):
                                self.mm(Op_[sidx][0:16, 128:129], Ps[sidx][:, kb * 16:(kb + 1) * 16], self.ones_b[:, 0:1], kb == 0, False,
                                        [pre + "Ps%d" % sidx, "ones_b"], [ok])
                            self.mm(Op_[sidx][0:16, 128:129], Pn[sidx][:], self.ones_b[0:16, 0:1], False, True,
                                    [pre + "Pn%d" % sidx, "ones_b"], [ok])
                        self.P.op("dve", lambda e: e.reciprocal(r[:, 0:1], Op_[0][0:16, 128:129]), [pre + "O0"], [pre + "r"])
                        self.P.op("dve", lambda e: e.reciprocal(r[:, 1:2], Op_[1][0:16, 128:129]), [pre + "O1", pre + "r"], [pre + "r"])
                        self.ts("dve", r[:, 1:2], r[:, 1:2], self.nlam[0:16, 0:1], None, ALU.mult, None, [pre + "r", "nlam"], [pre + "r"])
                        self.ts("dve", o[:], Op_[0][0:16, 0:128], r[:, 0:1], None, ALU.mult, None, [pre + "O0", pre + "r"], [pre + "o"])
                        self.stt("dve", o[:], Op_[1][0:16, 0:128], r[:, 1:2], o[:], ALU.mult, ALU.add, [pre + "O1", pre + "r", pre + "o"], [pre + "o"])
                        self.rstd(o[:], 128, sq[:], ss[:], rs[:], pre + "o", pre)
                        self.ts("dve", rs[:], rs[:], 1.0 - LAM_INIT0, None, ALU.mult, None, [pre + "rs"], [pre + "rs"])
                        self.stt("dve", ob[:], o[:], rs[:, 0:1], gsr[0:16, :], ALU.mult, ALU.mult, [pre + "o", pre + "rs", pre + "gsr"], [pre + "ob"])
                        self.tr(pst[:, 0:16], ob[:], self.ident[0:16, 0:16], [pre + "ob", "ident"], [pre + "pst"])
                        self.copy("act", oTs[:], pst[:, 0:16], [pre + "pst"], [pre + "oTs"])
                        self.st(S["oT"][h, :, NP + c0:NP + c0 + 16], oTs[:], [pre + "oTs"], ["d_oT"], semkey=pre + "sto")
            else:
                for h in range(16):
                    self.st(S["oT1"][h, :, NP:NP + 128], zt[0:64, :], [pre + "zt"], ["d_oT1"], semkey=pre + "z")
                wk = sb("wk", [128, 3, 16, 96], BF16); wv = sb("wv", [128, 2, 1024], BF16)
                self.build_wk(wk, wv, pre)
                Cc = sb("Cc", [128, PB, 288]); Ccb = sb("Ccb", [128, PB, 288], BF16); CT = sb("CT", [128, 3, PAST], BF16)
                Vcb = sb("Vcb", [128, PB, 16, 65], BF16)
                qsT = sb("qsT", [96, 16, 128], BF16); ksT = sb("ksT", [96, 16, 128], BF16)
                KaTh = sb("KaTh", [96, PAST], BF16)
                Ps = sb("Ps", [128, 512], BF16); Pn = sb("Pn", [16, 16], BF16)
                r = sb("r", [16, 1]); ob = sb("ob", [16, 64], BF16); oTs = sb("oTs", [64, 16], BF16)
                self.ld(qsT[:], S["qaT"][:, :, NP:NP + 128].rearrange("h p t -> p h t"), ["d_qaT"], [pre + "qsT"])
                self.ld(ksT[:], S["kaTs"].rearrange("h p t -> p h t"), ["d_kaTs"], [pre + "ksT"])
                self.memset("pool", Vcb[:, :, :, 64:65], 1.0, [pre + "Vcb"])
                vsl = [sb("vs%d" % i, [16, D], BF16) for i in range(2)]
                vsx = [sb("vsx%d" % i, [16, 16, 65], BF16) for i in range(2)]
                for i in range(2):
                    self.ld(vsl[i][:], S["v1s"][i * 32:i * 32 + 16, :], ["d_v1s"], [pre + "vs"])
                    self.memset("pool", vsx[i][:, :, 64:65], 1.0, [pre + "vsx"])
                    self.copy("pool", vsx[i][:, :, 0:64], vsl[i][:].rearrange("p (h e) -> p h e", h=16), [pre + "vs", pre + "vsx"], [pre + "vsx"])
                for s in range(2):
                    c0 = s * 32
                    self.ld(Cc[:, :, 0:256], I["cckv"][s].rearrange("(b p) e -> p b e", p=128), [], [pre + "Cc"])
                    self.ld(Cc[:, :, 256:288], I["ckpe"][s].rearrange("(b p) e -> p b e", p=128), [], [pre + "Cc"])
                    self.copy("pool", Ccb[:], Cc[:], [pre + "Cc"], [pre + "Ccb"])
                    for ch in range(3):
                        wd = 128 if ch < 2 else 32
                        for g8 in range(PB // 8):
                            for j in range(8):
                                kb = g8 * 8 + j
                                self.tr(pst[0:wd, j * 128:(j + 1) * 128], Ccb[:, kb, ch * 128:ch * 128 + wd], self.ident[:],
                                        [pre + "Ccb", "ident"], [pre + "pst"])
                            self.copy("act", CT[0:wd, ch, g8 * 1024:(g8 + 1) * 1024], pst[0:wd, :], [pre + "pst"], [pre + "CT"])
                    for kb in range(PB):
                        for n in range(2):
                            pp = Sp[n]
                            for kc in range(2):
                                self.mm(pp[:], CT[:, kc, kb * 128:(kb + 1) * 128], wv[:, kc, n * 512:(n + 1) * 512], kc == 0, kc == 1,
                                        [pre + "CT", pre + "wv"], [pre + "S%d" % n])
                            self.copy("dve" if n else "act", Vcb[:, kb, n * 8:(n + 1) * 8, 0:64],
                                      pp[:].rearrange("p (h e) -> p h e", h=8), [pre + "S%d" % n], [pre + "Vcb"])
                    for h in range(16):
                        for kt in range(PAST // 512):
                            for ch in range(3):
                                wd = 128 if ch < 2 else 32
                                self.mm(Sn[0:96, :], wk[0:wd, ch, h, :], CT[0:wd, ch, kt * 512:(kt + 1) * 512], ch == 0, ch == 2,
                                        [pre + "wk", pre + "CT"], [pre + "Sn"])
                            self.copy("act", KaTh[:, kt * 512:(kt + 1) * 512], Sn[0:96, :], [pre + "Sn"], [pre + "KaTh"])
                        for kb in range(PB):
                            self.mm(Sp[0][:, kb * 16:(kb + 1) * 16], KaTh[:, kb * 128:(kb + 1) * 128], qsT[:, h, c0:c0 + 16], True, True,
                                    [pre + "KaTh", pre + "qsT"], [pre + "S0"])
                        self.act(Ps[:, 0:PB * 16], Sp[0][:, 0:PB * 16], AF.Exp, [pre + "S0"], [pre + "Ps"], scale=scale1)
                        self.mm(Sp[1][0:16, 0:16], ksT[:, h, c0:c0 + 16], qsT[:, h, c0:c0 + 16], True, True,
                                [pre + "ksT", pre + "qsT"], [pre + "S1"])
                        self.act(Pn[:], Sp[1][0:16, 0:16], AF.Exp, [pre + "S1"], [pre + "Pn"], scale=scale1)
                        for kb in range(PB):
                            self.mm(Op_[0][0:16, 0:65], Ps[:, kb * 16:(kb + 1) * 16], Vcb[:, kb, h, :], kb == 0, False,
                                    [pre + "Ps", pre + "Vcb"], [pre + "O0"])
                        self.mm(Op_[0][0:16, 0:65], Pn[:], vsx[s][:, h, :], False, True, [pre + "Pn", pre + "vsx"], [pre + "O0"])
                        self.P.op("dve", lambda e: e.reciprocal(r[:], Op_[0][0:16, 64:65]), [pre + "O0"], [pre + "r"])
                        self.ts("dve", ob[:], Op_[0][0:16, 0:64], r[:, 0:1], None, ALU.mult, None, [pre + "O0", pre + "r"], [pre + "ob"])
                        self.tr(pst[0:64, 0:16], ob[:], self.ident[0:16, 0:16], [pre + "ob", "ident"], [pre + "pst"])
                        self.copy("act", oTs[:], pst[0:64, 0:16], [pre + "pst"], [pre + "oTs"])
                        self.st(S["oT1"][h, :, NP + c0:NP + c0 + 16], oTs[:], [pre + "oTs"], ["d_oT1"], semkey=pre + "sto")

    def build_wk(self, wk, wv, pre):
        I = self.I
        self.memset("pool", wk[:], 0.0, [pre + "wk"])
        w = I["w_ukv"].rearrange("(kc p) (h x) -> p kc h x", p=128, h=16)
        for kc in range(2):
            self.ld(wk[:, kc, :, 0:64], w[:, kc, :, 0:64], [], [pre + "wk"], eng="pool")
            self.ld(wv[:, kc, :].rearrange("p (h e) -> p h e", h=16), w[:, kc, :, 64:128], [], [pre + "wv"], eng="pool")
        for h in range(16):
            self.copy("dve", wk[0:32, 2, h, 64:96], self.ident[0:32, 0:32], ["ident", pre + "wk"], [pre + "wk"])

    def phase_C1(self):
        nc, P, I, O, S = self.nc, self.P, self.I, self.O, self.S
        pre = "C1_"
        with contextlib.ExitStack() as st:
            sb = lambda n, s, d=F32: st.enter_context(nc.sbuf_tensor(pre + n, list(s), d))
            psf = lambda n: st.enter_context(nc.psum_tensor(pre + n, [128, 512], F32))
            wo = sb("wo", [128, 8, D], BF16); wgu = sb("wgu", [128, 8, 2 * FD], BF16); gb = sb("gb", [128, D])
            xt = [sb("xt%d" % i, [128, D]) for i in range(2)]
            T = {"sq": sb("sq", [128, D], BF16), "ss": sb("ss", [128, 1]), "rs": sb("rs", [128, 1]),
                 "hb": sb("hb", [128, D], BF16), "pst": st.enter_context(nc.psum_tensor(pre + "pst", [128, 1024], BF16))}
            oTt = [sb("oTt%d" % i, [128, 8, 512], BF16) for i in range(2)]
            hT = [sb("hT%d" % i, [128, 8, 512], BF16) for i in range(2)]
            sg = [sb("sg%d" % i, [128, 512]) for i in range(2)]
            aT = [sb("aT%d" % i, [128, 22, 512], BF16) for i in range(1)]
            ps = [psf("ps%d" % i) for i in range(6)]
            pi = 0
            self.ld(wo[:], I["w_o0"].rearrange("(kc p) n -> p kc n", p=128), [], [pre + "wo"], eng="pool")
            self.ld(wgu[:], I["w_gu0"].rearrange("(kc p) n -> p kc n", p=128), [], [pre + "wgu"], eng="pool")
            self.ld(gb[:], I["g_ffn0"], [], ["gains"])
            bi = 0
            for ti, (t0, nt) in enumerate(self.tiles):
                hs = ti % 2
                self.ld(oTt[hs][:, :, 0:nt], S["oT"][:, :, t0:t0 + nt].rearrange("h p t -> p h t"), ["d_oT"], [pre + "oTt%d" % hs])
                for b in range(nt // 128):
                    xs = bi % 2; bi += 1
                    r0 = t0 + b * 128
                    xk = pre + "xt%d" % xs
                    self.ld(xt[xs][:], I["xin"][r0:r0 + 128, :], [], [xk])
                    for n in range(2):
                        p_ = ps[pi % 6]; pk = pre + "ps%d" % (pi % 6); pi += 1
                        for h in range(8):
                            self.mm(p_[:], oTt[hs][:, h, b * 128:(b + 1) * 128], wo[:, h, n * 512:(n + 1) * 512], h == 0, h == 7,
                                    [pre + "oTt%d" % hs, pre + "wo"], [pk])
                        self.tt("dve", xt[xs][:, n * 512:(n + 1) * 512], xt[xs][:, n * 512:(n + 1) * 512], p_[:], ALU.add, [xk, pk], [xk])
                    self.st(S["x1"][r0:r0 + 128, :], xt[xs][:], [xk], ["d_x1"], semkey=pre + "stx%d" % xs)
                    self.norm_T(T, xt[xs][:], xk, gb[:], D, None, None, pre)
                    self.copy("act", hT[hs][:, :, b * 128:(b + 1) * 128], T["pst"][:].rearrange("p (k c) -> p k c", k=8),
                              [pre + "pst"], [pre + "hT%d" % hs])
                for j in range(22):
                    pg = ps[pi % 6]; pgk = pre + "ps%d" % (pi % 6); pi += 1
                    pu = ps[pi % 6]; puk = pre + "ps%d" % (pi % 6); pi += 1
                    for kc in range(8):
                        self.mm(pg[:, 0:nt], wgu[:, kc, j * 128:(j + 1) * 128], hT[hs][:, kc, 0:nt], kc == 0, kc == 7,
                                [pre + "wgu", pre + "hT%d" % hs], [pgk])
                    for kc in range(8):
                        self.mm(pu[:, 0:nt], wgu[:, kc, FD + j * 128:FD + (j + 1) * 128], hT[hs][:, kc, 0:nt], kc == 0, kc == 7,
                                [pre + "wgu", pre + "hT%d" % hs], [puk])
                    sgs = j % 2
                    self.act(sg[sgs][:, 0:nt], pg[:, 0:nt], AF.Silu, [pgk], [pre + "sg%d" % sgs])
                    self.tt("dve", aT[0][:, j, 0:nt], sg[sgs][:, 0:nt], pu[:, 0:nt], ALU.mult, [pre + "sg%d" % sgs, puk], [pre + "aT0"])
                self.st(S["actT"][:, :, t0:t0 + nt].rearrange("j p t -> p j t"), aT[0][:, :, 0:nt], [pre + "aT0"], ["d_actT"], semkey=pre + "sta")

    def phase_C2(self):
        nc, P, I, O, S = self.nc, self.P, self.I, self.O, self.S
        NP = self.NP
        pre = "C2_"
        with contextlib.ExitStack() as st:
            sb = lambda n, s, d=F32: st.enter_context(nc.sbuf_tensor(pre + n, list(s), d))
            psf = lambda n: st.enter_context(nc.psum_tensor(pre + n, [128, 512], F32))
            wdn = sb("wdn", [128, 22, D], BF16); wa = sb("wa", [128, 8, 1056], BF16)
            wuq = sb("wuq", [128, 6, 1536], BF16); wuqs = sb("wuqs", [128, 6, 1536], BF16)
            gb = sb("gb", [128, D]); gq = sb("gq", [128, 768]); gkv = sb("gkv", [128, 256])
            csf = sb("csf", [96, 2, 512]); cst = sb("cst", [128, 32])
            xt = [sb("xt%d" % i, [128, D]) for i in range(2)]
            T = {"sq": sb("sq", [128, D], BF16), "ss": sb("ss", [128, 1]), "rs": sb("rs", [128, 1]),
                 "hb": sb("hb", [128, D], BF16), "pst": st.enter_context(nc.psum_tensor(pre + "pst", [128, 1024], BF16))}
            aT = [sb("aT%d" % i, [128, 22, 512], BF16) for i in range(1)]
            hT = sb("hT", [128, 8, 128], BF16)
            af = sb("af", [128, 1056]); ckv = sb("ckv", [128, 256]); kpe = sb("kpe", [128, 32]); rt = sb("rt", [128, 64])
            cpb = sb("cpb", [128, 288], BF16)
            cqT = [sb("cqT%d" % i, [128, 6, 512], BF16) for i in range(2)]
            cpT = [sb("cpT%d" % i, [128, 3, 512], BF16) for i in range(2)]
            qa = [sb("qa%d" % i, [96, 512], BF16) for i in range(2)]
            t1 = sb("t1", [96, 512]); t2 = sb("t2", [96, 512])
            ps = [psf("ps%d" % i) for i in range(6)]
            pi = 0
            self.ld(wdn[:], I["w_dn0"].rearrange("(j p) n -> p j n", p=128), [], [pre + "wdn"], eng="pool")
            self.ld(wa[:], I["w_a"].rearrange("(kc p) n -> p kc n", p=128), [], [pre + "wa"], eng="pool")
            self.ld(wuq[:], I["w_uq"].rearrange("(kc p) n -> p kc n", p=128), [], [pre + "wuq"], eng="pool")
            self.ld(wuqs[:], I["w_uqs"].rearrange("(kc p) n -> p kc n", p=128), [], [pre + "wuqs"], eng="pool")
            self.ld(gb[:], I["g_mix1"], [], ["gains"])
            self.ld(gq[:], I["g_q"], [], [pre + "gq"])
            self.ld(gkv[:], I["g_kv"], [], [pre + "gkv"])
            bi = 0
            qi = 0
            for ti, (t0, nt) in enumerate(self.tiles):
                hs = ti % 2
                for c in range(2):
                    self.ld(csf[64:96, c, 0:nt], I["cs_fm"][c, :, t0:t0 + nt], [], [pre + "csf"])
                self.ld(aT[0][:, :, 0:nt], S["actT"][:, :, t0:t0 + nt].rearrange("j p t -> p j t"), ["d_actT"], [pre + "aT0"])
                for b in range(nt // 128):
                    xs = bi % 2; bi += 1
                    r0 = t0 + b * 128
                    xk = pre + "xt%d" % xs
                    self.ld(xt[xs][:], S["x1"][r0:r0 + 128, :], ["d_x1"], [xk])
                    self.ld(cst[:], I["cs_tm"][r0:r0 + 128, :], [], [pre + "cst"])
                    for n in range(2):
                        p_ = ps[pi % 6]; pk = pre + "ps%d" % (pi % 6); pi += 1
                        for j in range(22):
                            self.mm(p_[:], aT[0][:, j, b * 128:(b + 1) * 128], wdn[:, j, n * 512:(n + 1) * 512], j == 0, j == 21,
                                    [pre + "aT0", pre + "wdn"], [pk])
                        self.tt("dve", xt[xs][:, n * 512:(n + 1) * 512], xt[xs][:, n * 512:(n + 1) * 512], p_[:], ALU.add, [xk, pk], [xk])
                    self.st(S["x2"][r0:r0 + 128, :], xt[xs][:], [xk], ["d_x2"], semkey=pre + "stx%d" % xs)
                    self.norm_T(T, xt[xs][:], xk, gb[:], D, None, None, pre)
                    self.copy("act", hT[:], T["pst"][:].rearrange("p (k c) -> p k c", k=8), [pre + "pst"], [pre + "hT"])
                    for (c0, cw) in ((0, 512), (512, 512), (1024, 32)):
                        p_ = ps[pi % 6]; pk = pre + "ps%d" % (pi % 6); pi += 1
                        for kc in range(8):
                            self.mm(p_[:, 0:cw], hT[:, kc, :], wa[:, kc, c0:c0 + cw], kc == 0, kc == 7, [pre + "hT", pre + "wa"], [pk])
                        self.copy("act" if c0 == 512 else "dve", af[:, c0:c0 + cw], p_[:, 0:cw], [pk], [pre + "af"])
                    self.norm_T(T, af[:, 0:768], pre + "af", gq[:], 768, None, None, pre)
                    self.copy("act", cqT[hs][:, :, b * 128:(b + 1) * 128], T["pst"][:, 0:768].rearrange("p (k c) -> p k c", k=6),
                              [pre + "pst"], [pre + "cqT%d" % hs, pre + "pst"])
                    self.rstd(af[:, 768:1024], 256, T["sq"][:, 0:256], T["ss"][:, 0:1], T["rs"][:, 0:1], pre + "af", pre)
                    self.stt("dve", ckv[:], af[:, 768:1024], T["rs"][:, 0:1], gkv[:], ALU.mult, ALU.mult,
                             [pre + "af", pre + "rs", pre + "gkv"], [pre + "ckv"])
                    self.st(O["ckv"][r0:r0 + 128, :], ckv[:], [pre + "ckv"], [], final=True, semkey=pre + "stc")
                    x1_, x2_ = af[:, 1024:1040], af[:, 1040:1056]
                    co, si_ = cst[:, 0:16], cst[:, 16:32]
                    self.tt("dve", rt[:, 0:16], x1_, co, ALU.mult, [pre + "af", pre + "cst"], [pre + "rt"])
                    self.tt("dve", rt[:, 16:32], x2_, si_, ALU.mult, [pre + "af", pre + "cst", pre + "rt"], [pre + "rt"])
                    self.tt("dve", rt[:, 32:48], x2_, co, ALU.mult, [pre + "af", pre + "cst", pre + "rt"], [pre + "rt"])
                    self.tt("dve", rt[:, 48:64], x1_, si_, ALU.mult, [pre + "af", pre + "cst", pre + "rt"], [pre + "rt"])
                    self.tt("dve", kpe[:, 0:16], rt[:, 0:16], rt[:, 16:32], ALU.subtract, [pre + "rt"], [pre + "kpe"])
                    self.tt("dve", kpe[:, 16:32], rt[:, 32:48], rt[:, 48:64], ALU.add, [pre + "rt", pre + "kpe"], [pre + "kpe"])
                    self.st(O["kpe"][r0:r0 + 128, :], kpe[:], [pre + "kpe"], [], final=True, semkey=pre + "stp")
                    self.copy("pool", cpb[:, 0:256], ckv[:], [pre + "ckv"], [pre + "cpb"])
                    self.copy("pool", cpb[:, 256:288], kpe[:], [pre + "kpe", pre + "cpb"], [pre + "cpb"])
                    for ch in range(3):
                        wd = 128 if ch < 2 else 32
                        self.tr(T["pst"][0:wd, ch * 128:(ch + 1) * 128], cpb[:, ch * 128:ch * 128 + wd], self.ident[:],
                                [pre + "cpb", "ident", pre + "pst"], [pre + "pst"])
                    self.copy("act", cpT[hs][:, 0:2, b * 128:(b + 1) * 128], T["pst"][:, 0:256].rearrange("p (k c) -> p k c", k=2),
                              [pre + "pst"], [pre + "cpT%d" % hs, pre + "pst"])
                    self.copy("act", cpT[hs][0:32, 2, b * 128:(b + 1) * 128], T["pst"][0:32, 256:384],
                              [pre + "pst"], [pre + "cpT%d" % hs, pre + "pst"])
                if t0 < NP:
                    for ch in range(3):
                        wd = 128 if ch < 2 else 32
                        self.st(S["cpl"][ch][t0 // self.CW][0:wd, t0 % self.CW:t0 % self.CW + nt], cpT[hs][0:wd, ch, 0:nt], [pre + "cpT%d" % hs], ["d_cpl"], semkey=pre + "stcp%d" % hs)
                else:
                    self.st(S["cps"][0:2].rearrange("c p t -> p c t"), cpT[hs][:, 0:2, 0:nt], [pre + "cpT%d" % hs], ["d_cps"], semkey=pre + "stcp%d" % hs)
                    self.st(S["cps"][2, 0:32, :], cpT[hs][0:32, 2, 0:nt], [pre + "cpT%d" % hs], ["d_cps"], semkey=pre + "stcp%d" % hs)
                for h in range(16):
                    pa = ps[pi % 6]; pak = pre + "ps%d" % (pi % 6); pi += 1
                    pb_ = ps[pi % 6]; pbk = pre + "ps%d" % (pi % 6); pi += 1
                    for kc in range(6):
                        self.mm(pa[0:96, 0:nt], wuq[:, kc, h * 96:(h + 1) * 96], cqT[hs][:, kc, 0:nt], kc == 0, kc == 5,
                                [pre + "wuq", pre + "cqT%d" % hs], [pak])
                    for kc in range(6):
                        self.mm(pb_[0:96, 0:nt], wuqs[:, kc, h * 96:(h + 1) * 96], cqT[hs][:, kc, 0:nt], kc == 0, kc == 5,
                                [pre + "wuqs", pre + "cqT%d" % hs], [pbk])
                    qs = qi % 2; qi += 1
                    qk_ = pre + "qa%d" % qs
                    self.copy("act", qa[qs][0:64, 0:nt], pa[0:64, 0:nt], [pak], [qk_])
                    self.tt("dve", t1[64:96, 0:nt], pa[64:96, 0:nt], csf[64:96, 0, 0:nt], ALU.mult, [pak, pre + "csf"], [pre + "t1"])
                    self.tt("dve", t2[64:96, 0:nt], pb_[64:96, 0:nt], csf[64:96, 1, 0:nt], ALU.mult, [pbk, pre + "csf"], [pre + "t2"])
                    self.tt("pool", qa[qs][64:96, 0:nt], t1[64:96, 0:nt], t2[64:96, 0:nt], ALU.add, [pre + "t1", pre + "t2", qk_], [qk_])
                    self.st(S["qaT"][h, :, t0:t0 + nt], qa[qs][:, 0:nt], [qk_], ["d_qaT"], semkey=pre + "stq%d" % qs)

    def phase_D0(self):
        nc, P, I, O, S = self.nc, self.P, self.I, self.O, self.S
        NP = self.NP
        pre = "D0_"
        with contextlib.ExitStack() as st:
            sb = lambda n, s, d=F32: st.enter_context(nc.sbuf_tensor(pre + n, list(s), d))
            psf = lambda n: st.enter_context(nc.psum_tensor(pre + n, [128, 512], F32))
            wk = sb("wk", [128, 3, 16, 96], BF16); wv = sb("wv", [128, 2, 1024], BF16)
            self.build_wk(wk, wv, pre)
            cT = [sb("cT%d" % i, [128, 3, 512], BF16) for i in range(2)]
            ka = [sb("ka%d" % i, [96, 16, 512], BF16) for i in range(2)]
            vt = [sb("vt%d" % i, [128, D], BF16) for i in range(2)]
            ps = [psf("ps%d" % i) for i in range(6)]
            pi = 0
            jobs = []
            for rk in range(4):
                for t0 in range(0, NP, 512):
                    jobs.append(("g", rk, t0, 512))
            for t0 in range(0, NP, 512):
                jobs.append(("l", 0, t0, 512))
            jobs.append(("s", 0, 0, 128))
            vi = 0
            for ji, (kind, rk, t0, nt) in enumerate(jobs):
                cs = ji % 2
                ck = pre + "cT%d" % cs
                if kind == "g":
                    for ch in range(3):
                        wd = 128 if ch < 2 else 32
                        self.ld(cT[cs][0:wd, ch, :], S["cpg"][ch][t0 // self.CW][rk * 128:rk * 128 + wd, t0 % self.CW:t0 % self.CW + 512], ["d_cpg"], [ck])
                elif kind == "l":
                    for ch in range(3):
                        wd = 128 if ch < 2 else 32
                        self.ld(cT[cs][0:wd, ch, :], S["cpl"][ch][t0 // self.CW][0:wd, t0 % self.CW:t0 % self.CW + 512], ["d_cpl"], [ck])
                else:
                    self.ld(cT[cs][:, 0:2, 0:128], S["cps"][0:2].rearrange("c p t -> p c t"), ["d_cps"], [ck])
                    self.ld(cT[cs][0:32, 2, 0:128], S["cps"][2, 0:32, :], ["d_cps"], [ck])
                kk = pre + "ka%d" % cs
                for h in range(16):
                    p_ = ps[pi % 6]; pk = pre + "ps%d" % (pi % 6); pi += 1
                    for ch in range(3):
                        wd = 128 if ch < 2 else 32
                        self.mm(p_[0:96, 0:nt], wk[0:wd, ch, h, :], cT[cs][0:wd, ch, 0:nt], ch == 0, ch == 2, [pre + "wk", ck], [pk])
                    self.copy("act" if h % 2 else "dve", ka[cs][:, h, 0:nt], p_[0:96, 0:nt], [pk], [kk])
                if kind == "g":
                    self.st(S["kaT"][:, :, rk * NP + t0:rk * NP + t0 + 512].rearrange("h p t -> p h t"), ka[cs][:], [kk], ["d_kaT"], semkey=pre + "stk%d" % cs)
                elif kind == "l":
                    self.st(S["kaTl"][:, :, t0:t0 + 512].rearrange("h p t -> p h t"), ka[cs][:], [kk], ["d_kTl"], semkey=pre + "stk%d" % cs)
                else:
                    self.st(S["kaTs"].rearrange("h p t -> p h t"), ka[cs][:, :, 0:128], [kk], ["d_kaTs"], semkey=pre + "stk%d" % cs)
                for b in range(nt // 128):
                    vs_ = vi % 2; vi += 1
                    vk = pre + "vt%d" % vs_
                    for n in range(2):
                        p_ = ps[pi % 6]; pk = pre + "ps%d" % (pi % 6); pi += 1
                        for kc in range(2):
                            self.mm(p_[:], cT[cs][:, kc, b * 128:(b + 1) * 128], wv[:, kc, n * 512:(n + 1) * 512], kc == 0, kc == 1,
                                    [ck, pre + "wv"], [pk])
                        self.copy("act" if n else "dve", vt[vs_][:, n * 512:(n + 1) * 512], p_[:], [pk], [vk])
                    r0 = t0 + b * 128
                    if kind == "g":
                        self.st(S["v1"][rk * NP + r0:rk * NP + r0 + 128, :], vt[vs_][:], [vk], ["d_v1"], semkey=pre + "stv%d" % vs_)
                    elif kind == "l":
                        self.st(S["v1l"][r0:r0 + 128, :], vt[vs_][:], [vk], ["d_vl"], semkey=pre + "stv%d" % vs_)
                    else:
                        self.st(S["v1s"], vt[vs_][:], [vk], ["d_v1s"], semkey=pre + "stv%d" % vs_)

    def phase_E1(self):
        nc, P, I, O, S = self.nc, self.P, self.I, self.O, self.S
        pre = "E1_"
        with contextlib.ExitStack() as st:
            sb = lambda n, s, d=F32: st.enter_context(nc.sbuf_tensor(pre + n, list(s), d))
            psf = lambda n: st.enter_context(nc.psum_tensor(pre + n, [128, 512], F32))
            wo = sb("wo", [64, 16, D], BF16); wr = sb("wr", [128, 8, 8], BF16); gb = sb("gb", [128, D])
            xt = [sb("xt%d" % i, [128, D]) for i in range(2)]
            T = {"sq": sb("sq", [128, D], BF16), "ss": sb("ss", [128, 1]), "rs": sb("rs", [128, 1]),
                 "hb": sb("hb", [128, D], BF16), "pst": st.enter_context(nc.psum_tensor(pre + "pst", [128, 1024], BF16))}
            oTt = [sb("oTt%d" % i, [64, 16, 512], BF16) for i in range(2)]
            hT = [sb("hT%d" % i, [128, 8, 128], BF16) for i in range(2)]
            lg = sb("lg", [128, 8]); m1 = sb("m1", [128, 1]); m2 = sb("m2", [128, 1]); eq = sb("eq", [128, 8]); l2 = sb("l2", [128, 8])
            ex = sb("ex", [128, 8]); sm = sb("sm", [128, 1]); cb = [sb("cb%d" % i, [128, 8]) for i in range(2)]
            ps = [psf("ps%d" % i) for i in range(6)]
            pi = 0
            self.ld(wo[:], I["w_o1"].rearrange("(h p) n -> p h n", p=64), [], [pre + "wo"], eng="pool")
            self.ld(wr[:], I["w_r"].rearrange("(kc p) n -> p kc n", p=128), [], [pre + "wr"], eng="pool")
            self.ld(gb[:], I["g_ffn1"], [], ["gains"])
            bi = 0
            for ti, (t0, nt) in enumerate(self.tiles):
                hs = ti % 2
                self.ld(oTt[hs][:, :, 0:nt], S["oT1"][:, :, t0:t0 + nt].rearrange("h p t -> p h t"), ["d_oT1"], [pre + "oTt%d" % hs])
                for b in range(nt // 128):
                    xs = bi % 2; bi += 1
                    r0 = t0 + b * 128
                    xk = pre + "xt%d" % xs
                    self.ld(xt[xs][:], S["x2"][r0:r0 + 128, :], ["d_x2"], [xk])
                    for n in range(2):
                        p_ = ps[pi % 6]; pk = pre + "ps%d" % (pi % 6); pi += 1
                        for h in range(16):
                            self.mm(p_[:], oTt[hs][:, h, b * 128:(b + 1) * 128], wo[:, h, n * 512:(n + 1) * 512], h == 0, h == 15,
                                    [pre + "oTt%d" % hs, pre + "wo"], [pk])
                        self.tt("dve", xt[xs][:, n * 512:(n + 1) * 512], xt[xs][:, n * 512:(n + 1) * 512], p_[:], ALU.add, [xk, pk], [xk])
                    self.st(S["x3"][r0:r0 + 128, :], xt[xs][:], [xk], ["d_x3"], semkey=pre + "stx%d" % xs)
                    self.norm_T(T, xt[xs][:], xk, gb[:], D, None, None, pre)
                    hk = pre + "hT%d" % xs
                    self.copy("act", hT[xs][:], T["pst"][:].rearrange("p (k c) -> p k c", k=8), [pre + "pst"], [hk])
                    self.st(S["hT"][:, :, r0:r0 + 128].rearrange("k p t -> p k t"), hT[xs][:], [hk], ["d_hT"], semkey=pre + "sth%d" % xs)
                    p_ = ps[pi % 6]; pk = pre + "ps%d" % (pi % 6); pi += 1
                    for kc in range(8):
                        self.mm(p_[:, 0:8], hT[xs][:, kc, :], wr[:, kc, :], kc == 0, kc == 7, [hk, pre + "wr"], [pk])
                    self.copy("dve", lg[:], p_[:, 0:8], [pk], [pre + "lg"])
                    self.P.op("dve", lambda e: e.reduce_max(m1[:], lg[:], AX.X), [pre + "lg"], [pre + "m1"])
                    self.ts("dve", eq[:], lg[:], m1[:, 0:1], -1e30, ALU.is_equal, ALU.mult, [pre + "lg", pre + "m1"], [pre + "eq"])
                    self.tt("dve", l2[:], lg[:], eq[:], ALU.add, [pre + "lg", pre + "eq"], [pre + "l2"])
                    self.P.op("dve", lambda e: e.reduce_max(m2[:], l2[:], AX.X), [pre + "l2"], [pre + "m2"])
                    self.ts("dve", eq[:], lg[:], m2[:, 0:1], None, ALU.is_ge, None, [pre + "lg", pre + "m2", pre + "eq"], [pre + "eq"])
                    self.ts("dve", l2[:], lg[:], m1[:, 0:1], None, ALU.subtract, None, [pre + "lg", pre + "m1", pre + "l2"], [pre + "l2"])
                    self.act(ex[:], l2[:], AF.Exp, [pre + "l2"], [pre + "ex"])
                    self.tt("dve", ex[:], ex[:], eq[:], ALU.mult, [pre + "ex", pre + "eq"], [pre + "ex"])
                    self.P.op("dve", lambda e: e.reduce_sum(sm[:], ex[:], AX.X), [pre + "ex"], [pre + "sm"])
                    self.P.op("dve", lambda e: e.reciprocal(sm[:], sm[:]), [pre + "sm"], [pre + "sm"])
                    ck = pre + "cb%d" % xs
                    self.ts("dve", cb[xs][:], ex[:], sm[:, 0:1], None, ALU.mult, None, [pre + "ex", pre + "sm"], [ck])
                    self.st(S["comb"][r0:r0 + 128, :], cb[xs][:], [ck], ["d_comb"], semkey=pre + "stc%d" % xs)

    def phase_E2(self):
        nc, P, I, O, S = self.nc, self.P, self.I, self.O, self.S
        NBLK = self.NBLK
        pre = "E2_"
        GB = 11 if NBLK % 11 == 0 else NBLK
        NG = NBLK // GB
        GT = GB * 128
        widths = []
        o_ = 0
        while o_ < GT:
            w_ = min(512, GT - o_); widths.append((o_, w_)); o_ += w_
        QF = FE // 4
        NCH = QF // 128
        with contextlib.ExitStack() as st:
            sb = lambda n, s, d=F32: st.enter_context(nc.sbuf_tensor(pre + n, list(s), d))
            psf = lambda n: st.enter_context(nc.psum_tensor(pre + n, [128, 512], F32))
            hT = sb("hT", [128, 8, GT], BF16)
            yacc = sb("yacc", [128, GB, D])
            cb = sb("cb", [128, GB, 8])
            wgu = [sb("wgu%d" % i, [128, 8, 2, QF], BF16) for i in range(2)]
            wdn = [sb("wdn%d" % i, [128, NCH, D], BF16) for i in range(2)]
            sg = [sb("sg%d" % i, [128, 512]) for i in range(2)]
            aT = [sb("aT%d" % i, [128, NCH, 512], BF16) for i in range(2)]
            gb = sb("gb", [128, D]); sq = sb("sq", [128, D], BF16); ss = sb("ss", [128, 1]); rs = sb("rs", [128, 1])
            yo = [sb("yo%d" % i, [128, D]) for i in range(2)]
            ps = [psf("ps%d" % i) for i in range(7)]
            pi = 0
            self.ld(gb[:], I["g_fin"], [], ["gains"])
            ui = 0
            ai = 0
            for gi in range(NG):
                g0 = gi * GT
                self.ld(hT[:], S["hT"][:, :, g0:g0 + GT].rearrange("k p t -> p k t"), ["d_hT"], [pre + "hT"])
                self.ld(cb[:], S["comb"][g0:g0 + GT, :].rearrange("(b p) e -> p b e", p=128), ["d_comb"], [pre + "cb"])
                self.ld(yacc[:], S["x3"][g0:g0 + GT, :].rearrange("(b p) e -> p b e", p=128), ["d_x3"], [pre + "yacc%d" % b_ for b_ in range(GB)])
                for e_ in range(NE):
                    for q in range(4):
                        ws = ui % 2; ui += 1
                        wgk, wdk = pre + "wgu%d" % ws, pre + "wdn%d" % ws
                        src = I["w_gu1"][e_].rearrange("(kc p) (two f) -> p kc two f", p=128, two=2)
                        for two in range(2):
                            self.ld(wgu[ws][:, :, two, :], src[:, :, two, q * QF:(q + 1) * QF], [], [wgk], eng="pool")
                        self.ld(wdn[ws][:], I["w_dn1"][e_, q * QF:(q + 1) * QF, :].rearrange("(j p) n -> p j n", p=128), [], [wdk], eng="pool")
                        for (o0, wd) in widths:
                            as_ = ai % 2; ai += 1
                            ak = pre + "aT%d" % as_
                            for j in range(NCH):
                                pg = ps[pi % 7]; pgk = pre + "ps%d" % (pi % 7); pi += 1
                                pu = ps[pi % 7]; puk = pre + "ps%d" % (pi % 7); pi += 1
                                for kc in range(8):
                                    self.mm(pg[:, 0:wd], wgu[ws][:, kc, 0, j * 128:(j + 1) * 128], hT[:, kc, o0:o0 + wd], kc == 0, kc == 7,
                                            [wgk, pre + "hT"], [pgk])
                                for kc in range(8):
                                    self.mm(pu[:, 0:wd], wgu[ws][:, kc, 1, j * 128:(j + 1) * 128], hT[:, kc, o0:o0 + wd], kc == 0, kc == 7,
                                            [wgk, pre + "hT"], [puk])
                                sgs = j % 2
                                self.act(sg[sgs][:, 0:wd], pg[:, 0:wd], AF.Silu, [pgk], [pre + "sg%d" % sgs])
                                self.tt("dve", aT[as_][:, j, 0:wd], sg[sgs][:, 0:wd], pu[:, 0:wd], ALU.mult, [pre + "sg%d" % sgs, puk], [ak])
                            for b in range(wd // 128):
                                blk = o0 // 128 + b
                                for n in range(2):
                                    p_ = ps[pi % 7]; pk = pre + "ps%d" % (pi % 7); pi += 1
                                    for j in range(NCH):
                                        self.mm(p_[:], aT[as_][:, j, b * 128:(b + 1) * 128], wdn[ws][:, j, n * 512:(n + 1) * 512], j == 0, j == NCH - 1,
                                                [ak, wdk], [pk])
                                    self.stt("dve", yacc[:, blk, n * 512:(n + 1) * 512], p_[:], cb[:, blk, e_:e_ + 1],
                                             yacc[:, blk, n * 512:(n + 1) * 512], ALU.mult, ALU.add, [pk, pre + "cb", pre + "yacc%d" % blk], [pre + "yacc%d" % blk])
                for blk in range(GB):
                    ys = blk % 2
                    x = yacc[:, blk, :]
                    self.memset("pool", ss[:], 0.0, [pre + "ss"])
                    self.act(sq[:], x, AF.Square, [pre + "yacc%d" % blk, pre + "ss"], [pre + "sq", pre + "ss"], accum_out=ss[:])
                    self.act(rs[:], ss[:], AF.Ln, [pre + "ss"], [pre + "rs"], scale=1.0 / D, bias=EPS)
                    self.act(rs[:], rs[:], AF.Exp, [pre + "rs"], [pre + "rs"], scale=-0.5)
                    self.stt("dve", yo[ys][:], x, rs[:, 0:1], gb[:], ALU.mult, ALU.mult, [pre + "yacc%d" % blk, pre + "rs", "gains"], [pre + "yo%d" % ys])
                    r0 = g0 + blk * 128
                    self.st(O["y"][r0:r0 + 128, :], yo[ys][:], [pre + "yo%d" % ys], [], final=True, semkey=pre + "sty%d" % ys)


def host_tables(T, PAST, r):
    NB = T // 2048
    NKB = 16 * NB
    NP = NB * 512
    NBLKP = NB * 4
    f = np.float32
    k = np.arange(128)[:, None]
    q = np.arange(512)[None, :]
    T0 = (-(q - k)).astype(f)
    Dneg = np.zeros((128, 4, 512), f)
    Dmask = np.zeros((128, 4, 512), f)
    for i in range(4):
        kp = 128 * i + k
        vis = (kp // 64) <= (q // 64)
        Dneg[:, i, :] = np.where(vis, -np.abs(q - kp), -1e30)
        Dmask[:, i, :] = np.where(vis, 0.0, -1e30)
    ct = np.zeros((8, NB, NKB), f)
    mt = np.zeros((NB, NKB), f)
    for J in range(NB):
        gq = 4 * J + r
        for rk in range(4):
            for Jk in range(NB):
                gk = 4 * Jk + rk
                for i in range(4):
                    kbi = rk * NBLKP + Jk * 4 + i
                    if gk < gq:
                        dlt = (gq - gk) * 512 - 128 * i
                        for h in range(8):
                            ct[h, J, kbi] = -SLOPES[h] * dlt
                        mt[J, kbi] = 0.0
                    else:
                        ct[:, J, kbi] = -30000.0
                        mt[J, kbi] = -30000.0
    ctab0 = np.ascontiguousarray(np.broadcast_to(ct.reshape(1, -1), (128, 8 * NB * NKB)))
    mtab = np.ascontiguousarray(np.broadcast_to(mt.reshape(1, -1), (128, NB * NKB)))
    NTOK = (NBLKP + 1) * 128
    pos = np.zeros(NTOK, np.int64)
    for J in range(NB):
        pos[J * 512:(J + 1) * 512] = (4 * J + r) * 512 + np.arange(512)
    for s in range(2):
        pos[NP + s * 32:NP + s * 32 + 16] = PAST + np.arange(16)
    half = 16
    freqs = np.power(np.float32(10000.0), -np.arange(half, dtype=f) * np.float32(2.0) / np.float32(32)).astype(f)
    ang = pos.astype(f)[:, None] * freqs[None, :]
    co, si = np.cos(ang).astype(f), np.sin(ang).astype(f)
    cs_tm = np.concatenate([co, si], 1).astype(f)
    cs_fm = np.stack([np.concatenate([co.T, co.T], 0), np.concatenate([-si.T, si.T], 0)], 0).astype(f)
    PB = PAST // 128
    kb = np.arange(PB)[None, :, None]
    ii = np.arange(16)[None, None, :]
    Dnegs = (-(PAST + ii - 128 * kb - k[:, :, None])).astype(f).reshape(128, PB * 16)
    kn = np.arange(16)[:, None]
    Dnegn = (-np.abs(np.arange(16)[None, :] - kn)).astype(f)
    return dict(T0=T0, Dneg=Dneg, Dmask=Dmask, ctab0=ctab0, mtab=mtab, cs_tm=cs_tm, cs_fm=np.ascontiguousarray(cs_fm),
                Dnegs=np.ascontiguousarray(Dnegs), Dnegn=Dnegn, ident=np.eye(128, dtype=f))


_CACHE = {}


def run(inputs, T, PAST, dbg=()):
    f = np.float32
    A = {k_: np.asarray(v) for k_, v in inputs.items()}
    NB = T // 2048
    NP = NB * 512
    NTOK = (NB * 4 + 1) * 128
    key = (T, PAST, tuple(dbg))
    if key not in _CACHE:
        _CACHE[key] = Builder(T, PAST, dbg).build()
    nc = _CACHE[key]
    bc = lambda v, n=128: np.ascontiguousarray(np.broadcast_to(np.asarray(v, f).reshape(1, -1), (n, np.asarray(v).size)))
    w_uq = A["mla_w_uq"][0]
    wq4 = w_uq.reshape(768, 16, 96)
    w_uqs = np.concatenate([wq4[:, :, 0:64], wq4[:, :, 80:96], wq4[:, :, 64:80]], axis=2).reshape(768, 1536)
    common = dict(
        g_mix0=bc(A["norm_mix"][0]), g_mix1=bc(A["norm_mix"][1]), g_ffn0=bc(A["norm_ffn"][0]), g_ffn1=bc(A["norm_ffn"][1]),
        g_fin=bc(A["norm_final"]), lam=bc(A["diff_lambda"][0].reshape(-1)), gsub_col=np.ascontiguousarray(A["diff_subln"][0].reshape(128, 1)),
        gsub_row=bc(A["diff_subln"][0]), g_q=bc(A["mla_norm_q"][0]), g_kv=bc(A["mla_norm_kv"][0]),
        w_qkv=A["diff_w_qkv"][0], w_o0=A["diff_w_o"][0], w_gu0=A["ffn_w_gu"][0], w_dn0=A["ffn_w_down"][0],
        w_a=A["mla_w_a"][0], w_uq=w_uq, w_uqs=np.ascontiguousarray(w_uqs), w_ukv=A["mla_w_ukv"][0], w_o1=A["mla_w_o"][0],
        w_r=A["moe_router"][0], w_gu1=A["moe_w_gu"][0], w_dn1=A["moe_w_down"][0])
    in_maps = []
    for c in range(8):
        b, r = c // 4, c % 4
        xin = np.zeros((NTOK, D), f)
        xp = A["x_prompt"][b].reshape(T // 512, 512, D)
        xin[:NP] = xp[r::4].reshape(NP, D)
        for s in range(2):
            xin[NP + s * 32:NP + s * 32 + 16] = A["x_sample"][2 * c + s]
        m = dict(common)
        m.update(host_tables(T, PAST, r))
        m["xin"] = xin
        m["cdk"] = np.ascontiguousarray(A["cache_diff_k"][0, 2 * c:2 * c + 2].reshape(2, PAST, D))
        m["cdv"] = np.ascontiguousarray(A["cache_diff_v"][0, 2 * c:2 * c + 2].reshape(2, PAST, D))
        m["cckv"] = np.ascontiguousarray(A["cache_mla_ckv"][0, 2 * c:2 * c + 2])
        m["ckpe"] = np.ascontiguousarray(A["cache_mla_kpe"][0, 2 * c:2 * c + 2])
        in_maps.append(m)
    res = run_bass_kernel_spmd(nc, in_maps, core_ids=list(range(8)))
    B = 2
    outs = {}
    shapes = dict(y=D, dk=D, dv=D, ckv=256, kpe=32)
    for name, wdt in shapes.items():
        pr = np.zeros((B, T // 512, 512, wdt), f)
        sm = np.zeros((16, 16, wdt), f)
        for c in range(8):
            b, r = c // 4, c % 4
            o = res.results[c][name]
            pr[b, r::4] = o[:NP].reshape(NB, 512, wdt)
            for s in range(2):
                sm[2 * c + s] = o[NP + s * 32:NP + s * 32 + 16]
        outs[name] = (pr.reshape(B, T, wdt), sm)
    dbgout = {n: [res.results[c][n] for c in range(8)] for n in dbg}
    y_p, y_s = outs["y"]
    dk_p, dk_s = outs["dk"]
    dv_p, dv_s = outs["dv"]
    ck_p, ck_s = outs["ckv"]
    kp_p, kp_s = outs["kpe"]
    out = (y_p, y_s, dk_p.reshape(1, B, T, 8, 128), dv_p.reshape(1, B, T, 8, 128), ck_p.reshape(1, B, T, 256), kp_p.reshape(1, B, T, 32),
           dk_s.reshape(1, 16, 16, 8, 128), dv_s.reshape(1, 16, 16, 8, 128), ck_s.reshape(1, 16, 16, 256), kp_s.reshape(1, 16, 16, 32))
    if dbg:
        return out, dbgout
    return out


def kernel(**inputs):
    T = inputs["x_prompt"].shape[1]
    PAST = inputs["cache_diff_k"].shape[2]
    return run(inputs, T, PAST)
```

```python
import contextlib
import math
import numpy as np
import concourse.bass as bass
import concourse.mybir as mybir
from concourse.bass_utils import run_bass_kernel_spmd

F32 = mybir.dt.float32
BF16 = mybir.dt.bfloat16
ALU = mybir.AluOpType
AF = mybir.ActivationFunctionType
AX = mybir.AxisListType
ENGS = ("pe", "act", "dve", "pool", "sp")


class Op:
    __slots__ = ("eng", "fn", "reads", "writes", "is_dma", "semkey", "waits", "sig", "signal")

    def __init__(self, eng, fn, reads, writes, is_dma, semkey):
        self.eng, self.fn, self.reads, self.writes = eng, fn, reads, writes
        self.is_dma, self.semkey = is_dma, semkey
        self.waits = {}
        self.sig = None
        self.signal = False


class Prog:
    def __init__(self, nc):
        self.nc = nc
        self.ops = []
        self.last_writer = {}
        self.readers = {}
        self.dma_count = {}
        self.final_dma = {}
        self.pending = {}
        self.last_op = {}
        self.sem_of = {}
        self.phys_count = []

    def barrier(self):
        lasts = [o for o in self.last_op.values()]
        for o in lasts:
            o.signal = True
        dm = {i: v for i, v in enumerate(self.phys_count)}
        for e in ENGS:
            self.pending[e] = (list(lasts), dict(dm))
        self.last_writer = {}
        self.readers = {}
        self.sem_of = {}

    def _add(self, op):
        deps = []
        for r in op.reads:
            w = self.last_writer.get(r)
            if w is not None:
                deps.append(w)
        for r in op.writes:
            w = self.last_writer.get(r)
            if w is not None:
                deps.append(w)
            deps.extend(self.readers.get(r, ()))
        pend = self.pending.pop(op.eng, None)
        if pend is not None:
            for d in pend[0]:
                if d.eng != op.eng:
                    op.waits.setdefault("_dep", []).append(d)
            for k, v in pend[1].items():
                kk = "dma:%s" % (k,)
                op.waits[kk] = max(op.waits.get(kk, 0), v)
        for d in deps:
            if d is op:
                continue
            if d.eng == "pe" and op.eng == "pe" and not d.is_dma and not op.is_dma:
                continue
            d.signal = True
            if d.is_dma:
                key = "dma:%s" % (d.semkey,)
                op.waits[key] = max(op.waits.get(key, 0), self.phys_count[d.semkey])
            else:
                op.waits.setdefault("_dep", []).append(d)
        for r in op.reads:
            self.readers.setdefault(r, []).append(op)
        for r in op.writes:
            self.last_writer[r] = op
            self.readers[r] = []
        self.ops.append(op)
        if not op.is_dma:
            self.last_op[op.eng] = op
        return op

    def op(self, eng, fn, reads=(), writes=()):
        return self._add(Op(eng, fn, tuple(reads), tuple(writes), False, None))

    def dma(self, eng, out, in_, reads=(), writes=(), semkey=None, final=False, **kw):
        if semkey is None:
            semkey = writes[0] if writes else reads[0]
        if semkey not in self.sem_of:
            self.sem_of[semkey] = len(self.sem_of)
            if len(self.phys_count) < len(self.sem_of):
                self.phys_count.append(0)
        phys = self.sem_of[semkey]
        o = Op(eng, (lambda e, out=out, in_=in_, kw=kw: e.dma_start(out=out, in_=in_, **kw)),
               tuple(reads), tuple(writes), True, phys)
        r = self._add(o)
        self.phys_count[phys] += 16
        o.sig = ("dma:%s" % (phys,), self.phys_count[phys])
        return r

    def emit(self, final_engine="sp"):
        nc = self.nc
        cnt = {e: 0 for e in ENGS}
        for o in self.ops:
            if not o.is_dma and o.signal:
                cnt[o.eng] += 1
                o.sig = ("eng:%s" % o.eng, cnt[o.eng])
        for o in self.ops:
            for d in o.waits.pop("_dep", []):
                k, v = d.sig
                o.waits[k] = max(o.waits.get(k, 0), v)
        semnames = set()
        for o in self.ops:
            if o.sig is not None:
                semnames.add(o.sig[0])
            semnames.update(o.waits.keys())
        semnames = sorted(semnames)
        self.n_sems = len(semnames)
        with contextlib.ExitStack() as st:
            sems = {n: st.enter_context(nc.semaphore("s%d" % i)) for i, n in enumerate(semnames)}
            block = st.enter_context(nc.Block())
            per = {e: [o for o in self.ops if o.eng == e] for e in ENGS}
            final_dma = {i: v for i, v in enumerate(self.phys_count)}

            def run(engname, e):
                waited = {}
                for o in per[engname]:
                    for k, v in o.waits.items():
                        if waited.get(k, 0) >= v:
                            continue
                        e.wait_ge(sems[k], v)
                        waited[k] = v
                    ins = o.fn(e)
                    if o.sig is not None and (o.signal or o.is_dma):
                        ins.then_inc(sems[o.sig[0]], 16 if o.is_dma else 1)
                if engname == final_engine:
                    for key, v in final_dma.items():
                        k = "dma:%s" % (key,)
                        if waited.get(k, 0) < v:
                            e.wait_ge(sems[k], v)

            block.tensor(lambda e: run("pe", e))
            block.scalar(lambda e: run("act", e))
            block.vector(lambda e: run("dve", e))
            block.gpsimd(lambda e: run("pool", e))
            block.sync(lambda e: run("sp", e))


D = 1024
EPS = 1e-6
SLOPES = [2.0 ** (-(h + 1)) for h in range(8)]
LAM_INIT0 = 0.8 - 0.6 * math.exp(-0.3 * 0)
FD = 2816
FE = 3584
NE = 8


class Builder:
    def __init__(self, T, PAST, dbg=()):
        self.T, self.PAST = T, PAST
        self.NB = T // 2048
        self.NP = self.NB * 512
        self.NBLKP = self.NB * 4
        self.NBLK = self.NBLKP + 1
        self.NTOK = self.NBLK * 128
        self.PB = PAST // 128
        self.NKB = 16 * self.NB
        self.dbg = set(dbg)
        self.nc = bass.Bass("TRN2", target_bir_lowering=False)
        self.P = Prog(self.nc)
        self.tiles = [(j * 512, 512) for j in range(self.NB)] + [(self.NP, 128)]
        self.uid = 0

    def din(self, name, shape, dt=F32):
        return self.nc.dram_tensor(name, list(shape), dt, kind="ExternalInput").ap()

    def dout(self, name, shape, dt=F32):
        return self.nc.dram_tensor(name, list(shape), dt, kind="ExternalOutput").ap()

    def dscr(self, name, shape, dt=BF16):
        if name in self.dbg:
            return self.nc.dram_tensor(name, list(shape), dt, kind="ExternalOutput").ap()
        return self.nc.dram_tensor(name, list(shape), dt).ap()

    def mm(self, out, lhsT, rhs, start, stop, reads, writes):
        self.P.op("pe", lambda e: e.matmul(out, lhsT, rhs, start=start, stop=stop), reads, writes)

    def tr(self, out, in_, ident, reads, writes):
        self.P.op("pe", lambda e: e.transpose(out, in_, ident), reads, writes)

    def act(self, out, in_, func, reads, writes, **kw):
        self.P.op("act", lambda e: e.activation(out, in_, func, **kw), reads, writes)

    def copy(self, eng, out, in_, reads, writes):
        if eng == "act":
            self.P.op("act", lambda e: e.copy(out, in_), reads, writes)
        else:
            self.P.op(eng, lambda e: e.tensor_copy(out, in_), reads, writes)

    def tt(self, eng, out, a, b, op, reads, writes):
        self.P.op(eng, lambda e: e.tensor_tensor(out, a, b, op), reads, writes)

    def ts(self, eng, out, a, s1, s2, op0, op1, reads, writes):
        if s2 is None:
            self.P.op(eng, lambda e: e.tensor_scalar(out, a, s1, None, op0), reads, writes)
        else:
            self.P.op(eng, lambda e: e.tensor_scalar(out, a, s1, s2, op0, op1), reads, writes)

    def stt(self, eng, out, a, s, b, op0, op1, reads, writes):
        self.P.op(eng, lambda e: e.scalar_tensor_tensor(out, a, s, b, op0, op1), reads, writes)

    def memset(self, eng, ap, v, writes):
        self.P.op(eng, lambda e: e.memset(ap, v), (), writes)

    def ld(self, out, in_, reads, writes, eng="sp", **kw):
        self.P.dma(eng, out, in_, reads=reads, writes=writes, **kw)

    def st(self, out, in_, reads, writes, eng="sp", final=False, semkey=None):
        self.P.dma(eng, out, in_, reads=reads, writes=writes, final=final, semkey=semkey)

    def rstd(self, x, width, sq, ss, rs, xkey, tag):
        n = x.shape[0]
        self.memset("pool", ss, 0.0, [tag + "ss"])
        self.act(sq, x, AF.Square, [xkey, tag + "ss"], [tag + "sq", tag + "ss"], accum_out=ss)
        self.act(rs, ss, AF.Ln, [tag + "ss"], [tag + "rs"], scale=1.0 / width, bias=EPS)
        self.act(rs, rs, AF.Exp, [tag + "rs"], [tag + "rs"], scale=-0.5)

    def build(self):
        nc, P = self.nc, self.P
        NB, NP, NBLKP, NBLK, NTOK, PB, NKB, PAST = self.NB, self.NP, self.NBLKP, self.NBLK, self.NTOK, self.PB, self.NKB, self.PAST
        I = {}
        I["xin"] = self.din("xin", [NTOK, D])
        for n, s in [("g_mix0", [128, D]), ("g_mix1", [128, D]), ("g_ffn0", [128, D]), ("g_ffn1", [128, D]),
                     ("g_fin", [128, D]), ("lam", [128, 256]), ("gsub_col", [128, 1]), ("gsub_row", [128, 128]),
                     ("g_q", [128, 768]), ("g_kv", [128, 256]),
                     ("w_qkv", [D, 3072]), ("w_o0", [D, D]), ("w_gu0", [D, 2 * FD]), ("w_dn0", [FD, D]),
                     ("w_a", [D, 1056]), ("w_uq", [768, 1536]), ("w_uqs", [768, 1536]), ("w_ukv", [256, 2048]),
                     ("w_o1", [D, D]), ("w_r", [D, 8]), ("w_gu1", [NE, D, 2 * FE]), ("w_dn1", [NE, FE, D]),
                     ("cdk", [2, PAST, D]), ("cdv", [2, PAST, D]), ("cckv", [2, PAST, 256]), ("ckpe", [2, PAST, 32]),
                     ("ident", [128, 128]), ("T0", [128, 512]), ("Dneg", [128, 4, 512]), ("Dmask", [128, 4, 512]),
                     ("ctab0", [128, 8 * NB * NKB]), ("mtab", [128, NB * NKB]),
                     ("cs_tm", [NTOK, 32]), ("cs_fm", [2, 32, NTOK]),
                     ("Dnegs", [128, PB * 16]), ("Dnegn", [16, 16])]:
            I[n] = self.din(n, s)
        O = {}
        O["y"] = self.dout("y", [NTOK, D])
        O["dk"] = self.dout("dk", [NTOK, D])
        O["dv"] = self.dout("dv", [NTOK, D])
        O["ckv"] = self.dout("ckv", [NTOK, 256])
        O["kpe"] = self.dout("kpe", [NTOK, 32])
        S = {}
        S["qT"] = self.dscr("qT", [8, 128, NTOK])
        CW = min(NP, 1024); NCW = NP // CW
        self.CW, self.NCW = CW, NCW
        S["kTl"] = [[self.dscr("kTl%d_%d" % (h, c), [128, CW]) for c in range(NCW)] for h in range(8)]
        S["vl"] = [self.dscr("vl%d" % j, [128, D]) for j in range(NBLKP)]
        S["kTg"] = [[self.dscr("kTg%d_%d" % (h, c), [4 * 128, CW]) for c in range(NCW)] for h in range(8)]
        S["vg"] = [self.dscr("vg%d" % j, [4 * 128, D]) for j in range(NBLKP)]
        S["ksT"] = self.dscr("ksT", [8, 128, 128])
        S["vs"] = self.dscr("vs", [128, D])
        S["oT"] = self.dscr("oT", [8, 128, NTOK])
        S["x1"] = self.dscr("x1", [NTOK, D], F32)
        S["actT"] = self.dscr("actT", [22, 128, NTOK])
        S["x2"] = self.dscr("x2", [NTOK, D], F32)
        S["qaT"] = self.dscr("qaT", [16, 96, NTOK])
        S["cpl"] = [[self.dscr("cpl%d_%d" % (j, c), [128, CW]) for c in range(NCW)] for j in range(3)]
        S["cpg"] = [[self.dscr("cpg%d_%d" % (j, c), [4 * 128, CW]) for c in range(NCW)] for j in range(3)]
        S["cps"] = self.dscr("cps", [3, 128, 128])
        S["kaT"] = self.dscr("kaT", [16, 96, 4 * NP])
        S["v1"] = self.dscr("v1", [4 * NP, D])
        S["kaTl"] = self.dscr("kaTl", [16, 96, NP])
        S["v1l"] = self.dscr("v1l", [NP, D])
        S["kaTs"] = self.dscr("kaTs", [16, 96, 128])
        S["v1s"] = self.dscr("v1s", [128, D])
        S["oT1"] = self.dscr("oT1", [16, 64, NTOK])
        S["x3"] = self.dscr("x3", [NTOK, D], F32)
        S["hT"] = self.dscr("hT", [8, 128, NTOK])
        S["comb"] = self.dscr("comb", [NTOK, 8], F32)
        self.I, self.O, self.S = I, O, S

        with contextlib.ExitStack() as g:
            self.g = g
            sb = lambda n, s, d=F32: g.enter_context(nc.sbuf_tensor(n, list(s), d))
            self.ident_f = sb("ident_f", [128, 128])
            self.ident = sb("ident_b", [128, 128], BF16)
            self.ones_f = sb("ones_f", [128, 128])
            self.ones_b = sb("ones_b", [128, 128], BF16)
            self.nlam = sb("nlam", [128, 1])
            self.ld(self.ident_f[:], I["ident"], [], ["ident_f"])
            self.copy("dve", self.ident[:], self.ident_f[:], ["ident_f"], ["ident"])
            self.memset("pool", self.ones_f[:], 1.0, ["ones_f"])
            self.memset("pool", self.ones_b[:], 1.0, ["ones_b"])
            import os
            upto = int(os.environ.get("K_UPTO", "99"))
            steps = [self.phase_lam, self.phase_A,
                     lambda: ([self.gather(S["kTl"][h][c], S["kTg"][h][c], "kT") for h in range(8) for c in range(NCW)], [self.gather(S["vl"][j], S["vg"][j], "v") for j in range(NBLKP)]),
                     lambda: self.phase_attn(0), lambda: self.phase_sample_attn(0), self.phase_C1, self.phase_C2,
                     lambda: [self.gather(S["cpl"][j][c], S["cpg"][j][c], "cp") for j in range(3) for c in range(NCW)], self.phase_D0,
                     lambda: self.phase_attn(1), lambda: self.phase_sample_attn(1), self.phase_E1, self.phase_E2]
            for i, stp in enumerate(steps):
                if i >= upto:
                    break
                stp()
                P.barrier()
            P.emit()
        return nc

    def gather(self, src, dst, key):
        import os
        if key in os.environ.get("K_FAKEG", "").split(","):
            n = src.shape[0]
            for r in range(4):
                self.P.dma("sp", dst[r * n:(r + 1) * n, :], src, reads=["d_" + key + "l"], writes=["d_" + key + "g"], semkey="fakeg")
            return
        self.P.op("pool", lambda e: e.collective_compute(
            "AllGather", ALU.bypass, replica_groups=[[0, 1, 2, 3], [4, 5, 6, 7]],
            ins=[src.opt()], outs=[dst.opt()]), reads=["d_" + key + "l", "cc_chain"], writes=["d_" + key + "g", "cc_chain"])

    def phase_lam(self):
        nc, I = self.nc, self.I
        with contextlib.ExitStack() as st:
            sb = lambda n, s, d=F32: st.enter_context(nc.sbuf_tensor(n, list(s), d))
            lt = sb("lam_t", [128, 256]); pr = sb("lam_p", [128, 128]); s2 = sb("lam_s", [128, 2])
            self.ld(lt[:], I["lam"], [], ["lam_t"])
            self.tt("dve", pr[:, 0:64], lt[:, 0:64], lt[:, 64:128], ALU.mult, ["lam_t"], ["lam_p"])
            self.tt("dve", pr[:, 64:128], lt[:, 128:192], lt[:, 192:256], ALU.mult, ["lam_p", "lam_t"], ["lam_p"])
            self.P.op("dve", lambda e: e.reduce_sum(s2[:, 0:1], pr[:, 0:64], AX.X), ["lam_p"], ["lam_s"])
            self.P.op("dve", lambda e: e.reduce_sum(s2[:, 1:2], pr[:, 64:128], AX.X), ["lam_s", "lam_p"], ["lam_s"])
            self.act(s2[:], s2[:], AF.Exp, ["lam_s"], ["lam_s"])
            self.tt("dve", self.nlam[:], s2[:, 1:2], s2[:, 0:1], ALU.subtract, ["lam_s"], ["nlam"])
            self.ts("dve", self.nlam[:], self.nlam[:], -LAM_INIT0, None, ALU.add, None, ["nlam"], ["nlam"])

    def norm_T(self, st_tiles, x, xkey, gb, width, hT_dst, dstkey, tag, ev="act"):
        t = st_tiles
        self.rstd(x, width, t["sq"][:, 0:width], t["ss"][:, 0:1], t["rs"][:, 0:1], xkey, tag)
        self.stt("dve", t["hb"][:, 0:width], x, t["rs"][:, 0:1], gb, ALU.mult, ALU.mult,
                 [xkey, tag + "rs", "gains"], [tag + "hb"])
        nch = (width + 127) // 128
        for kc in range(nch):
            w = min(128, width - kc * 128)
            self.tr(t["pst"][0:w, kc * 128:(kc + 1) * 128], t["hb"][:, kc * 128:kc * 128 + w], self.ident[:],
                    [tag + "hb", "ident"], [tag + "pst"])
        return nch

    def phase_A(self):
        nc, P, I, O, S = self.nc, self.P, self.I, self.O, self.S
        NP = self.NP
        with contextlib.ExitStack() as st:
            sb = lambda n, s, d=F32: st.enter_context(nc.sbuf_tensor(n, list(s), d))
            psf = lambda n: st.enter_context(nc.psum_tensor(n, [128, 512], F32))
            w = sb("A_w", [128, 8, 3072], BF16)
            gb = sb("A_gb", [128, D])
            xt = [sb("A_xt%d" % i, [128, D]) for i in range(2)]
            T = {"sq": sb("A_sq", [128, D], BF16), "ss": sb("A_ss", [128, 1]), "rs": sb("A_rs", [128, 1]),
                 "hb": sb("A_hb", [128, D], BF16),
                 "pst": st.enter_context(nc.psum_tensor("A_pst", [128, 1024], BF16))}
            hT = [sb("A_hT%d" % i, [128, 8, 512], BF16) for i in range(2)]
            qTt = [sb("A_qT%d" % i, [128, 8, 512], BF16) for i in range(2)]
            kTt = [sb("A_kT%d" % i, [128, 8, 512], BF16) for i in range(2)]
            kvf = [sb("A_kvf%d" % i, [128, 2048]) for i in range(2)]
            vb = [sb("A_vb%d" % i, [128, D], BF16) for i in range(2)]
            ps = [psf("A_ps%d" % i) for i in range(6)]
            pi = 0
            self.ld(w[:], I["w_qkv"].rearrange("(kc p) n -> p kc n", p=128), [], ["A_w"], eng="pool")
            self.ld(gb[:], I["g_mix0"], [], ["gains"])
            bi = 0
            for ti, (t0, nt) in enumerate(self.tiles):
                hs = ti % 2
                nblk = nt // 128
                for b in range(nblk):
                    xs = bi % 2
                    r0 = t0 + b * 128
                    self.ld(xt[xs][:], I["xin"][r0:r0 + 128, :], [], ["A_xt%d" % xs])
                    self.norm_T(T, xt[xs][:], "A_xt%d" % xs, gb[:], D, None, None, "A_")
                    self.copy("act", hT[hs][:, :, b * 128:(b + 1) * 128],
                              T["pst"][:].rearrange("p (k c) -> p k c", k=8), ["A_pst"], ["A_hT%d" % hs])
                    bi += 1
                for (dst, dkey, coff, scl) in ((qTt[hs], "A_qT%d" % hs, 0, 0.125), (kTt[hs], "A_kT%d" % hs, 1024, 1.0)):
                    for h in range(8):
                        p_ = ps[pi % 6]; pk = "A_ps%d" % (pi % 6); pi += 1
                        for kc in range(8):
                            self.mm(p_[:, 0:nt], w[:, kc, coff + h * 128:coff + (h + 1) * 128], hT[hs][:, kc, 0:nt],
                                    kc == 0, kc == 7, ["A_w", "A_hT%d" % hs], [pk])
                        self.act(dst[:, h, 0:nt], p_[:, 0:nt], AF.Copy, [pk], [dkey], scale=scl)
                self.st(S["qT"][:, :, t0:t0 + nt].rearrange("h p t -> p h t"), qTt[hs][:, :, 0:nt],
                        ["A_qT%d" % hs], ["d_qT"], semkey="A_stq%d" % hs)
                if t0 < NP:
                    for h in range(8):
                        self.st(S["kTl"][h][t0 // self.CW][:, t0 % self.CW:t0 % self.CW + nt], kTt[hs][:, h, 0:nt],
                                ["A_kT%d" % hs], ["d_kTl"], semkey="A_stk%d" % hs)
                else:
                    self.st(S["ksT"].rearrange("h p t -> p h t"), kTt[hs][:, :, 0:nt],
                            ["A_kT%d" % hs], ["d_ksT"], semkey="A_stk%d" % hs)
                for b in range(nblk):
                    r0 = t0 + b * 128
                    ks = (bi + b) % 2
                    for n in range(4):
                        p_ = ps[pi % 6]; pk = "A_ps%d" % (pi % 6); pi += 1
                        for kc in range(8):
                            self.mm(p_[:], hT[hs][:, kc, b * 128:(b + 1) * 128], w[:, kc, 1024 + n * 512:1024 + (n + 1) * 512],
                                    kc == 0, kc == 7, ["A_w", "A_hT%d" % hs], [pk])
                        self.copy("dve" if n % 2 else "act", kvf[ks][:, n * 512:(n + 1) * 512], p_[:], [pk], ["A_kvf%d" % ks])
                    self.copy("pool", vb[ks][:], kvf[ks][:, 1024:2048], ["A_kvf%d" % ks], ["A_vb%d" % ks])
                    self.st(O["dk"][r0:r0 + 128, :], kvf[ks][:, 0:1024], ["A_kvf%d" % ks], [], final=True, semkey="A_sto%d" % ks)
                    self.st(O["dv"][r0:r0 + 128, :], kvf[ks][:, 1024:2048], ["A_kvf%d" % ks], [], final=True, semkey="A_sto%d" % ks)
                    if t0 < NP:
                        self.st(S["vl"][r0 // 128], vb[ks][:], ["A_vb%d" % ks], ["d_vl"], semkey="A_stv%d" % ks)
                    else:
                        self.st(S["vs"], vb[ks][:], ["A_vb%d" % ks], ["d_vs"], semkey="A_stv%d" % ks)

    def phase_attn(self, layer):
        nc, P, I, O, S = self.nc, self.P, self.I, self.O, self.S
        NB, NP, NBLKP, NKB = self.NB, self.NP, self.NBLKP, self.NKB
        diff = layer == 0
        NH = 8 if diff else 16
        KR = 128 if diff else 96
        VW = 128 if diff else 65
        nS = 2 if diff else 1
        scale1 = (64 + 32) ** -0.5
        kT_g, v_g = (S["kTg"], S["vg"]) if diff else (S["kaT"], S["v1"])
        kT_l, v_l = (S["kTl"], S["vl"]) if diff else (S["kaTl"], S["v1l"])
        qT_d = S["qT"] if diff else S["qaT"]
        oT_d = S["oT"] if diff else S["oT1"]
        pre = "B%d_" % layer
        with contextlib.ExitStack() as st:
            sb = lambda n, s, d=F32: st.enter_context(nc.sbuf_tensor(pre + n, list(s), d))
            psf = lambda n: st.enter_context(nc.psum_tensor(pre + n, [128, 512], F32))
            KT = sb("KT", [KR, 4, NP], BF16)
            V = sb("V", [128, 4 * NBLKP, VW], BF16)
            QT = [sb("QT%d" % i, [KR, 512], BF16) for i in range(2)]
            KTd = [sb("KTd%d" % i, [KR, 512], BF16) for i in range(2)]
            Vd = [sb("Vd%d" % i, [128, 4, VW], BF16) for i in range(2)]
            ctj = [sb("ct%d" % i, [128, NKB]) for i in range(2)]
            Dn = sb("Dn", [128, 4, 512])
            tmp = [sb("tmp%d" % i, [128, 512]) for i in range(4)]
            Pt = [sb("P%d" % i, [128, 512], BF16) for i in range(4)]
            Lsb = sb("Lsb", [128, 512]); Bs = [sb("Bs%d" % i, [128, 512]) for i in range(2)]
            Pacc = [sb("Pacc%d" % i, [128, 512]) for i in range(2)] if diff else None
            oa = sb("oa", [128, 512]); ob = sb("ob", [128, 512]); osq = sb("osq", [128, 512]); rsd = sb("rsd", [128, 512])
            oTt = [sb("oTt%d" % i, [128, 512], BF16) for i in range(2)]
            Sps = [psf("S%d" % i) for i in range(4)]
            Ops = [psf("O%d" % i) for i in range(2)]
            Lps = psf("L")
            if diff:
                T0 = sb("T0", [128, 512]); Th = sb("Th", [128, 512]); gs = sb("gs", [128, 1])
                self.ld(T0[:], I["T0"], [], [pre + "T0"])
                self.ld(Dn[:], I["Dneg"], [], [pre + "Dn"])
                self.ld(gs[:], I["gsub_col"], [], [pre + "gs"])
                self.ts("dve", gs[:], gs[:], 1.0 - LAM_INIT0, None, ALU.mult, None, [pre + "gs"], [pre + "gs"])
            else:
                self.ld(Dn[:], I["Dmask"], [], [pre + "Dn"])
                self.memset("pool", V[:, :, 64:65], 1.0, [pre + "V"])
                for i in range(2):
                    self.memset("pool", Vd[i][:, :, 64:65], 1.0, [pre + "Vd%d" % i])
            si = 0
            it = 0
            for h in range(NH):
                for rk in range(4):
                    if diff:
                        for c in range(self.NCW):
                            self.ld(KT[:, rk, c * self.CW:(c + 1) * self.CW], kT_g[h][c][rk * 128:(rk + 1) * 128, :], ["d_kTg"], [pre + "KT"], semkey=pre + "ldK")
                        for blk in range(NBLKP):
                            self.ld(V[:, rk * NBLKP + blk, :], v_g[blk][rk * 128:(rk + 1) * 128, h * 128:(h + 1) * 128],
                                    ["d_vg"], [pre + "V"], semkey=pre + "ldV")
                    else:
                        self.ld(KT[:, rk, :], kT_g[h, :, rk * NP:(rk + 1) * NP], ["d_kaT"], [pre + "KT"], semkey=pre + "ldK")
                        self.ld(V[:, rk * NBLKP:(rk + 1) * NBLKP, 0:64],
                                v_g[rk * NP:(rk + 1) * NP, h * 64:(h + 1) * 64].rearrange("(b p) e -> p b e", p=128),
                                ["d_v1"], [pre + "V"], semkey=pre + "ldV")
                if diff:
                    self.ts("pool", Th[:], T0[:], SLOPES[h], None, ALU.mult, None, [pre + "T0"], [pre + "Th"])
                for J in range(NB):
                    js = it % 2; it += 1
                    q0 = J * 512
                    qk, kdk, vdk, ck = pre + "QT%d" % js, pre + "KTd%d" % js, pre + "Vd%d" % js, pre + "ct%d" % js
                    self.ld(QT[js][:], qT_d[h, 0:KR, q0:q0 + 512], ["d_qT"], [qk])
                    if diff:
                        self.ld(KTd[js][:], kT_l[h][q0 // self.CW][:, q0 % self.CW:q0 % self.CW + 512], ["d_kTl"], [kdk])
                        for i_ in range(4):
                            self.ld(Vd[js][:, i_, :], v_l[J * 4 + i_][:, h * 128:(h + 1) * 128], ["d_vl"], [vdk])
                        self.ld(ctj[js][:], I["ctab0"][:, (h * NB + J) * NKB:(h * NB + J + 1) * NKB], [], [ck])
                    else:
                        self.ld(KTd[js][:], kT_l[h, 0:KR, q0:q0 + 512], ["d_kTl"], [kdk])
                        self.ld(Vd[js][:, :, 0:64], v_l[q0:q0 + 512, h * 64:(h + 1) * 64].rearrange("(b p) e -> p b e", p=128),
                                ["d_vl"], [vdk])
                        self.ld(ctj[js][:], I["mtab"][:, J * NKB:(J + 1) * NKB], [], [ck])
                    blocks = []
                    for rk in range(4):
                        for Jk in range(J + 1):
                            for i in range(4):
                                blocks.append(("far", rk, Jk, i))
                    for i in range(4):
                        blocks.append(("diag", 0, 0, i))
                    nblocks = len(blocks)
                    units = [(bi_, blk_, s_) for bi_, blk_ in enumerate(blocks) for s_ in range(nS)]
                    si_base = si
                    si += len(units)

                    def unit_info(u):
                        bi_, (kind, rk, Jk, i), s = units[u]
                        sl = (si_base + u) % 4
                        if kind == "far":
                            kbi = rk * NBLKP + Jk * 4 + i
                            kcol = Jk * 512 + i * 128
                            kfn = lambda lo, hi: KT[lo:hi, rk, kcol:kcol + 128]
                            vap = V[:, kbi, :]
                            krd, vrd = pre + "KT", pre + "V"
                        else:
                            kbi = 0
                            kfn = lambda lo, hi: KTd[js][lo:hi, i * 128:(i + 1) * 128]
                            vap = Vd[js][:, i, :]
                            krd, vrd = kdk, vdk
                        if diff:
                            lo, hi = s * 64, (s + 1) * 64
                        else:
                            lo, hi = 0, 96
                        return bi_, kind, i, s, sl, kbi, kfn, vap, krd, vrd, lo, hi

                    def stage1(u):
                        bi_, kind, i, s, sl, kbi, kfn, vap, krd, vrd, lo, hi = unit_info(u)
                        self.mm(Sps[sl][:], kfn(lo, hi), QT[js][lo:hi, :], True, True, [krd, qk], [pre + "S%d" % sl])

                    def stage2(u):
                        bi_, kind, i, s, sl, kbi, kfn, vap, krd, vrd, lo, hi = unit_info(u)
                        first, last = bi_ == 0, bi_ == nblocks - 1
                        sp_, sk = Sps[sl], pre + "S%d" % sl
                        tp_, tk = tmp[sl], pre + "tmp%d" % sl
                        pp_, pk = Pt[sl], pre + "P%d" % sl
                        if diff:
                            if kind == "far":
                                self.stt("dve", tp_[:], sp_[:], ctj[js][:, kbi:kbi + 1], Th[:], ALU.add, ALU.add,
                                         [sk, ck, pre + "Th"], [tk])
                            else:
                                self.stt("dve", tp_[:], Dn[:, i, :], SLOPES[h], sp_[:], ALU.mult, ALU.add,
                                         [sk, pre + "Dn"], [tk])
                            self.act(pp_[:], tp_[:], AF.Exp, [tk], [pk])
                        else:
                            if kind == "far":
                                self.act(pp_[:], sp_[:], AF.Exp, [sk, ck], [pk], bias=ctj[js][:, kbi:kbi + 1], scale=scale1)
                            else:
                                self.stt("dve", tp_[:], sp_[:], scale1, Dn[:, i, :], ALU.mult, ALU.add,
                                         [sk, pre + "Dn"], [tk])
                                self.act(pp_[:], tp_[:], AF.Exp, [tk], [pk])
                        self.mm(Ops[s][0:VW, :], vap, pp_[:], first, last, [vrd, pk], [pre + "O%d" % s])
                        if diff and s == 0:
                            if first:
                                self.copy("pool", Pacc[s][:], pp_[:], [pk], [pre + "Pacc%d" % s])
                            else:
                                self.tt("pool", Pacc[s][:], Pacc[s][:], pp_[:], ALU.add, [pk, pre + "Pacc%d" % s], [pre + "Pacc%d" % s])
                        if diff and s == 1:
                            self.mm(Lps[32:33, :], self.ones_b[:, 0:1], pp_[:], first, last, ["ones_b", pk], [pre + "L"])

                    LA = 2
                    for u in range(len(units) + LA):
                        if u < len(units):
                            stage1(u)
                        if u - LA >= 0:
                            stage2(u - LA)
                    ot = oTt[js]; otk = pre + "oTt%d" % js
                    if diff:
                        for s_ in range(1):
                            self.mm(Lps[32 * s_:32 * s_ + 1, :], self.ones_f[:, 0:1], Pacc[s_][:], True, True,
                                    ["ones_f", pre + "Pacc%d" % s_], [pre + "L"])
                        self.copy("dve", Lsb[0:64, :], Lps[0:64, :], [pre + "L"], [pre + "Lsb"])
                        self.P.op("dve", lambda e: e.reciprocal(Lsb[0:1, :], Lsb[0:1, :]), [pre + "Lsb"], [pre + "Lsb"])
                        self.P.op("dve", lambda e: e.reciprocal(Lsb[32:33, :], Lsb[32:33, :]), [pre + "Lsb"], [pre + "Lsb"])
                        self.ts("dve", Lsb[32:33, :], Lsb[32:33, :], self.nlam[32:33, 0:1], None, ALU.mult, None,
                                [pre + "Lsb", "nlam"], [pre + "Lsb"])
                        for s in range(2):
                            self.mm(Sps[s][:], self.ones_f[32 * s:32 * s + 1, :], Lsb[32 * s:32 * s + 1, :], True, True,
                                    ["ones_f", pre + "Lsb"], [pre + "S%d" % s])
                            self.copy("act", Bs[s][:], Sps[s][:], [pre + "S%d" % s], [pre + "Bs%d" % s])
                        self.tt("dve", oa[:], Ops[0][:], Bs[0][:], ALU.mult, [pre + "O0", pre + "Bs0"], [pre + "oa"])
                        self.tt("dve", ob[:], Ops[1][:], Bs[1][:], ALU.mult, [pre + "O1", pre + "Bs1"], [pre + "ob"])
                        self.tt("pool", oa[:], oa[:], ob[:], ALU.add, [pre + "oa", pre + "ob"], [pre + "oa"])
                        self.act(osq[:], oa[:], AF.Square, [pre + "oa"], [pre + "osq"])
                        self.mm(Sps[2][:], self.ones_f[:], osq[:], True, True, ["ones_f", pre + "osq"], [pre + "S2"])
                        self.act(rsd[:], Sps[2][:], AF.Ln, [pre + "S2"], [pre + "rsd"], scale=1.0 / 128, bias=EPS)
                        self.act(rsd[:], rsd[:], AF.Exp, [pre + "rsd"], [pre + "rsd"], scale=-0.5)
                        self.stt("dve", ot[:], oa[:], gs[:, 0:1], rsd[:], ALU.mult, ALU.mult,
                                 [pre + "oa", pre + "gs", pre + "rsd"], [otk])
                        self.st(oT_d[h, :, q0:q0 + 512], ot[:], [otk], ["d_oT"], semkey=pre + "sto%d" % js)
                    else:
                        self.copy("dve", Lsb[64:65, :], Ops[0][64:65, :], [pre + "O0"], [pre + "Lsb"])
                        self.P.op("dve", lambda e: e.reciprocal(Lsb[64:65, :], Lsb[64:65, :]), [pre + "Lsb"], [pre + "Lsb"])
                        self.mm(Sps[0][0:64, :], self.ones_f[64:65, 0:64], Lsb[64:65, :], True, True,
                                ["ones_f", pre + "Lsb"], [pre + "S0"])
                        self.copy("act", Bs[0][0:64, :], Sps[0][0:64, :], [pre + "S0"], [pre + "Bs0"])
                        self.tt("dve", ot[0:64, :], Ops[0][0:64, :], Bs[0][0:64, :], ALU.mult, [pre + "O0", pre + "Bs0"], [otk])
                        self.st(oT_d[h, :, q0:q0 + 512], ot[0:64, :], [otk], ["d_oT1"], semkey=pre + "sto%d" % js)

    def phase_sample_attn(self, layer):
        nc, P, I, O, S = self.nc, self.P, self.I, self.O, self.S
        NP, PB, PAST = self.NP, self.PB, self.PAST
        diff = layer == 0
        pre = "Bs%d_" % layer
        NH = 8 if diff else 16
        scale1 = (64 + 32) ** -0.5
        with contextlib.ExitStack() as st:
            sb = lambda n, s, d=F32: st.enter_context(nc.sbuf_tensor(pre + n, list(s), d))
            psf = lambda n: st.enter_context(nc.psum_tensor(pre + n, [128, 512], F32))
            pst = st.enter_context(nc.psum_tensor(pre + "pst", [128, 1024], BF16))
            Sp = [psf("S%d" % i) for i in range(2)]
            Sn = psf("Sn"); Op_ = [psf("O%d" % i) for i in range(2)]
            zt = sb("zt", [128, 128], BF16)
            self.memset("pool", zt[:], 0.0, [pre + "zt"])
            if diff:
                for h in range(8):
                    self.st(S["oT"][h, :, NP:NP + 128], zt[:], [pre + "zt"], ["d_oT"], semkey=pre + "z")
                Kc = sb("Kc", [128, PB, 128]); Kcb = sb("Kcb", [128, PB, 128], BF16); KcT = sb("KcT", [128, PAST], BF16)
                Vc = sb("Vc", [128, PB, 128]); Vcb = sb("Vcb", [128, PB, 128], BF16)
                qsT = sb("qsT", [128, 8, 128], BF16); ksT = sb("ksT", [128, 8, 128], BF16)
                vsl = [sb("vs%d" % i, [16, D], BF16) for i in range(2)]
                Dns = sb("Dns", [128, PB * 16]); Dnn = sb("Dnn", [16, 16])
                tm = [sb("tm%d" % i, [128, 512]) for i in range(2)]; Ps = [sb("Ps%d" % i, [128, 512], BF16) for i in range(2)]
                tn = [sb("tn%d" % i, [16, 16]) for i in range(2)]; Pn = [sb("Pn%d" % i, [16, 16], BF16) for i in range(2)]
                r = sb("r", [16, 2]); o = sb("o", [16, 128]); sq = sb("sq", [16, 128]); ss = sb("ss", [16, 1]); rs = sb("rs", [16, 1])
                gsr = sb("gsr", [128, 128]); ob = sb("ob", [16, 128], BF16); oTs = sb("oTs", [128, 16], BF16)
                self.ld(qsT[:], S["qT"][:, :, NP:NP + 128].rearrange("h p t -> p h t"), ["d_qT"], [pre + "qsT"])
                self.ld(ksT[:], S["ksT"].rearrange("h p t -> p h t"), ["d_ksT"], [pre + "ksT"])
                for i in range(2):
                    self.ld(vsl[i][:], S["vs"][i * 32:i * 32 + 16, :], ["d_vs"], [pre + "vs"])
                self.ld(Dns[:], I["Dnegs"], [], [pre + "Dns"])
                self.ld(Dnn[:], I["Dnegn"], [], [pre + "Dnn"])
                self.ld(gsr[:], I["gsub_row"], [], [pre + "gsr"])
                for s in range(2):
                    c0 = s * 32
                    for h in range(8):
                        self.ld(Kc[:], I["cdk"][s, :, h * 128:(h + 1) * 128].rearrange("(b p) e -> p b e", p=128), [], [pre + "Kc"])
                        self.ld(Vc[:], I["cdv"][s, :, h * 128:(h + 1) * 128].rearrange("(b p) e -> p b e", p=128), [], [pre + "Vc"])
                        self.copy("pool", Kcb[:], Kc[:], [pre + "Kc"], [pre + "Kcb"])
                        self.copy("pool", Vcb[:], Vc[:], [pre + "Vc"], [pre + "Vcb"])
                        for g8 in range(PB // 8):
                            for j in range(8):
                                kb = g8 * 8 + j
                                self.tr(pst[:, j * 128:(j + 1) * 128], Kcb[:, kb, :], self.ident[:], [pre + "Kcb", "ident"], [pre + "pst"])
                            self.copy("act", KcT[:, g8 * 1024:(g8 + 1) * 1024], pst[:], [pre + "pst"], [pre + "KcT"])
                        for sidx in range(2):
                            lo, hi = sidx * 64, (sidx + 1) * 64
                            for kb in range(PB):
                                self.mm(Sp[sidx][:, kb * 16:(kb + 1) * 16], KcT[lo:hi, kb * 128:(kb + 1) * 128],
                                        qsT[lo:hi, h, c0:c0 + 16], True, True, [pre + "KcT", pre + "qsT"], [pre + "S%d" % sidx])
                            self.stt("dve", tm[sidx][:, 0:PB * 16], Dns[:], SLOPES[h], Sp[sidx][:, 0:PB * 16], ALU.mult, ALU.add,
                                     [pre + "Dns", pre + "S%d" % sidx], [pre + "tm%d" % sidx])
                            self.act(Ps[sidx][:, 0:PB * 16], tm[sidx][:, 0:PB * 16], AF.Exp, [pre + "tm%d" % sidx], [pre + "Ps%d" % sidx])
                            self.mm(Sn[0:16, sidx * 16:(sidx + 1) * 16], ksT[lo:hi, h, c0:c0 + 16], qsT[lo:hi, h, c0:c0 + 16],
                                    True, True, [pre + "ksT", pre + "qsT"], [pre + "Sn"])
                            self.stt("dve", tn[sidx][:], Dnn[:], SLOPES[h], Sn[0:16, sidx * 16:(sidx + 1) * 16], ALU.mult, ALU.add,
                                     [pre + "Dnn", pre + "Sn"], [pre + "tn%d" % sidx])
                            self.act(Pn[sidx][:], tn[sidx][:], AF.Exp, [pre + "tn%d" % sidx], [pre + "Pn%d" % sidx])
                            ok = pre + "O%d" % sidx
                            for kb in range(PB):
                                self.mm(Op_[sidx][0:16, 0:128], Ps[sidx][:, kb * 16:(kb + 1) * 16], Vcb[:, kb, :], kb == 0, False,
                                        [pre + "Ps%d" % sidx, pre + "Vcb"], [ok])
                            self.mm(Op_[sidx][0:16, 0:128], Pn[sidx][:], vsl[s][:, h * 128:(h + 1) * 128], False, True,
                                    [pre + "Pn%d" % sidx, pre + "vs"], [ok])
                            for kb in range(PB):
                                self.mm(Op_[sidx][0:16, 128:129], Ps[sidx][:, kb * 16:(kb + 1) * 16], self.ones_b[:, 0:1], kb == 0, False,
                                        [pre + "Ps%d" % sidx, "ones_b"], [ok])
                            self.mm(Op_[sidx][0:16, 128:129], Pn[sidx][:], self.ones_b[0:16, 0:1], False, True,
                                    [pre + "Pn%d" % sidx, "ones_b"], [ok])
                        self.P.op("dve", lambda e: e.reciprocal(r[:, 0:1], Op_[0][0:16, 128:129]), [pre + "O0"], [pre + "r"])
                        self.P.op("dve", lambda e: e.reciprocal(r[:, 1:2], Op_[1][0:16, 128:129]), [pre + "O1", pre + "r"], [pre + "r"])
                        self.ts("dve", r[:, 1:2], r[:, 1:2], self.nlam[0:16, 0:1], None, ALU.mult, None, [pre + "r", "nlam"], [pre + "r"])
                        self.ts("dve", o[:], Op_[0][0:16, 0:128], r[:, 0:1], None, ALU.mult, None, [pre + "O0", pre + "r"], [pre + "o"])
                        self.stt("dve", o[:], Op_[1][0:16, 0:128], r[:, 1:2], o[:], ALU.mult, ALU.add, [pre + "O1", pre + "r", pre + "o"], [pre + "o"])
                        self.rstd(o[:], 128, sq[:], ss[:], rs[:], pre + "o", pre)
                        self.ts("dve", rs[:], rs[:], 1.0 - LAM_INIT0, None, ALU.mult, None, [pre + "rs"], [pre + "rs"])
                        self.stt("dve", ob[:], o[:], rs[:, 0:1], gsr[0:16, :], ALU.mult, ALU.mult, [pre + "o", pre + "rs", pre + "gsr"], [pre + "ob"])
                        self.tr(pst[:, 0:16], ob[:], self.ident[0:16, 0:16], [pre + "ob", "ident"], [pre + "pst"])
                        self.copy("act", oTs[:], pst[:, 0:16], [pre + "pst"], [pre + "oTs"])
                        self.st(S["oT"][h, :, NP + c0:NP + c0 + 16], oTs[:], [pre + "oTs"], ["d_oT"], semkey=pre + "sto")
            else:
                for h in range(16):
                    self.st(S["oT1"][h, :, NP:NP + 128], zt[0:64, :], [pre + "zt"], ["d_oT1"], semkey=pre + "z")
                wk = sb("wk", [128, 3, 16, 96], BF16); wv = sb("wv", [128, 2, 1024], BF16)
                self.build_wk(wk, wv, pre)
                Cc = sb("Cc", [128, PB, 288]); Ccb = sb("Ccb", [128, PB, 288], BF16); CT = sb("CT", [128, 3, PAST], BF16)
                Vcb = sb("Vcb", [128, PB, 16, 65], BF16)
                qsT = sb("qsT", [96, 16, 128], BF16); ksT = sb("ksT", [96, 16, 128], BF16)
                KaTh = sb("KaTh", [96, PAST], BF16)
                Ps = sb("Ps", [128, 512], BF16); Pn = sb("Pn", [16, 16], BF16)
                r = sb("r", [16, 1]); ob = sb("ob", [16, 64], BF16); oTs = sb("oTs", [64, 16], BF16)
                self.ld(qsT[:], S["qaT"][:, :, NP:NP + 128].rearrange("h p t -> p h t"), ["d_qaT"], [pre + "qsT"])
                self.ld(ksT[:], S["kaTs"].rearrange("h p t -> p h t"), ["d_kaTs"], [pre + "ksT"])
                self.memset("pool", Vcb[:, :, :, 64:65], 1.0, [pre + "Vcb"])
                vsl = [sb("vs%d" % i, [16, D], BF16) for i in range(2)]
                vsx = [sb("vsx%d" % i, [16, 16, 65], BF16) for i in range(2)]
                for i in range(2):
                    self.ld(vsl[i][:], S["v1s"][i * 32:i * 32 + 16, :], ["d_v1s"], [pre + "vs"])
                    self.memset("pool", vsx[i][:, :, 64:65], 1.0, [pre + "vsx"])
                    self.copy("pool", vsx[i][:, :, 0:64], vsl[i][:].rearrange("p (h e) -> p h e", h=16), [pre + "vs", pre + "vsx"], [pre + "vsx"])
                for s in range(2):
                    c0 = s * 32
                    self.ld(Cc[:, :, 0:256], I["cckv"][s].rearrange("(b p) e -> p b e", p=128), [], [pre + "Cc"])
                    self.ld(Cc[:, :, 256:288], I["ckpe"][s].rearrange("(b p) e -> p b e", p=128), [], [pre + "Cc"])
                    self.copy("pool", Ccb[:], Cc[:], [pre + "Cc"], [pre + "Ccb"])
                    for ch in range(3):
                        wd = 128 if ch < 2 else 32
                        for g8 in range(PB // 8):
                            for j in range(8):
                                kb = g8 * 8 + j
                                self.tr(pst[0:wd, j * 128:(j + 1) * 128], Ccb[:, kb, ch * 128:ch * 128 + wd], self.ident[:],
                                        [pre + "Ccb", "ident"], [pre + "pst"])
                            self.copy("act", CT[0:wd, ch, g8 * 1024:(g8 + 1) * 1024], pst[0:wd, :], [pre + "pst"], [pre + "CT"])
                    for kb in range(PB):
                        for n in range(2):
                            pp = Sp[n]
                            for kc in range(2):
                                self.mm(pp[:], CT[:, kc, kb * 128:(kb + 1) * 128], wv[:, kc, n * 512:(n + 1) * 512], kc == 0, kc == 1,
                                        [pre + "CT", pre + "wv"], [pre + "S%d" % n])
                            self.copy("dve" if n else "act", Vcb[:, kb, n * 8:(n + 1) * 8, 0:64],
                                      pp[:].rearrange("p (h e) -> p h e", h=8), [pre + "S%d" % n], [pre + "Vcb"])
                    for h in range(16):
                        for kt in range(PAST // 512):
                            for ch in range(3):
                                wd = 128 if ch < 2 else 32
                                self.mm(Sn[0:96, :], wk[0:wd, ch, h, :], CT[0:wd, ch, kt * 512:(kt + 1) * 512], ch == 0, ch == 2,
                                        [pre + "wk", pre + "CT"], [pre + "Sn"])
                            self.copy("act", KaTh[:, kt * 512:(kt + 1) * 512], Sn[0:96, :], [pre + "Sn"], [pre + "KaTh"])
                        for kb in range(PB):
                            self.mm(Sp[0][:, kb * 16:(kb + 1) * 16], KaTh[:, kb * 128:(kb + 1) * 128], qsT[:, h, c0:c0 + 16], True, True,
                                    [pre + "KaTh", pre + "qsT"], [pre + "S0"])
                        self.act(Ps[:, 0:PB * 16], Sp[0][:, 0:PB * 16], AF.Exp, [pre + "S0"], [pre + "Ps"], scale=scale1)
                        self.mm(Sp[1][0:16, 0:16], ksT[:, h, c0:c0 + 16], qsT[:, h, c0:c0 + 16], True, True,
                                [pre + "ksT", pre + "qsT"], [pre + "S1"])
                        self.act(Pn[:], Sp[1][0:16, 0:16], AF.Exp, [pre + "S1"], [pre + "Pn"], scale=scale1)
                        for kb in range(PB):
                            self.mm(Op_[0][0:16, 0:65], Ps[:, kb * 16:(kb + 1) * 16], Vcb[:, kb, h, :], kb == 0, False,
                                    [pre + "Ps", pre + "Vcb"], [pre + "O0"])
                        self.mm(Op_[0][0:16, 0:65], Pn[:], vsx[s][:, h, :], False, True, [pre + "Pn", pre + "vsx"], [pre + "O0"])
                        self.P.op("dve", lambda e: e.reciprocal(r[:], Op_[0][0:16, 64:65]), [pre + "O0"], [pre + "r"])
                        self.ts("dve", ob[:], Op_[0][0:16, 0:64], r[:, 0:1], None, ALU.mult, None, [pre + "O0", pre + "r"], [pre + "ob"])
                        self.tr(pst[0:64, 0:16], ob[:], self.ident[0:16, 0:16], [pre + "ob", "ident"], [pre + "pst"])
                        self.copy("act", oTs[:], pst[0:64, 0:16], [pre + "pst"], [pre + "oTs"])
                        self.st(S["oT1"][h, :, NP + c0:NP + c0 + 16], oTs[:], [pre + "oTs"], ["d_oT1"], semkey=pre + "sto")

    def build_wk(self, wk, wv, pre):
        I = self.I
        self.memset("pool", wk[:], 0.0, [pre + "wk"])
        w = I["w_ukv"].rearrange("(kc p) (h x) -> p kc h x", p=128, h=16)
        for kc in range(2):
            self.ld(wk[:, kc, :, 0:64], w[:, kc, :, 0:64], [], [pre + "wk"], eng="pool")
            self.ld(wv[:, kc, :].rearrange("p (h e) -> p h e", h=16), w[:, kc, :, 64:128], [], [pre + "wv"], eng="pool")
        for h in range(16):
            self.copy("dve", wk[0:32, 2, h, 64:96], self.ident[0:32, 0:32], ["ident", pre + "wk"], [pre + "wk"])

    def phase_C1(self):
        nc, P, I, O, S = self.nc, self.P, self.I, self.O, self.S
        pre = "C1_"
        with contextlib.ExitStack() as st:
            sb = lambda n, s, d=F32: st.enter_context(nc.sbuf_tensor(pre + n, list(s), d))
            psf = lambda n: st.enter_context(nc.psum_tensor(pre + n, [128, 512], F32))
            wo = sb("wo", [128, 8, D], BF16); wgu = sb("wgu", [128, 8, 2 * FD], BF16); gb = sb("gb", [128, D])
            xt = [sb("xt%d" % i, [128, D]) for i in range(2)]
            T = {"sq": sb("sq", [128, D], BF16), "ss": sb("ss", [128, 1]), "rs": sb("rs", [128, 1]),
                 "hb": sb("hb", [128, D], BF16), "pst": st.enter_context(nc.psum_tensor(pre + "pst", [128, 1024], BF16))}
            oTt = [sb("oTt%d" % i, [128, 8, 512], BF16) for i in range(2)]
            hT = [sb("hT%d" % i, [128, 8, 512], BF16) for i in range(2)]
            sg = [sb("sg%d" % i, [128, 512]) for i in range(2)]
            aT = [sb("aT%d" % i, [128, 22, 512], BF16) for i in range(1)]
            ps = [psf("ps%d" % i) for i in range(6)]
            pi = 0
            self.ld(wo[:], I["w_o0"].rearrange("(kc p) n -> p kc n", p=128), [], [pre + "wo"], eng="pool")
            self.ld(wgu[:], I["w_gu0"].rearrange("(kc p) n -> p kc n", p=128), [], [pre + "wgu"], eng="pool")
            self.ld(gb[:], I["g_ffn0"], [], ["gains"])
            bi = 0
            for ti, (t0, nt) in enumerate(self.tiles):
                hs = ti % 2
                self.ld(oTt[hs][:, :, 0:nt], S["oT"][:, :, t0:t0 + nt].rearrange("h p t -> p h t"), ["d_oT"], [pre + "oTt%d" % hs])
                for b in range(nt // 128):
                    xs = bi % 2; bi += 1
                    r0 = t0 + b * 128
                    xk = pre + "xt%d" % xs
                    self.ld(xt[xs][:], I["xin"][r0:r0 + 128, :], [], [xk])
                    for n in range(2):
                        p_ = ps[pi % 6]; pk = pre + "ps%d" % (pi % 6); pi += 1
                        for h in range(8):
                            self.mm(p_[:], oTt[hs][:, h, b * 128:(b + 1) * 128], wo[:, h, n * 512:(n + 1) * 512], h == 0, h == 7,
                                    [pre + "oTt%d" % hs, pre + "wo"], [pk])
                        self.tt("dve", xt[xs][:, n * 512:(n + 1) * 512], xt[xs][:, n * 512:(n + 1) * 512], p_[:], ALU.add, [xk, pk], [xk])
                    self.st(S["x1"][r0:r0 + 128, :], xt[xs][:], [xk], ["d_x1"], semkey=pre + "stx%d" % xs)
                    self.norm_T(T, xt[xs][:], xk, gb[:], D, None, None, pre)
                    self.copy("act", hT[hs][:, :, b * 128:(b + 1) * 128], T["pst"][:].rearrange("p (k c) -> p k c", k=8),
                              [pre + "pst"], [pre + "hT%d" % hs])
                for j in range(22):
                    pg = ps[pi % 6]; pgk = pre + "ps%d" % (pi % 6); pi += 1
                    pu = ps[pi % 6]; puk = pre + "ps%d" % (pi % 6); pi += 1
                    for kc in range(8):
                        self.mm(pg[:, 0:nt], wgu[:, kc, j * 128:(j + 1) * 128], hT[hs][:, kc, 0:nt], kc == 0, kc == 7,
                                [pre + "wgu", pre + "hT%d" % hs], [pgk])
                    for kc in range(8):
                        self.mm(pu[:, 0:nt], wgu[:, kc, FD + j * 128:FD + (j + 1) * 128], hT[hs][:, kc, 0:nt], kc == 0, kc == 7,
                                [pre + "wgu", pre + "hT%d" % hs], [puk])
                    sgs = j % 2
                    self.act(sg[sgs][:, 0:nt], pg[:, 0:nt], AF.Silu, [pgk], [pre + "sg%d" % sgs])
                    self.tt("dve", aT[0][:, j, 0:nt], sg[sgs][:, 0:nt], pu[:, 0:nt], ALU.mult, [pre + "sg%d" % sgs, puk], [pre + "aT0"])
                self.st(S["actT"][:, :, t0:t0 + nt].rearrange("j p t -> p j t"), aT[0][:, :, 0:nt], [pre + "aT0"], ["d_actT"], semkey=pre + "sta")

    def phase_C2(self):
        nc, P, I, O, S = self.nc, self.P, self.I, self.O, self.S
        NP = self.NP
        pre = "C2_"
        with contextlib.ExitStack() as st:
            sb = lambda n, s, d=F32: st.enter_context(nc.sbuf_tensor(pre + n, list(s), d))
            psf = lambda n: st.enter_context(nc.psum_tensor(pre + n, [128, 512], F32))
            wdn = sb("wdn", [128, 22, D], BF16); wa = sb("wa", [128, 8, 1056], BF16)
            wuq = sb("wuq", [128, 6, 1536], BF16); wuqs = sb("wuqs", [128, 6, 1536], BF16)
            gb = sb("gb", [128, D]); gq = sb("gq", [128, 768]); gkv = sb("gkv", [128, 256])
            csf = sb("csf", [96, 2, 512]); cst = sb("cst", [128, 32])
            xt = [sb("xt%d" % i, [128, D]) for i in range(2)]
            T = {"sq": sb("sq", [128, D], BF16), "ss": sb("ss", [128, 1]), "rs": sb("rs", [128, 1]),
                 "hb": sb("hb", [128, D], BF16), "pst": st.enter_context(nc.psum_tensor(pre + "pst", [128, 1024], BF16))}
            aT = [sb("aT%d" % i, [128, 22, 512], BF16) for i in range(1)]
            hT = sb("hT", [128, 8, 128], BF16)
            af = sb("af", [128, 1056]); ckv = sb("ckv", [128, 256]); kpe = sb("kpe", [128, 32]); rt = sb("rt", [128, 64])
            cpb = sb("cpb", [128, 288], BF16)
            cqT = [sb("cqT%d" % i, [128, 6, 512], BF16) for i in range(2)]
            cpT = [sb("cpT%d" % i, [128, 3, 512], BF16) for i in range(2)]
            qa = [sb("qa%d" % i, [96, 512], BF16) for i in range(2)]
            t1 = sb("t1", [96, 512]); t2 = sb("t2", [96, 512])
            ps = [psf("ps%d" % i) for i in range(6)]
            pi = 0
            self.ld(wdn[:], I["w_dn0"].rearrange("(j p) n -> p j n", p=128), [], [pre + "wdn"], eng="pool")
            self.ld(wa[:], I["w_a"].rearrange("(kc p) n -> p kc n", p=128), [], [pre + "wa"], eng="pool")
            self.ld(wuq[:], I["w_uq"].rearrange("(kc p) n -> p kc n", p=128), [], [pre + "wuq"], eng="pool")
            self.ld(wuqs[:], I["w_uqs"].rearrange("(kc p) n -> p kc n", p=128), [], [pre + "wuqs"], eng="pool")
            self.ld(gb[:], I["g_mix1"], [], ["gains"])
            self.ld(gq[:], I["g_q"], [], [pre + "gq"])
            self.ld(gkv[:], I["g_kv"], [], [pre + "gkv"])
            bi = 0
            qi = 0
            for ti, (t0, nt) in enumerate(self.tiles):
                hs = ti % 2
                for c in range(2):
                    self.ld(csf[64:96, c, 0:nt], I["cs_fm"][c, :, t0:t0 + nt], [], [pre + "csf"])
                self.ld(aT[0][:, :, 0:nt], S["actT"][:, :, t0:t0 + nt].rearrange("j p t -> p j t"), ["d_actT"], [pre + "aT0"])
                for b in range(nt // 128):
                    xs = bi % 2; bi += 1
                    r0 = t0 + b * 128
                    xk = pre + "xt%d" % xs
                    self.ld(xt[xs][:], S["x1"][r0:r0 + 128, :], ["d_x1"], [xk])
                    self.ld(cst[:], I["cs_tm"][r0:r0 + 128, :], [], [pre + "cst"])
                    for n in range(2):
                        p_ = ps[pi % 6]; pk = pre + "ps%d" % (pi % 6); pi += 1
                        for j in range(22):
                            self.mm(p_[:], aT[0][:, j, b * 128:(b + 1) * 128], wdn[:, j, n * 512:(n + 1) * 512], j == 0, j == 21,
                                    [pre + "aT0", pre + "wdn"], [pk])
                        self.tt("dve", xt[xs][:, n * 512:(n + 1) * 512], xt[xs][:, n * 512:(n + 1) * 512], p_[:], ALU.add, [xk, pk], [xk])
                    self.st(S["x2"][r0:r0 + 128, :], xt[xs][:], [xk], ["d_x2"], semkey=pre + "stx%d" % xs)
                    self.norm_T(T, xt[xs][:], xk, gb[:], D, None, None, pre)
                    self.copy("act", hT[:], T["pst"][:].rearrange("p (k c) -> p k c", k=8), [pre + "pst"], [pre + "hT"])
                    for (c0, cw) in ((0, 512), (512, 512), (1024, 32)):
                        p_ = ps[pi % 6]; pk = pre + "ps%d" % (pi % 6); pi += 1
                        for kc in range(8):
                            self.mm(p_[:, 0:cw], hT[:, kc, :], wa[:, kc, c0:c0 + cw], kc == 0, kc == 7, [pre + "hT", pre + "wa"], [pk])
                        self.copy("act" if c0 == 512 else "dve", af[:, c0:c0 + cw], p_[:, 0:cw], [pk], [pre + "af"])
                    self.norm_T(T, af[:, 0:768], pre + "af", gq[:], 768, None, None, pre)
                    self.copy("act", cqT[hs][:, :, b * 128:(b + 1) * 128], T["pst"][:, 0:768].rearrange("p (k c) -> p k c", k=6),
                              [pre + "pst"], [pre + "cqT%d" % hs, pre + "pst"])
                    self.rstd(af[:, 768:1024], 256, T["sq"][:, 0:256], T["ss"][:, 0:1], T["rs"][:, 0:1], pre + "af", pre)
                    self.stt("dve", ckv[:], af[:, 768:1024], T["rs"][:, 0:1], gkv[:], ALU.mult, ALU.mult,
                             [pre + "af", pre + "rs", pre + "gkv"], [pre + "ckv"])
                    self.st(O["ckv"][r0:r0 + 128, :], ckv[:], [pre + "ckv"], [], final=True, semkey=pre + "stc")
                    x1_, x2_ = af[:, 1024:1040], af[:, 1040:1056]
                    co, si_ = cst[:, 0:16], cst[:, 16:32]
                    self.tt("dve", rt[:, 0:16], x1_, co, ALU.mult, [pre + "af", pre + "cst"], [pre + "rt"])
                    self.tt("dve", rt[:, 16:32], x2_, si_, ALU.mult, [pre + "af", pre + "cst", pre + "rt"], [pre + "rt"])
                    self.tt("dve", rt[:, 32:48], x2_, co, ALU.mult, [pre + "af", pre + "cst", pre + "rt"], [pre + "rt"])
                    self.tt("dve", rt[:, 48:64], x1_, si_, ALU.mult, [pre + "af", pre + "cst", pre + "rt"], [pre + "rt"])
                    self.tt("dve", kpe[:, 0:16], rt[:, 0:16], rt[:, 16:32], ALU.subtract, [pre + "rt"], [pre + "kpe"])
                    self.tt("dve", kpe[:, 16:32], rt[:, 32:48], rt[:, 48:64], ALU.add, [pre + "rt", pre + "kpe"], [pre + "kpe"])
                    self.st(O["kpe"][r0:r0 + 128, :], kpe[:], [pre + "kpe"], [], final=True, semkey=pre + "stp")
                    self.copy("pool", cpb[:, 0:256], ckv[:], [pre + "ckv"], [pre + "cpb"])
                    self.copy("pool", cpb[:, 256:288], kpe[:], [pre + "kpe", pre + "cpb"], [pre + "cpb"])
                    for ch in range(3):
                        wd = 128 if ch < 2 else 32
                        self.tr(T["pst"][0:wd, ch * 128:(ch + 1) * 128], cpb[:, ch * 128:ch * 128 + wd], self.ident[:],
                                [pre + "cpb", "ident", pre + "pst"], [pre + "pst"])
                    self.copy("act", cpT[hs][:, 0:2, b * 128:(b + 1) * 128], T["pst"][:, 0:256].rearrange("p (k c) -> p k c", k=2),
                              [pre + "pst"], [pre + "cpT%d" % hs, pre + "pst"])
                    self.copy("act", cpT[hs][0:32, 2, b * 128:(b + 1) * 128], T["pst"][0:32, 256:384],
                              [pre + "pst"], [pre + "cpT%d" % hs, pre + "pst"])
                if t0 < NP:
                    for ch in range(3):
                        wd = 128 if ch < 2 else 32
                        self.st(S["cpl"][ch][t0 // self.CW][0:wd, t0 % self.CW:t0 % self.CW + nt], cpT[hs][0:wd, ch, 0:nt], [pre + "cpT%d" % hs], ["d_cpl"], semkey=pre + "stcp%d" % hs)
                else:
                    self.st(S["cps"][0:2].rearrange("c p t -> p c t"), cpT[hs][:, 0:2, 0:nt], [pre + "cpT%d" % hs], ["d_cps"], semkey=pre + "stcp%d" % hs)
                    self.st(S["cps"][2, 0:32, :], cpT[hs][0:32, 2, 0:nt], [pre + "cpT%d" % hs], ["d_cps"], semkey=pre + "stcp%d" % hs)
                for h in range(16):
                    pa = ps[pi % 6]; pak = pre + "ps%d" % (pi % 6); pi += 1
                    pb_ = ps[pi % 6]; pbk = pre + "ps%d" % (pi % 6); pi += 1
                    for kc in range(6):
                        self.mm(pa[0:96, 0:nt], wuq[:, kc, h * 96:(h + 1) * 96], cqT[hs][:, kc, 0:nt], kc == 0, kc == 5,
                                [pre + "wuq", pre + "cqT%d" % hs], [pak])
                    for kc in range(6):
                        self.mm(pb_[0:96, 0:nt], wuqs[:, kc, h * 96:(h + 1) * 96], cqT[hs][:, kc, 0:nt], kc == 0, kc == 5,
                                [pre + "wuqs", pre + "cqT%d" % hs], [pbk])
                    qs = qi % 2; qi += 1
                    qk_ = pre + "qa%d" % qs
                    self.copy("act", qa[qs][0:64, 0:nt], pa[0:64, 0:nt], [pak], [qk_])
                    self.tt("dve", t1[64:96, 0:nt], pa[64:96, 0:nt], csf[64:96, 0, 0:nt], ALU.mult, [pak, pre + "csf"], [pre + "t1"])
                    self.tt("dve", t2[64:96, 0:nt], pb_[64:96, 0:nt], csf[64:96, 1, 0:nt], ALU.mult, [pbk, pre + "csf"], [pre + "t2"])
                    self.tt("pool", qa[qs][64:96, 0:nt], t1[64:96, 0:nt], t2[64:96, 0:nt], ALU.add, [pre + "t1", pre + "t2", qk_], [qk_])
                    self.st(S["qaT"][h, :, t0:t0 + nt], qa[qs][:, 0:nt], [qk_], ["d_qaT"], semkey=pre + "stq%d" % qs)

    def phase_D0(self):
        nc, P, I, O, S = self.nc, self.P, self.I, self.O, self.S
        NP = self.NP
        pre = "D0_"
        with contextlib.ExitStack() as st:
            sb = lambda n, s, d=F32: st.enter_context(nc.sbuf_tensor(pre + n, list(s), d))
            psf = lambda n: st.enter_context(nc.psum_tensor(pre + n, [128, 512], F32))
            wk = sb("wk", [128, 3, 16, 96], BF16); wv = sb("wv", [128, 2, 1024], BF16)
            self.build_wk(wk, wv, pre)
            cT = [sb("cT%d" % i, [128, 3, 512], BF16) for i in range(2)]
            ka = [sb("ka%d" % i, [96, 16, 512], BF16) for i in range(2)]
            vt = [sb("vt%d" % i, [128, D], BF16) for i in range(2)]
            ps = [psf("ps%d" % i) for i in range(6)]
            pi = 0
            jobs = []
            for rk in range(4):
                for t0 in range(0, NP, 512):
                    jobs.append(("g", rk, t0, 512))
            for t0 in range(0, NP, 512):
                jobs.append(("l", 0, t0, 512))
            jobs.append(("s", 0, 0, 128))
            vi = 0
            for ji, (kind, rk, t0, nt) in enumerate(jobs):
                cs = ji % 2
                ck = pre + "cT%d" % cs
                if kind == "g":
                    for ch in range(3):
                        wd = 128 if ch < 2 else 32
                        self.ld(cT[cs][0:wd, ch, :], S["cpg"][ch][t0 // self.CW][rk * 128:rk * 128 + wd, t0 % self.CW:t0 % self.CW + 512], ["d_cpg"], [ck])
                elif kind == "l":
                    for ch in range(3):
                        wd = 128 if ch < 2 else 32
                        self.ld(cT[cs][0:wd, ch, :], S["cpl"][ch][t0 // self.CW][0:wd, t0 % self.CW:t0 % self.CW + 512], ["d_cpl"], [ck])
                else:
                    self.ld(cT[cs][:, 0:2, 0:128], S["cps"][0:2].rearrange("c p t -> p c t"), ["d_cps"], [ck])
                    self.ld(cT[cs][0:32, 2, 0:128], S["cps"][2, 0:32, :], ["d_cps"], [ck])
                kk = pre + "ka%d" % cs
                for h in range(16):
                    p_ = ps[pi % 6]; pk = pre + "ps%d" % (pi % 6); pi += 1
                    for ch in range(3):
                        wd = 128 if ch < 2 else 32
                        self.mm(p_[0:96, 0:nt], wk[0:wd, ch, h, :], cT[cs][0:wd, ch, 0:nt], ch == 0, ch == 2, [pre + "wk", ck], [pk])
                    self.copy("act" if h % 2 else "dve", ka[cs][:, h, 0:nt], p_[0:96, 0:nt], [pk], [kk])
                if kind == "g":
                    self.st(S["kaT"][:, :, rk * NP + t0:rk * NP + t0 + 512].rearrange("h p t -> p h t"), ka[cs][:], [kk], ["d_kaT"], semkey=pre + "stk%d" % cs)
                elif kind == "l":
                    self.st(S["kaTl"][:, :, t0:t0 + 512].rearrange("h p t -> p h t"), ka[cs][:], [kk], ["d_kTl"], semkey=pre + "stk%d" % cs)
                else:
                    self.st(S["kaTs"].rearrange("h p t -> p h t"), ka[cs][:, :, 0:128], [kk], ["d_kaTs"], semkey=pre + "stk%d" % cs)
                for b in range(nt // 128):
                    vs_ = vi % 2; vi += 1
                    vk = pre + "vt%d" % vs_
                    for n in range(2):
                        p_ = ps[pi % 6]; pk = pre + "ps%d" % (pi % 6); pi += 1
                        for kc in range(2):
                            self.mm(p_[:], cT[cs][:, kc, b * 128:(b + 1) * 128], wv[:, kc, n * 512:(n + 1) * 512], kc == 0, kc == 1,
                                    [ck, pre + "wv"], [pk])
                        self.copy("act" if n else "dve", vt[vs_][:, n * 512:(n + 1) * 512], p_[:], [pk], [vk])
                    r0 = t0 + b * 128
                    if kind == "g":
                        self.st(S["v1"][rk * NP + r0:rk * NP + r0 + 128, :], vt[vs_][:], [vk], ["d_v1"], semkey=pre + "stv%d" % vs_)
                    elif kind == "l":
                        self.st(S["v1l"][r0:r0 + 128, :], vt[vs_][:], [vk], ["d_vl"], semkey=pre + "stv%d" % vs_)
                    else:
                        self.st(S["v1s"], vt[vs_][:], [vk], ["d_v1s"], semkey=pre + "stv%d" % vs_)

    def phase_E1(self):
        nc, P, I, O, S = self.nc, self.P, self.I, self.O, self.S
        pre = "E1_"
        with contextlib.ExitStack() as st:
            sb = lambda n, s, d=F32: st.enter_context(nc.sbuf_tensor(pre + n, list(s), d))
            psf = lambda n: st.enter_context(nc.psum_tensor(pre + n, [128, 512], F32))
            wo = sb("wo", [64, 16, D], BF16); wr = sb("wr", [128, 8, 8], BF16); gb = sb("gb", [128, D])
            xt = [sb("xt%d" % i, [128, D]) for i in range(2)]
            T = {"sq": sb("sq", [128, D], BF16), "ss": sb("ss", [128, 1]), "rs": sb("rs", [128, 1]),
                 "hb": sb("hb", [128, D], BF16), "pst": st.enter_context(nc.psum_tensor(pre + "pst", [128, 1024], BF16))}
            oTt = [sb("oTt%d" % i, [64, 16, 512], BF16) for i in range(2)]
            hT = [sb("hT%d" % i, [128, 8, 128], BF16) for i in range(2)]
            lg = sb("lg", [128, 8]); m1 = sb("m1", [128, 1]); m2 = sb("m2", [128, 1]); eq = sb("eq", [128, 8]); l2 = sb("l2", [128, 8])
            ex = sb("ex", [128, 8]); sm = sb("sm", [128, 1]); cb = [sb("cb%d" % i, [128, 8]) for i in range(2)]
            ps = [psf("ps%d" % i) for i in range(6)]
            pi = 0
            self.ld(wo[:], I["w_o1"].rearrange("(h p) n -> p h n", p=64), [], [pre + "wo"], eng="pool")
            self.ld(wr[:], I["w_r"].rearrange("(kc p) n -> p kc n", p=128), [], [pre + "wr"], eng="pool")
            self.ld(gb[:], I["g_ffn1"], [], ["gains"])
            bi = 0
            for ti, (t0, nt) in enumerate(self.tiles):
                hs = ti % 2
                self.ld(oTt[hs][:, :, 0:nt], S["oT1"][:, :, t0:t0 + nt].rearrange("h p t -> p h t"), ["d_oT1"], [pre + "oTt%d" % hs])
                for b in range(nt // 128):
                    xs = bi % 2; bi += 1
                    r0 = t0 + b * 128
                    xk = pre + "xt%d" % xs
                    self.ld(xt[xs][:], S["x2"][r0:r0 + 128, :], ["d_x2"], [xk])
                    for n in range(2):
                        p_ = ps[pi % 6]; pk = pre + "ps%d" % (pi % 6); pi += 1
                        for h in range(16):
                            self.mm(p_[:], oTt[hs][:, h, b * 128:(b + 1) * 128], wo[:, h, n * 512:(n + 1) * 512], h == 0, h == 15,
                                    [pre + "oTt%d" % hs, pre + "wo"], [pk])
                        self.tt("dve", xt[xs][:, n * 512:(n + 1) * 512], xt[xs][:, n * 512:(n + 1) * 512], p_[:], ALU.add, [xk, pk], [xk])
                    self.st(S["x3"][r0:r0 + 128, :], xt[xs][:], [xk], ["d_x3"], semkey=pre + "stx%d" % xs)
                    self.norm_T(T, xt[xs][:], xk, gb[:], D, None, None, pre)
                    hk = pre + "hT%d" % xs
                    self.copy("act", hT[xs][:], T["pst"][:].rearrange("p (k c) -> p k c", k=8), [pre + "pst"], [hk])
                    self.st(S["hT"][:, :, r0:r0 + 128].rearrange("k p t -> p k t"), hT[xs][:], [hk], ["d_hT"], semkey=pre + "sth%d" % xs)
                    p_ = ps[pi % 6]; pk = pre + "ps%d" % (pi % 6); pi += 1
                    for kc in range(8):
                        self.mm(p_[:, 0:8], hT[xs][:, kc, :], wr[:, kc, :], kc == 0, kc == 7, [hk, pre + "wr"], [pk])
                    self.copy("dve", lg[:], p_[:, 0:8], [pk], [pre + "lg"])
                    self.P.op("dve", lambda e: e.reduce_max(m1[:], lg[:], AX.X), [pre + "lg"], [pre + "m1"])
                    self.ts("dve", eq[:], lg[:], m1[:, 0:1], -1e30, ALU.is_equal, ALU.mult, [pre + "lg", pre + "m1"], [pre + "eq"])
                    self.tt("dve", l2[:], lg[:], eq[:], ALU.add, [pre + "lg", pre + "eq"], [pre + "l2"])
                    self.P.op("dve", lambda e: e.reduce_max(m2[:], l2[:], AX.X), [pre + "l2"], [pre + "m2"])
                    self.ts("dve", eq[:], lg[:], m2[:, 0:1], None, ALU.is_ge, None, [pre + "lg", pre + "m2", pre + "eq"], [pre + "eq"])
                    self.ts("dve", l2[:], lg[:], m1[:, 0:1], None, ALU.subtract, None, [pre + "lg", pre + "m1", pre + "l2"], [pre + "l2"])
                    self.act(ex[:], l2[:], AF.Exp, [pre + "l2"], [pre + "ex"])
                    self.tt("dve", ex[:], ex[:], eq[:], ALU.mult, [pre + "ex", pre + "eq"], [pre + "ex"])
                    self.P.op("dve", lambda e: e.reduce_sum(sm[:], ex[:], AX.X), [pre + "ex"], [pre + "sm"])
                    self.P.op("dve", lambda e: e.reciprocal(sm[:], sm[:]), [pre + "sm"], [pre + "sm"])
                    ck = pre + "cb%d" % xs
                    self.ts("dve", cb[xs][:], ex[:], sm[:, 0:1], None, ALU.mult, None, [pre + "ex", pre + "sm"], [ck])
                    self.st(S["comb"][r0:r0 + 128, :], cb[xs][:], [ck], ["d_comb"], semkey=pre + "stc%d" % xs)

    def phase_E2(self):
        nc, P, I, O, S = self.nc, self.P, self.I, self.O, self.S
        NBLK = self.NBLK
        pre = "E2_"
        GB = 11 if NBLK % 11 == 0 else NBLK
        NG = NBLK // GB
        GT = GB * 128
        widths = []
        o_ = 0
        while o_ < GT:
            w_ = min(512, GT - o_); widths.append((o_, w_)); o_ += w_
        QF = FE // 4
        NCH = QF // 128
        with contextlib.ExitStack() as st:
            sb = lambda n, s, d=F32: st.enter_context(nc.sbuf_tensor(pre + n, list(s), d))
            psf = lambda n: st.enter_context(nc.psum_tensor(pre + n, [128, 512], F32))
            hT = sb("hT", [128, 8, GT], BF16)
            yacc = sb("yacc", [128, GB, D])
            cb = sb("cb", [128, GB, 8])
            wgu = [sb("wgu%d" % i, [128, 8, 2, QF], BF16) for i in range(2)]
            wdn = [sb("wdn%d" % i, [128, NCH, D], BF16) for i in range(2)]
            sg = [sb("sg%d" % i, [128, 512]) for i in range(2)]
            aT = [sb("aT%d" % i, [128, NCH, 512], BF16) for i in range(2)]
            gb = sb("gb", [128, D]); sq = sb("sq", [128, D], BF16); ss = sb("ss", [128, 1]); rs = sb("rs", [128, 1])
            yo = [sb("yo%d" % i, [128, D]) for i in range(2)]
            ps = [psf("ps%d" % i) for i in range(7)]
            pi = 0
            self.ld(gb[:], I["g_fin"], [], ["gains"])
            ui = 0
            ai = 0
            for gi in range(NG):
                g0 = gi * GT
                self.ld(hT[:], S["hT"][:, :, g0:g0 + GT].rearrange("k p t -> p k t"), ["d_hT"], [pre + "hT"])
                self.ld(cb[:], S["comb"][g0:g0 + GT, :].rearrange("(b p) e -> p b e", p=128), ["d_comb"], [pre + "cb"])
                self.ld(yacc[:], S["x3"][g0:g0 + GT, :].rearrange("(b p) e -> p b e", p=128), ["d_x3"], [pre + "yacc%d" % b_ for b_ in range(GB)])
                for e_ in range(NE):
                    for q in range(4):
                        ws = ui % 2; ui += 1
                        wgk, wdk = pre + "wgu%d" % ws, pre + "wdn%d" % ws
                        src = I["w_gu1"][e_].rearrange("(kc p) (two f) -> p kc two f", p=128, two=2)
                        for two in range(2):
                            self.ld(wgu[ws][:, :, two, :], src[:, :, two, q * QF:(q + 1) * QF], [], [wgk], eng="pool")
                        self.ld(wdn[ws][:], I["w_dn1"][e_, q * QF:(q + 1) * QF, :].rearrange("(j p) n -> p j n", p=128), [], [wdk], eng="pool")
                        for (o0, wd) in widths:
                            as_ = ai % 2; ai += 1
                            ak = pre + "aT%d" % as_
                            for j in range(NCH):
                                pg = ps[pi % 7]; pgk = pre + "ps%d" % (pi % 7); pi += 1
                                pu = ps[pi % 7]; puk = pre + "ps%d" % (pi % 7); pi += 1
                                for kc in range(8):
                                    self.mm(pg[:, 0:wd], wgu[ws][:, kc, 0, j * 128:(j + 1) * 128], hT[:, kc, o0:o0 + wd], kc == 0, kc == 7,
                                            [wgk, pre + "hT"], [pgk])
                                for kc in range(8):
                                    self.mm(pu[:, 0:wd], wgu[ws][:, kc, 1, j * 128:(j + 1) * 128], hT[:, kc, o0:o0 + wd], kc == 0, kc == 7,
                                            [wgk, pre + "hT"], [puk])
                                sgs = j % 2
                                self.act(sg[sgs][:, 0:wd], pg[:, 0:wd], AF.Silu, [pgk], [pre + "sg%d" % sgs])
                                self.tt("dve", aT[as_][:, j, 0:wd], sg[sgs][:, 0:wd], pu[:, 0:wd], ALU.mult, [pre + "sg%d" % sgs, puk], [ak])
                            for b in range(wd // 128):
                                blk = o0 // 128 + b
                                for n in range(2):
                                    p_ = ps[pi % 7]; pk = pre + "ps%d" % (pi % 7); pi += 1
                                    for j in range(NCH):
                                        self.mm(p_[:], aT[as_][:, j, b * 128:(b + 1) * 128], wdn[ws][:, j, n * 512:(n + 1) * 512], j == 0, j == NCH - 1,
                                                [ak, wdk], [pk])
                                    self.stt("dve", yacc[:, blk, n * 512:(n + 1) * 512], p_[:], cb[:, blk, e_:e_ + 1],
                                             yacc[:, blk, n * 512:(n + 1) * 512], ALU.mult, ALU.add, [pk, pre + "cb", pre + "yacc%d" % blk], [pre + "yacc%d" % blk])
                for blk in range(GB):
                    ys = blk % 2
                    x = yacc[:, blk, :]
                    self.memset("pool", ss[:], 0.0, [pre + "ss"])
                    self.act(sq[:], x, AF.Square, [pre + "yacc%d" % blk, pre + "ss"], [pre + "sq", pre + "ss"], accum_out=ss[:])
                    self.act(rs[:], ss[:], AF.Ln, [pre + "ss"], [pre + "rs"], scale=1.0 / D, bias=EPS)
                    self.act(rs[:], rs[:], AF.Exp, [pre + "rs"], [pre + "rs"], scale=-0.5)
                    self.stt("dve", yo[ys][:], x, rs[:, 0:1], gb[:], ALU.mult, ALU.mult, [pre + "yacc%d" % blk, pre + "rs", "gains"], [pre + "yo%d" % ys])
                    r0 = g0 + blk * 128
                    self.st(O["y"][r0:r0 + 128, :], yo[ys][:], [pre + "yo%d" % ys], [], final=True, semkey=pre + "sty%d" % ys)


def host_tables(T, PAST, r):
    NB = T // 2048
    NKB = 16 * NB
    NP = NB * 512
    NBLKP = NB * 4
    f = np.float32
    k = np.arange(128)[:, None]
    q = np.arange(512)[None, :]
    T0 = (-(q - k)).astype(f)
    Dneg = np.zeros((128, 4, 512), f)
    Dmask = np.zeros((128, 4, 512), f)
    for i in range(4):
        kp = 128 * i + k
        vis = (kp // 64) <= (q // 64)
        Dneg[:, i, :] = np.where(vis, -np.abs(q - kp), -1e30)
        Dmask[:, i, :] = np.where(vis, 0.0, -1e30)
    ct = np.zeros((8, NB, NKB), f)
    mt = np.zeros((NB, NKB), f)
    for J in range(NB):
        gq = 4 * J + r
        for rk in range(4):
            for Jk in range(NB):
                gk = 4 * Jk + rk
                for i in range(4):
                    kbi = rk * NBLKP + Jk * 4 + i
                    if gk < gq:
                        dlt = (gq - gk) * 512 - 128 * i
                        for h in range(8):
                            ct[h, J, kbi] = -SLOPES[h] * dlt
                        mt[J, kbi] = 0.0
                    else:
                        ct[:, J, kbi] = -30000.0
                        mt[J, kbi] = -30000.0
    ctab0 = np.ascontiguousarray(np.broadcast_to(ct.reshape(1, -1), (128, 8 * NB * NKB)))
    mtab = np.ascontiguousarray(np.broadcast_to(mt.reshape(1, -1), (128, NB * NKB)))
    NTOK = (NBLKP + 1) * 128
    pos = np.zeros(NTOK, np.int64)
    for J in range(NB):
        pos[J * 512:(J + 1) * 512] = (4 * J + r) * 512 + np.arange(512)
    for s in range(2):
        pos[NP + s * 32:NP + s * 32 + 16] = PAST + np.arange(16)
    half = 16
    freqs = np.power(np.float32(10000.0), -np.arange(half, dtype=f) * np.float32(2.0) / np.float32(32)).astype(f)
    ang = pos.astype(f)[:, None] * freqs[None, :]
    co, si = np.cos(ang).astype(f), np.sin(ang).astype(f)
    cs_tm = np.concatenate([co, si], 1).astype(f)
    cs_fm = np.stack([np.concatenate([co.T, co.T], 0), np.concatenate([-si.T, si.T], 0)], 0).astype(f)
    PB = PAST // 128
    kb = np.arange(PB)[None, :, None]
    ii = np.arange(16)[None, None, :]
    Dnegs = (-(PAST + ii - 128 * kb - k[:, :, None])).astype(f).reshape(128, PB * 16)
    kn = np.arange(16)[:, None]
    Dnegn = (-np.abs(np.arange(16)[None, :] - kn)).astype(f)
    return dict(T0=T0, Dneg=Dneg, Dmask=Dmask, ctab0=ctab0, mtab=mtab, cs_tm=cs_tm, cs_fm=np.ascontiguousarray(cs_fm),
                Dnegs=np.ascontiguousarray(Dnegs), Dnegn=Dnegn, ident=np.eye(128, dtype=f))


_CACHE = {}


def run(inputs, T, PAST, dbg=()):
    f = np.float32
    A = {k_: np.asarray(v) for k_, v in inputs.items()}
    NB = T // 2048
    NP = NB * 512
    NTOK = (NB * 4 + 1) * 128
    key = (T, PAST, tuple(dbg))
    if key not in _CACHE:
        _CACHE[key] = Builder(T, PAST, dbg).build()
    nc = _CACHE[key]
    bc = lambda v, n=128: np.ascontiguousarray(np.broadcast_to(np.asarray(v, f).reshape(1, -1), (n, np.asarray(v).size)))
    w_uq = A["mla_w_uq"][0]
    wq4 = w_uq.reshape(768, 16, 96)
    w_uqs = np.concatenate([wq4[:, :, 0:64], wq4[:, :, 80:96], wq4[:, :, 64:80]], axis=2).reshape(768, 1536)
    common = dict(
        g_mix0=bc(A["norm_mix"][0]), g_mix1=bc(A["norm_mix"][1]), g_ffn0=bc(A["norm_ffn"][0]), g_ffn1=bc(A["norm_ffn"][1]),
        g_fin=bc(A["norm_final"]), lam=bc(A["diff_lambda"][0].reshape(-1)), gsub_col=np.ascontiguousarray(A["diff_subln"][0].reshape(128, 1)),
        gsub_row=bc(A["diff_subln"][0]), g_q=bc(A["mla_norm_q"][0]), g_kv=bc(A["mla_norm_kv"][0]),
        w_qkv=A["diff_w_qkv"][0], w_o0=A["diff_w_o"][0], w_gu0=A["ffn_w_gu"][0], w_dn0=A["ffn_w_down"][0],
        w_a=A["mla_w_a"][0], w_uq=w_uq, w_uqs=np.ascontiguousarray(w_uqs), w_ukv=A["mla_w_ukv"][0], w_o1=A["mla_w_o"][0],
        w_r=A["moe_router"][0], w_gu1=A["moe_w_gu"][0], w_dn1=A["moe_w_down"][0])
    in_maps = []
    for c in range(8):
        b, r = c // 4, c % 4
        xin = np.zeros((NTOK, D), f)
        xp = A["x_prompt"][b].reshape(T // 512, 512, D)
        xin[:NP] = xp[r::4].reshape(NP, D)
        for s in range(2):
            xin[NP + s * 32:NP + s * 32 + 16] = A["x_sample"][2 * c + s]
        m = dict(common)
        m.update(host_tables(T, PAST, r))
        m["xin"] = xin
        m["cdk"] = np.ascontiguousarray(A["cache_diff_k"][0, 2 * c:2 * c + 2].reshape(2, PAST, D))
        m["cdv"] = np.ascontiguousarray(A["cache_diff_v"][0, 2 * c:2 * c + 2].reshape(2, PAST, D))
        m["cckv"] = np.ascontiguousarray(A["cache_mla_ckv"][0, 2 * c:2 * c + 2])
        m["ckpe"] = np.ascontiguousarray(A["cache_mla_kpe"][0, 2 * c:2 * c + 2])
        in_maps.append(m)
    res = run_bass_kernel_spmd(nc, in_maps, core_ids=list(range(8)))
    B = 2
    outs = {}
    shapes = dict(y=D, dk=D, dv=D, ckv=256, kpe=32)
    for name, wdt in shapes.items():
        pr = np.zeros((B, T // 512, 512, wdt), f)
        sm = np.zeros((16, 16, wdt), f)
        for c in range(8):
            b, r = c // 4, c % 4
            o = res.results[c][name]
            pr[b, r::4] = o[:NP].reshape(NB, 512, wdt)
            for s in range(2):
                sm[2 * c + s] = o[NP + s * 32:NP + s * 32 + 16]
        outs[name] = (pr.reshape(B, T, wdt), sm)
    dbgout = {n: [res.results[c][n] for c in range(8)] for n in dbg}
    y_p, y_s = outs["y"]
    dk_p, dk_s = outs["dk"]
    dv_p, dv_s = outs["dv"]
    ck_p, ck_s = outs["ckv"]
    kp_p, kp_s = outs["kpe"]
    out = (y_p, y_s, dk_p.reshape(1, B, T, 8, 128), dv_p.reshape(1, B, T, 8, 128), ck_p.reshape(1, B, T, 256), kp_p.reshape(1, B, T, 32),
           dk_s.reshape(1, 16, 16, 8, 128), dv_s.reshape(1, 16, 16, 8, 128), ck_s.reshape(1, 16, 16, 256), kp_s.reshape(1, 16, 16, 32))
    if dbg:
        return out, dbgout
    return out


def kernel(**inputs):
    T = inputs["x_prompt"].shape[1]
    PAST = inputs["cache_diff_k"].shape[2]
    return run(inputs, T, PAST)
```

```python
import contextlib
import math
import numpy as np
import concourse.bass as bass
import concourse.mybir as mybir
from concourse.bass_utils import run_bass_kernel_spmd

F32 = mybir.dt.float32
BF16 = mybir.dt.bfloat16
ALU = mybir.AluOpType
AF = mybir.ActivationFunctionType
AX = mybir.AxisListType
ENGS = ("pe", "act", "dve", "pool", "sp")


class Op:
    __slots__ = ("eng", "fn", "reads", "writes", "is_dma", "semkey", "waits", "sig", "signal")

    def __init__(self, eng, fn, reads, writes, is_dma, semkey):
        self.eng, self.fn, self.reads, self.writes = eng, fn, reads, writes
        self.is_dma, self.semkey = is_dma, semkey
        self.waits = {}
        self.sig = None
        self.signal = False


class Prog:
    def __init__(self, nc):
        self.nc = nc
        self.ops = []
        self.last_writer = {}
        self.readers = {}
        self.dma_count = {}
        self.final_dma = {}
        self.pending = {}
        self.last_op = {}
        self.sem_of = {}
        self.phys_count = []

    def barrier(self):
        lasts = [o for o in self.last_op.values()]
        for o in lasts:
            o.signal = True
        dm = {i: v for i, v in enumerate(self.phys_count)}
        for e in ENGS:
            self.pending[e] = (list(lasts), dict(dm))
        self.last_writer = {}
        self.readers = {}
        self.sem_of = {}

    def _add(self, op):
        deps = []
        for r in op.reads:
            w = self.last_writer.get(r)
            if w is not None:
                deps.append(w)
        for r in op.writes:
            w = self.last_writer.get(r)
            if w is not None:
                deps.append(w)
            deps.extend(self.readers.get(r, ()))
        pend = self.pending.pop(op.eng, None)
        if pend is not None:
            for d in pend[0]:
                if d.eng != op.eng:
                    op.waits.setdefault("_dep", []).append(d)
            for k, v in pend[1].items():
                kk = "dma:%s" % (k,)
                op.waits[kk] = max(op.waits.get(kk, 0), v)
        for d in deps:
            if d is op:
                continue
            if d.eng == "pe" and op.eng == "pe" and not d.is_dma and not op.is_dma:
                continue
            d.signal = True
            if d.is_dma:
                key = "dma:%s" % (d.semkey,)
                op.waits[key] = max(op.waits.get(key, 0), self.phys_count[d.semkey])
            else:
                op.waits.setdefault("_dep", []).append(d)
        for r in op.reads:
            self.readers.setdefault(r, []).append(op)
        for r in op.writes:
            self.last_writer[r] = op
            self.readers[r] = []
        self.ops.append(op)
        if not op.is_dma:
            self.last_op[op.eng] = op
        return op

    def op(self, eng, fn, reads=(), writes=()):
        return self._add(Op(eng, fn, tuple(reads), tuple(writes), False, None))

    def dma(self, eng, out, in_, reads=(), writes=(), semkey=None, final=False, **kw):
        if semkey is None:
            semkey = writes[0] if writes else reads[0]
        if semkey not in self.sem_of:
            self.sem_of[semkey] = len(self.sem_of)
            if len(self.phys_count) < len(self.sem_of):
                self.phys_count.append(0)
        phys = self.sem_of[semkey]
        o = Op(eng, (lambda e, out=out, in_=in_, kw=kw: e.dma_start(out=out, in_=in_, **kw)),
               tuple(reads), tuple(writes), True, phys)
        r = self._add(o)
        self.phys_count[phys] += 16
        o.sig = ("dma:%s" % (phys,), self.phys_count[phys])
        return r

    def emit(self, final_engine="sp"):
        nc = self.nc
        cnt = {e: 0 for e in ENGS}
        for o in self.ops:
            if not o.is_dma and o.signal:
                cnt[o.eng] += 1
                o.sig = ("eng:%s" % o.eng, cnt[o.eng])
        for o in self.ops:
            for d in o.waits.pop("_dep", []):
                k, v = d.sig
                o.waits[k] = max(o.waits.get(k, 0), v)
        semnames = set()
        for o in self.ops:
            if o.sig is not None:
                semnames.add(o.sig[0])
            semnames.update(o.waits.keys())
        semnames = sorted(semnames)
        self.n_sems = len(semnames)
        with contextlib.ExitStack() as st:
            sems = {n: st.enter_context(nc.semaphore("s%d" % i)) for i, n in enumerate(semnames)}
            block = st.enter_context(nc.Block())
            per = {e: [o for o in self.ops if o.eng == e] for e in ENGS}
            final_dma = {i: v for i, v in enumerate(self.phys_count)}

            def run(engname, e):
                waited = {}
                for o in per[engname]:
                    for k, v in o.waits.items():
                        if waited.get(k, 0) >= v:
                            continue
                        e.wait_ge(sems[k], v)
                        waited[k] = v
                    ins = o.fn(e)
                    if o.sig is not None and (o.signal or o.is_dma):
                        ins.then_inc(sems[o.sig[0]], 16 if o.is_dma else 1)
                if engname == final_engine:
                    for key, v in final_dma.items():
                        k = "dma:%s" % (key,)
                        if waited.get(k, 0) < v:
                            e.wait_ge(sems[k], v)

            block.tensor(lambda e: run("pe", e))
            block.scalar(lambda e: run("act", e))
            block.vector(lambda e: run("dve", e))
            block.gpsimd(lambda e: run("pool", e))
            block.sync(lambda e: run("sp", e))


D = 1024
EPS = 1e-6
SLOPES = [2.0 ** (-(h + 1)) for h in range(8)]
LAM_INIT0 = 0.8 - 0.6 * math.exp(-0.3 * 0)
FD = 2816
FE = 3584
NE = 8


class Builder:
    def __init__(self, T, PAST, dbg=()):
        self.T, self.PAST = T, PAST
        self.NB = T // 2048
        self.NP = self.NB * 512
        self.NBLKP = self.NB * 4
        self.NBLK = self.NBLKP + 1
        self.NTOK = self.NBLK * 128
        self.PB = PAST // 128
        self.NKB = 16 * self.NB
        self.dbg = set(dbg)
        self.nc = bass.Bass("TRN2", target_bir_lowering=False)
        self.P = Prog(self.nc)
        self.tiles = [(j * 512, 512) for j in range(self.NB)] + [(self.NP, 128)]
        self.uid = 0

    def din(self, name, shape, dt=F32):
        return self.nc.dram_tensor(name, list(shape), dt, kind="ExternalInput").ap()

    def dout(self, name, shape, dt=F32):
        return self.nc.dram_tensor(name, list(shape), dt, kind="ExternalOutput").ap()

    def dscr(self, name, shape, dt=BF16):
        if name in self.dbg:
            return self.nc.dram_tensor(name, list(shape), dt, kind="ExternalOutput").ap()
        return self.nc.dram_tensor(name, list(shape), dt).ap()

    def mm(self, out, lhsT, rhs, start, stop, reads, writes):
        self.P.op("pe", lambda e: e.matmul(out, lhsT, rhs, start=start, stop=stop), reads, writes)

    def tr(self, out, in_, ident, reads, writes):
        self.P.op("pe", lambda e: e.transpose(out, in_, ident), reads, writes)

    def act(self, out, in_, func, reads, writes, **kw):
        self.P.op("act", lambda e: e.activation(out, in_, func, **kw), reads, writes)

    def copy(self, eng, out, in_, reads, writes):
        if eng == "act":
            self.P.op("act", lambda e: e.copy(out, in_), reads, writes)
        else:
            self.P.op(eng, lambda e: e.tensor_copy(out, in_), reads, writes)

    def tt(self, eng, out, a, b, op, reads, writes):
        self.P.op(eng, lambda e: e.tensor_tensor(out, a, b, op), reads, writes)

    def ts(self, eng, out, a, s1, s2, op0, op1, reads, writes):
        if s2 is None:
            self.P.op(eng, lambda e: e.tensor_scalar(out, a, s1, None, op0), reads, writes)
        else:
            self.P.op(eng, lambda e: e.tensor_scalar(out, a, s1, s2, op0, op1), reads, writes)

    def stt(self, eng, out, a, s, b, op0, op1, reads, writes):
        self.P.op(eng, lambda e: e.scalar_tensor_tensor(out, a, s, b, op0, op1), reads, writes)

    def memset(self, eng, ap, v, writes):
        self.P.op(eng, lambda e: e.memset(ap, v), (), writes)

    def ld(self, out, in_, reads, writes, eng="sp", **kw):
        self.P.dma(eng, out, in_, reads=reads, writes=writes, **kw)

    def st(self, out, in_, reads, writes, eng="sp", final=False, semkey=None):
        self.P.dma(eng, out, in_, reads=reads, writes=writes, final=final, semkey=semkey)

    def rstd(self, x, width, sq, ss, rs, xkey, tag):
        n = x.shape[0]
        self.memset("pool", ss, 0.0, [tag + "ss"])
        self.act(sq, x, AF.Square, [xkey, tag + "ss"], [tag + "sq", tag + "ss"], accum_out=ss)
        self.act(rs, ss, AF.Ln, [tag + "ss"], [tag + "rs"], scale=1.0 / width, bias=EPS)
        self.act(rs, rs, AF.Exp, [tag + "rs"], [tag + "rs"], scale=-0.5)

    def build(self):
        nc, P = self.nc, self.P
        NB, NP, NBLKP, NBLK, NTOK, PB, NKB, PAST = self.NB, self.NP, self.NBLKP, self.NBLK, self.NTOK, self.PB, self.NKB, self.PAST
        I = {}
        I["xin"] = self.din("xin", [NTOK, D])
        for n, s in [("g_mix0", [128, D]), ("g_mix1", [128, D]), ("g_ffn0", [128, D]), ("g_ffn1", [128, D]),
                     ("g_fin", [128, D]), ("lam", [128, 256]), ("gsub_col", [128, 1]), ("gsub_row", [128, 128]),
                     ("g_q", [128, 768]), ("g_kv", [128, 256]),
                     ("w_qkv", [D, 3072]), ("w_o0", [D, D]), ("w_gu0", [D, 2 * FD]), ("w_dn0", [FD, D]),
                     ("w_a", [D, 1056]), ("w_uq", [768, 1536]), ("w_uqs", [768, 1536]), ("w_ukv", [256, 2048]),
                     ("w_o1", [D, D]), ("w_r", [D, 8]), ("w_gu1", [NE, D, 2 * FE]), ("w_dn1", [NE, FE, D]),
                     ("cdk", [2, PAST, D]), ("cdv", [2, PAST, D]), ("cckv", [2, PAST, 256]), ("ckpe", [2, PAST, 32]),
                     ("ident", [128, 128]), ("T0", [128, 512]), ("Dneg", [128, 4, 512]), ("Dmask", [128, 4, 512]),
                     ("ctab0", [128, 8 * NB * NKB]), ("mtab", [128, NB * NKB]),
                     ("cs_tm", [NTOK, 32]), ("cs_fm", [2, 32, NTOK]),
                     ("Dnegs", [128, PB * 16]), ("Dnegn", [16, 16])]:
            I[n] = self.din(n, s)
        O = {}
        O["y"] = self.dout("y", [NTOK, D])
        O["dk"] = self.dout("dk", [NTOK, D])
        O["dv"] = self.dout("dv", [NTOK, D])
        O["ckv"] = self.dout("ckv", [NTOK, 256])
        O["kpe"] = self.dout("kpe", [NTOK, 32])
        S = {}
        S["qT"] = self.dscr("qT", [8, 128, NTOK])
        CW = min(NP, 1024); NCW = NP // CW
        self.CW, self.NCW = CW, NCW
        S["kTl"] = [[self.dscr("kTl%d_%d" % (h, c), [128, CW]) for c in range(NCW)] for h in range(8)]
        S["vl"] = [self.dscr("vl%d" % j, [128, D]) for j in range(NBLKP)]
        S["kTg"] = [[self.dscr("kTg%d_%d" % (h, c), [4 * 128, CW]) for c in range(NCW)] for h in range(8)]
        S["vg"] = [self.dscr("vg%d" % j, [4 * 128, D]) for j in range(NBLKP)]
        S["ksT"] = self.dscr("ksT", [8, 128, 128])
        S["vs"] = self.dscr("vs", [128, D])
        S["oT"] = self.dscr("oT", [8, 128, NTOK])
        S["x1"] = self.dscr("x1", [NTOK, D], F32)
        S["actT"] = self.dscr("actT", [22, 128, NTOK])
        S["x2"] = self.dscr("x2", [NTOK, D], F32)
        S["qaT"] = self.dscr("qaT", [16, 96, NTOK])
        S["cpl"] = [[self.dscr("cpl%d_%d" % (j, c), [128, CW]) for c in range(NCW)] for j in range(3)]
        S["cpg"] = [[self.dscr("cpg%d_%d" % (j, c), [4 * 128, CW]) for c in range(NCW)] for j in range(3)]
        S["cps"] = self.dscr("cps", [3, 128, 128])
        S["kaT"] = self.dscr("kaT", [16, 96, 4 * NP])
        S["v1"] = self.dscr("v1", [4 * NP, D])
        S["kaTl"] = self.dscr("kaTl", [16, 96, NP])
        S["v1l"] = self.dscr("v1l", [NP, D])
        S["kaTs"] = self.dscr("kaTs", [16, 96, 128])
        S["v1s"] = self.dscr("v1s", [128, D])
        S["oT1"] = self.dscr("oT1", [16, 64, NTOK])
        S["x3"] = self.dscr("x3", [NTOK, D], F32)
        S["hT"] = self.dscr("hT", [8, 128, NTOK])
        S["comb"] = self.dscr("comb", [NTOK, 8], F32)
        self.I, self.O, self.S = I, O, S

        with contextlib.ExitStack() as g:
            self.g = g
            sb = lambda n, s, d=F32: g.enter_context(nc.sbuf_tensor(n, list(s), d))
            self.ident_f = sb("ident_f", [128, 128])
            self.ident = sb("ident_b", [128, 128], BF16)
            self.ones_f = sb("ones_f", [128, 128])
            self.ones_b = sb("ones_b", [128, 128], BF16)
            self.nlam = sb("nlam", [128, 1])
            self.ld(self.ident_f[:], I["ident"], [], ["ident_f"])
            self.copy("dve", self.ident[:], self.ident_f[:], ["ident_f"], ["ident"])
            self.memset("pool", self.ones_f[:], 1.0, ["ones_f"])
            self.memset("pool", self.ones_b[:], 1.0, ["ones_b"])
            import os
            upto = int(os.environ.get("K_UPTO", "99"))
            steps = [self.phase_lam, self.phase_A,
                     lambda: ([self.gather(S["kTl"][h][c], S["kTg"][h][c], "kT") for h in range(8) for c in range(NCW)], [self.gather(S["vl"][j], S["vg"][j], "v") for j in range(NBLKP)]),
                     lambda: self.phase_attn(0), lambda: self.phase_sample_attn(0), self.phase_C1, self.phase_C2,
                     lambda: [self.gather(S["cpl"][j][c], S["cpg"][j][c], "cp") for j in range(3) for c in range(NCW)], self.phase_D0,
                     lambda: self.phase_attn(1), lambda: self.phase_sample_attn(1), self.phase_E1, self.phase_E2]
            for i, stp in enumerate(steps):
                if i >= upto:
                    break
                stp()
                P.barrier()
            P.emit()
        return nc

    def gather(self, src, dst, key):
        import os
        if key in os.environ.get("K_FAKEG", "").split(","):
            n = src.shape[0]
            for r in range(4):
                self.P.dma("sp", dst[r * n:(r + 1) * n, :], src, reads=["d_" + key + "l"], writes=["d_" + key + "g"], semkey="fakeg")
            return
        self.P.op("pool", lambda e: e.collective_compute(
            "AllGather", ALU.bypass, replica_groups=[[0, 1, 2, 3], [4, 5, 6, 7]],
            ins=[src.opt()], outs=[dst.opt()]), reads=["d_" + key + "l", "cc_chain"], writes=["d_" + key + "g", "cc_chain"])

    def phase_lam(self):
        nc, I = self.nc, self.I
        with contextlib.ExitStack() as st:
            sb = lambda n, s, d=F32: st.enter_context(nc.sbuf_tensor(n, list(s), d))
            lt = sb("lam_t", [128, 256]); pr = sb("lam_p", [128, 128]); s2 = sb("lam_s", [128, 2])
            self.ld(lt[:], I["lam"], [], ["lam_t"])
            self.tt("dve", pr[:, 0:64], lt[:, 0:64], lt[:, 64:128], ALU.mult, ["lam_t"], ["lam_p"])
            self.tt("dve", pr[:, 64:128], lt[:, 128:192], lt[:, 192:256], ALU.mult, ["lam_p", "lam_t"], ["lam_p"])
            self.P.op("dve", lambda e: e.reduce_sum(s2[:, 0:1], pr[:, 0:64], AX.X), ["lam_p"], ["lam_s"])
            self.P.op("dve", lambda e: e.reduce_sum(s2[:, 1:2], pr[:, 64:128], AX.X), ["lam_s", "lam_p"], ["lam_s"])
            self.act(s2[:], s2[:], AF.Exp, ["lam_s"], ["lam_s"])
            self.tt("dve", self.nlam[:], s2[:, 1:2], s2[:, 0:1], ALU.subtract, ["lam_s"], ["nlam"])
            self.ts("dve", self.nlam[:], self.nlam[:], -LAM_INIT0, None, ALU.add, None, ["nlam"], ["nlam"])

    def norm_T(self, st_tiles, x, xkey, gb, width, hT_dst, dstkey, tag, ev="act"):
        t = st_tiles
        self.rstd(x, width, t["sq"][:, 0:width], t["ss"][:, 0:1], t["rs"][:, 0:1], xkey, tag)
        self.stt("dve", t["hb"][:, 0:width], x, t["rs"][:, 0:1], gb, ALU.mult, ALU.mult,
                 [xkey, tag + "rs", "gains"], [tag + "hb"])
        nch = (width + 127) // 128
        for kc in range(nch):
            w = min(128, width - kc * 128)
            self.tr(t["pst"][0:w, kc * 128:(kc + 1) * 128], t["hb"][:, kc * 128:kc * 128 + w], self.ident[:],
                    [tag + "hb", "ident"], [tag + "pst"])
        return nch

    def phase_A(self):
        nc, P, I, O, S = self.nc, self.P, self.I, self.O, self.S
        NP = self.NP
        with contextlib.ExitStack() as st:
            sb = lambda n, s, d=F32: st.enter_context(nc.sbuf_tensor(n, list(s), d))
            psf = lambda n: st.enter_context(nc.psum_tensor(n, [128, 512], F32))
            w = sb("A_w", [128, 8, 3072], BF16)
            gb = sb("A_gb", [128, D])
            xt = [sb("A_xt%d" % i, [128, D]) for i in range(2)]
            T = {"sq": sb("A_sq", [128, D], BF16), "ss": sb("A_ss", [128, 1]), "rs": sb("A_rs", [128, 1]),
                 "hb": sb("A_hb", [128, D], BF16),
                 "pst": st.enter_context(nc.psum_tensor("A_pst", [128, 1024], BF16))}
            hT = [sb("A_hT%d" % i, [128, 8, 512], BF16) for i in range(2)]
            qTt = [sb("A_qT%d" % i, [128, 8, 512], BF16) for i in range(2)]
            kTt = [sb("A_kT%d" % i, [128, 8, 512], BF16) for i in range(2)]
            kvf = [sb("A_kvf%d" % i, [128, 2048]) for i in range(2)]
            vb = [sb("A_vb%d" % i, [128, D], BF16) for i in range(2)]
            ps = [psf("A_ps%d" % i) for i in range(6)]
            pi = 0
            self.ld(w[:], I["w_qkv"].rearrange("(kc p) n -> p kc n", p=128), [], ["A_w"], eng="pool")
            self.ld(gb[:], I["g_mix0"], [], ["gains"])
            bi = 0
            for ti, (t0, nt) in enumerate(self.tiles):
                hs = ti % 2
                nblk = nt // 128
                for b in range(nblk):
                    xs = bi % 2
                    r0 = t0 + b * 128
                    self.ld(xt[xs][:], I["xin"][r0:r0 + 128, :], [], ["A_xt%d" % xs])
                    self.norm_T(T, xt[xs][:], "A_xt%d" % xs, gb[:], D, None, None, "A_")
                    self.copy("act", hT[hs][:, :, b * 128:(b + 1) * 128],
                              T["pst"][:].rearrange("p (k c) -> p k c", k=8), ["A_pst"], ["A_hT%d" % hs])
                    bi += 1
                for (dst, dkey, coff, scl) in ((qTt[hs], "A_qT%d" % hs, 0, 0.125), (kTt[hs], "A_kT%d" % hs, 1024, 1.0)):
                    for h in range(8):
                        p_ = ps[pi % 6]; pk = "A_ps%d" % (pi % 6); pi += 1
                        for kc in range(8):
                            self.mm(p_[:, 0:nt], w[:, kc, coff + h * 128:coff + (h + 1) * 128], hT[hs][:, kc, 0:nt],
                                    kc == 0, kc == 7, ["A_w", "A_hT%d" % hs], [pk])
                        self.act(dst[:, h, 0:nt], p_[:, 0:nt], AF.Copy, [pk], [dkey], scale=scl)
                self.st(S["qT"][:, :, t0:t0 + nt].rearrange("h p t -> p h t"), qTt[hs][:, :, 0:nt],
                        ["A_qT%d" % hs], ["d_qT"], semkey="A_stq%d" % hs)
                if t0 < NP:
                    for h in range(8):
                        self.st(S["kTl"][h][t0 // self.CW][:, t0 % self.CW:t0 % self.CW + nt], kTt[hs][:, h, 0:nt],
                                ["A_kT%d" % hs], ["d_kTl"], semkey="A_stk%d" % hs)
                else:
                    self.st(S["ksT"].rearrange("h p t -> p h t"), kTt[hs][:, :, 0:nt],
                            ["A_kT%d" % hs], ["d_ksT"], semkey="A_stk%d" % hs)
                for b in range(nblk):
                    r0 = t0 + b * 128
                    ks = (bi + b) % 2
                    for n in range(4):
                        p_ = ps[pi % 6]; pk = "A_ps%d" % (pi % 6); pi += 1
                        for kc in range(8):
                            self.mm(p_[:], hT[hs][:, kc, b * 128:(b + 1) * 128], w[:, kc, 1024 + n * 512:1024 + (n + 1) * 512],
                                    kc == 0, kc == 7, ["A_w", "A_hT%d" % hs], [pk])
                        self.copy("dve" if n % 2 else "act", kvf[ks][:, n * 512:(n + 1) * 512], p_[:], [pk], ["A_kvf%d" % ks])
                    self.copy("pool", vb[ks][:], kvf[ks][:, 1024:2048], ["A_kvf%d" % ks], ["A_vb%d" % ks])
                    self.st(O["dk"][r0:r0 + 128, :], kvf[ks][:, 0:1024], ["A_kvf%d" % ks], [], final=True, semkey="A_sto%d" % ks)
                    self.st(O["dv"][r0:r0 + 128, :], kvf[ks][:, 1024:2048], ["A_kvf%d" % ks], [], final=True, semkey="A_sto%d" % ks)
                    if t0 < NP:
                        self.st(S["vl"][r0 // 128], vb[ks][:], ["A_vb%d" % ks], ["d_vl"], semkey="A_stv%d" % ks)
                    else:
                        self.st(S["vs"], vb[ks][:], ["A_vb%d" % ks], ["d_vs"], semkey="A_stv%d" % ks)

    def phase_attn(self, layer):
        nc, P, I, O, S = self.nc, self.P, self.I, self.O, self.S
        NB, NP, NBLKP, NKB = self.NB, self.NP, self.NBLKP, self.NKB
        diff = layer == 0
        NH = 8 if diff else 16
        KR = 128 if diff else 96
        VW = 128 if diff else 65
        nS = 2 if diff else 1
        scale1 = (64 + 32) ** -0.5
        kT_g, v_g = (S["kTg"], S["vg"]) if diff else (S["kaT"], S["v1"])
        kT_l, v_l = (S["kTl"], S["vl"]) if diff else (S["kaTl"], S["v1l"])
        qT_d = S["qT"] if diff else S["qaT"]
        oT_d = S["oT"] if diff else S["oT1"]
        pre = "B%d_" % layer
        with contextlib.ExitStack() as st:
            sb = lambda n, s, d=F32: st.enter_context(nc.sbuf_tensor(pre + n, list(s), d))
            psf = lambda n: st.enter_context(nc.psum_tensor(pre + n, [128, 512], F32))
            KT = sb("KT", [KR, 4, NP], BF16)
            V = sb("V", [128, 4 * NBLKP, VW], BF16)
            QT = [sb("QT%d" % i, [KR, 512], BF16) for i in range(2)]
            KTd = [sb("KTd%d" % i, [KR, 512], BF16) for i in range(2)]
            Vd = [sb("Vd%d" % i, [128, 4, VW], BF16) for i in range(2)]
            ctj = [sb("ct%d" % i, [128, NKB]) for i in range(2)]
            Dn = sb("Dn", [128, 4, 512])
            tmp = [sb("tmp%d" % i, [128, 512]) for i in range(4)]
            Pt = [sb("P%d" % i, [128, 512], BF16) for i in range(4)]
            Lsb = sb("Lsb", [128, 512]); Bs = [sb("Bs%d" % i, [128, 512]) for i in range(2)]
            Pacc = [sb("Pacc%d" % i, [128, 512]) for i in range(2)] if diff else None
            oa = sb("oa", [128, 512]); ob = sb("ob", [128, 512]); osq = sb("osq", [128, 512]); rsd = sb("rsd", [128, 512])
            oTt = [sb("oTt%d" % i, [128, 512], BF16) for i in range(2)]
            Sps = [psf("S%d" % i) for i in range(4)]
            Ops = [psf("O%d" % i) for i in range(2)]
            Lps = psf("L")
            if diff:
                T0 = sb("T0", [128, 512]); Th = sb("Th", [128, 512]); gs = sb("gs", [128, 1])
                self.ld(T0[:], I["T0"], [], [pre + "T0"])
                self.ld(Dn[:], I["Dneg"], [], [pre + "Dn"])
                self.ld(gs[:], I["gsub_col"], [], [pre + "gs"])
                self.ts("dve", gs[:], gs[:], 1.0 - LAM_INIT0, None, ALU.mult, None, [pre + "gs"], [pre + "gs"])
            else:
                self.ld(Dn[:], I["Dmask"], [], [pre + "Dn"])
                self.memset("pool", V[:, :, 64:65], 1.0, [pre + "V"])
                for i in range(2):
                    self.memset("pool", Vd[i][:, :, 64:65], 1.0, [pre + "Vd%d" % i])
            si = 0
            it = 0
            for h in range(NH):
                for rk in range(4):
                    if diff:
                        for c in range(self.NCW):
                            self.ld(KT[:, rk, c * self.CW:(c + 1) * self.CW], kT_g[h][c][rk * 128:(rk + 1) * 128, :], ["d_kTg"], [pre + "KT"], semkey=pre + "ldK")
                        for blk in range(NBLKP):
                            self.ld(V[:, rk * NBLKP + blk, :], v_g[blk][rk * 128:(rk + 1) * 128, h * 128:(h + 1) * 128],
                                    ["d_vg"], [pre + "V"], semkey=pre + "ldV")
                    else:
                        self.ld(KT[:, rk, :], kT_g[h, :, rk * NP:(rk + 1) * NP], ["d_kaT"], [pre + "KT"], semkey=pre + "ldK")
                        self.ld(V[:, rk * NBLKP:(rk + 1) * NBLKP, 0:64],
                                v_g[rk * NP:(rk + 1) * NP, h * 64:(h + 1) * 64].rearrange("(b p) e -> p b e", p=128),
                                ["d_v1"], [pre + "V"], semkey=pre + "ldV")
                if diff:
                    self.ts("pool", Th[:], T0[:], SLOPES[h], None, ALU.mult, None, [pre + "T0"], [pre + "Th"])
                for J in range(NB):
                    js = it % 2; it += 1
                    q0 = J * 512
                    qk, kdk, vdk, ck = pre + "QT%d" % js, pre + "KTd%d" % js, pre + "Vd%d" % js, pre + "ct%d" % js
                    self.ld(QT[js][:], qT_d[h, 0:KR, q0:q0 + 512], ["d_qT"], [qk])
                    if diff:
                        self.ld(KTd[js][:], kT_l[h][q0 // self.CW][:, q0 % self.CW:q0 % self.CW + 512], ["d_kTl"], [kdk])
                        for i_ in range(4):
                            self.ld(Vd[js][:, i_, :], v_l[J * 4 + i_][:, h * 128:(h + 1) * 128], ["d_vl"], [vdk])
                        self.ld(ctj[js][:], I["ctab0"][:, (h * NB + J) * NKB:(h * NB + J + 1) * NKB], [], [ck])
                    else:
                        self.ld(KTd[js][:], kT_l[h, 0:KR, q0:q0 + 512], ["d_kTl"], [kdk])
                        self.ld(Vd[js][:, :, 0:64], v_l[q0:q0 + 512, h * 64:(h + 1) * 64].rearrange("(b p) e -> p b e", p=128),
                                ["d_vl"], [vdk])
                        self.ld(ctj[js][:], I["mtab"][:, J * NKB:(J + 1) * NKB], [], [ck])
                    blocks = []
                    for rk in range(4):
                        for Jk in range(J + 1):
                            for i in range(4):
                                blocks.append(("far", rk, Jk, i))
                    for i in range(4):
                        blocks.append(("diag", 0, 0, i))
                    nblocks = len(blocks)
                    units = [(bi_, blk_, s_) for bi_, blk_ in enumerate(blocks) for s_ in range(nS)]
                    si_base = si
                    si += len(units)

                    def unit_info(u):
                        bi_, (kind, rk, Jk, i), s = units[u]
                        sl = (si_base + u) % 4
                        if kind == "far":
                            kbi = rk * NBLKP + Jk * 4 + i
                            kcol = Jk * 512 + i * 128
                            kfn = lambda lo, hi: KT[lo:hi, rk, kcol:kcol + 128]
                            vap = V[:, kbi, :]
                            krd, vrd = pre + "KT", pre + "V"
                        else:
                            kbi = 0
                            kfn = lambda lo, hi: KTd[js][lo:hi, i * 128:(i + 1) * 128]
                            vap = Vd[js][:, i, :]
                            krd, vrd = kdk, vdk
                        if diff:
                            lo, hi = s * 64, (s + 1) * 64
                        else:
                            lo, hi = 0, 96
                        return bi_, kind, i, s, sl, kbi, kfn, vap, krd, vrd, lo, hi

                    def stage1(u):
                        bi_, kind, i, s, sl, kbi, kfn, vap, krd, vrd, lo, hi = unit_info(u)
                        self.mm(Sps[sl][:], kfn(lo, hi), QT[js][lo:hi, :], True, True, [krd, qk], [pre + "S%d" % sl])

                    def stage2(u):
                        bi_, kind, i, s, sl, kbi, kfn, vap, krd, vrd, lo, hi = unit_info(u)
                        first, last = bi_ == 0, bi_ == nblocks - 1
                        sp_, sk = Sps[sl], pre + "S%d" % sl
                        tp_, tk = tmp[sl], pre + "tmp%d" % sl
                        pp_, pk = Pt[sl], pre + "P%d" % sl
                        if diff:
                            if kind == "far":
                                self.stt("dve", tp_[:], sp_[:], ctj[js][:, kbi:kbi + 1], Th[:], ALU.add, ALU.add,
                                         [sk, ck, pre + "Th"], [tk])
                            else:
                                self.stt("dve", tp_[:], Dn[:, i, :], SLOPES[h], sp_[:], ALU.mult, ALU.add,
                                         [sk, pre + "Dn"], [tk])
                            self.act(pp_[:], tp_[:], AF.Exp, [tk], [pk])
                        else:
                            if kind == "far":
                                self.act(pp_[:], sp_[:], AF.Exp, [sk, ck], [pk], bias=ctj[js][:, kbi:kbi + 1], scale=scale1)
                            else:
                                self.stt("dve", tp_[:], sp_[:], scale1, Dn[:, i, :], ALU.mult, ALU.add,
                                         [sk, pre + "Dn"], [tk])
                                self.act(pp_[:], tp_[:], AF.Exp, [tk], [pk])
                        self.mm(Ops[s][0:VW, :], vap, pp_[:], first, last, [vrd, pk], [pre + "O%d" % s])
                        if diff:
                            if first:
                                self.copy("pool", Pacc[s][:], pp_[:], [pk], [pre + "Pacc%d" % s])
                            else:
                                self.tt("pool", Pacc[s][:], Pacc[s][:], pp_[:], ALU.add, [pk, pre + "Pacc%d" % s], [pre + "Pacc%d" % s])

                    LA = 3
                    for u in range(len(units) + LA):
                        if u < len(units):
                            stage1(u)
                        if u - LA >= 0:
                            stage2(u - LA)
                    ot = oTt[js]; otk = pre + "oTt%d" % js
                    if diff:
                        for s_ in range(2):
                            self.mm(Lps[32 * s_:32 * s_ + 1, :], self.ones_f[:, 0:1], Pacc[s_][:], True, True,
                                    ["ones_f", pre + "Pacc%d" % s_], [pre + "L"])
                        self.copy("dve", Lsb[0:64, :], Lps[0:64, :], [pre + "L"], [pre + "Lsb"])
                        self.P.op("dve", lambda e: e.reciprocal(Lsb[0:1, :], Lsb[0:1, :]), [pre + "Lsb"], [pre + "Lsb"])
                        self.P.op("dve", lambda e: e.reciprocal(Lsb[32:33, :], Lsb[32:33, :]), [pre + "Lsb"], [pre + "Lsb"])
                        self.ts("dve", Lsb[32:33, :], Lsb[32:33, :], self.nlam[32:33, 0:1], None, ALU.mult, None,
                                [pre + "Lsb", "nlam"], [pre + "Lsb"])
                        for s in range(2):
                            self.mm(Sps[s][:], self.ones_f[32 * s:32 * s + 1, :], Lsb[32 * s:32 * s + 1, :], True, True,
                                    ["ones_f", pre + "Lsb"], [pre + "S%d" % s])
                            self.copy("act", Bs[s][:], Sps[s][:], [pre + "S%d" % s], [pre + "Bs%d" % s])
                        self.tt("dve", oa[:], Ops[0][:], Bs[0][:], ALU.mult, [pre + "O0", pre + "Bs0"], [pre + "oa"])
                        self.tt("dve", ob[:], Ops[1][:], Bs[1][:], ALU.mult, [pre + "O1", pre + "Bs1"], [pre + "ob"])
                        self.tt("pool", oa[:], oa[:], ob[:], ALU.add, [pre + "oa", pre + "ob"], [pre + "oa"])
                        self.act(osq[:], oa[:], AF.Square, [pre + "oa"], [pre + "osq"])
                        self.mm(Sps[2][:], self.ones_f[:], osq[:], True, True, ["ones_f", pre + "osq"], [pre + "S2"])
                        self.act(rsd[:], Sps[2][:], AF.Ln, [pre + "S2"], [pre + "rsd"], scale=1.0 / 128, bias=EPS)
                        self.act(rsd[:], rsd[:], AF.Exp, [pre + "rsd"], [pre + "rsd"], scale=-0.5)
                        self.stt("dve", ot[:], oa[:], gs[:, 0:1], rsd[:], ALU.mult, ALU.mult,
                                 [pre + "oa", pre + "gs", pre + "rsd"], [otk])
                        self.st(oT_d[h, :, q0:q0 + 512], ot[:], [otk], ["d_oT"], semkey=pre + "sto%d" % js)
                    else:
                        self.copy("dve", Lsb[64:65, :], Ops[0][64:65, :], [pre + "O0"], [pre + "Lsb"])
                        self.P.op("dve", lambda e: e.reciprocal(Lsb[64:65, :], Lsb[64:65, :]), [pre + "Lsb"], [pre + "Lsb"])
                        self.mm(Sps[0][0:64, :], self.ones_f[64:65, 0:64], Lsb[64:65, :], True, True,
                                ["ones_f", pre + "Lsb"], [pre + "S0"])
                        self.copy("act", Bs[0][0:64, :], Sps[0][0:64, :], [pre + "S0"], [pre + "Bs0"])
                        self.tt("dve", ot[0:64, :], Ops[0][0:64, :], Bs[0][0:64, :], ALU.mult, [pre + "O0", pre + "Bs0"], [otk])
                        self.st(oT_d[h, :, q0:q0 + 512], ot[0:64, :], [otk], ["d_oT1"], semkey=pre + "sto%d" % js)

    def phase_sample_attn(self, layer):
        nc, P, I, O, S = self.nc, self.P, self.I, self.O, self.S
        NP, PB, PAST = self.NP, self.PB, self.PAST
        diff = layer == 0
        pre = "Bs%d_" % layer
        NH = 8 if diff else 16
        scale1 = (64 + 32) ** -0.5
        with contextlib.ExitStack() as st:
            sb = lambda n, s, d=F32: st.enter_context(nc.sbuf_tensor(pre + n, list(s), d))
            psf = lambda n: st.enter_context(nc.psum_tensor(pre + n, [128, 512], F32))
            pst = st.enter_context(nc.psum_tensor(pre + "pst", [128, 1024], BF16))
            Sp = [psf("S%d" % i) for i in range(2)]
            Sn = psf("Sn"); Op_ = [psf("O%d" % i) for i in range(2)]
            zt = sb("zt", [128, 128], BF16)
            self.memset("pool", zt[:], 0.0, [pre + "zt"])
            if diff:
                for h in range(8):
                    self.st(S["oT"][h, :, NP:NP + 128], zt[:], [pre + "zt"], ["d_oT"], semkey=pre + "z")
                Kc = sb("Kc", [128, PB, 128]); Kcb = sb("Kcb", [128, PB, 128], BF16); KcT = sb("KcT", [128, PAST], BF16)
                Vc = sb("Vc", [128, PB, 128]); Vcb = sb("Vcb", [128, PB, 128], BF16)
                qsT = sb("qsT", [128, 8, 128], BF16); ksT = sb("ksT", [128, 8, 128], BF16)
                vsl = [sb("vs%d" % i, [16, D], BF16) for i in range(2)]
                Dns = sb("Dns", [128, PB * 16]); Dnn = sb("Dnn", [16, 16])
                tm = [sb("tm%d" % i, [128, 512]) for i in range(2)]; Ps = [sb("Ps%d" % i, [128, 512], BF16) for i in range(2)]
                tn = [sb("tn%d" % i, [16, 16]) for i in range(2)]; Pn = [sb("Pn%d" % i, [16, 16], BF16) for i in range(2)]
                r = sb("r", [16, 2]); o = sb("o", [16, 128]); sq = sb("sq", [16, 128]); ss = sb("ss", [16, 1]); rs = sb("rs", [16, 1])
                gsr = sb("gsr", [128, 128]); ob = sb("ob", [16, 128], BF16); oTs = sb("oTs", [128, 16], BF16)
                self.ld(qsT[:], S["qT"][:, :, NP:NP + 128].rearrange("h p t -> p h t"), ["d_qT"], [pre + "qsT"])
                self.ld(ksT[:], S["ksT"].rearrange("h p t -> p h t"), ["d_ksT"], [pre + "ksT"])
                for i in range(2):
                    self.ld(vsl[i][:], S["vs"][i * 32:i * 32 + 16, :], ["d_vs"], [pre + "vs"])
                self.ld(Dns[:], I["Dnegs"], [], [pre + "Dns"])
                self.ld(Dnn[:], I["Dnegn"], [], [pre + "Dnn"])
                self.ld(gsr[:], I["gsub_row"], [], [pre + "gsr"])
                for s in range(2):
                    c0 = s * 32
                    for h in range(8):
                        self.ld(Kc[:], I["cdk"][s, :, h * 128:(h + 1) * 128].rearrange("(b p) e -> p b e", p=128), [], [pre + "Kc"])
                        self.ld(Vc[:], I["cdv"][s, :, h * 128:(h + 1) * 128].rearrange("(b p) e -> p b e", p=128), [], [pre + "Vc"])
                        self.copy("pool", Kcb[:], Kc[:], [pre + "Kc"], [pre + "Kcb"])
                        self.copy("pool", Vcb[:], Vc[:], [pre + "Vc"], [pre + "Vcb"])
                        for g8 in range(PB // 8):
                            for j in range(8):
                                kb = g8 * 8 + j
                                self.tr(pst[:, j * 128:(j + 1) * 128], Kcb[:, kb, :], self.ident[:], [pre + "Kcb", "ident"], [pre + "pst"])
                            self.copy("act", KcT[:, g8 * 1024:(g8 + 1) * 1024], pst[:], [pre + "pst"], [pre + "KcT"])
                        for sidx in range(2):
                            lo, hi = sidx * 64, (sidx + 1) * 64
                            for kb in range(PB):
                                self.mm(Sp[sidx][:, kb * 16:(kb + 1) * 16], KcT[lo:hi, kb * 128:(kb + 1) * 128],
                                        qsT[lo:hi, h, c0:c0 + 16], True, True, [pre + "KcT", pre + "qsT"], [pre + "S%d" % sidx])
                            self.stt("dve", tm[sidx][:, 0:PB * 16], Dns[:], SLOPES[h], Sp[sidx][:, 0:PB * 16], ALU.mult, ALU.add,
                                     [pre + "Dns", pre + "S%d" % sidx], [pre + "tm%d" % sidx])
                            self.act(Ps[sidx][:, 0:PB * 16], tm[sidx][:, 0:PB * 16], AF.Exp, [pre + "tm%d" % sidx], [pre + "Ps%d" % sidx])
                            self.mm(Sn[0:16, sidx * 16:(sidx + 1) * 16], ksT[lo:hi, h, c0:c0 + 16], qsT[lo:hi, h, c0:c0 + 16],
                                    True, True, [pre + "ksT", pre + "qsT"], [pre + "Sn"])
                            self.stt("dve", tn[sidx][:], Dnn[:], SLOPES[h], Sn[0:16, sidx * 16:(sidx + 1) * 16], ALU.mult, ALU.add,
                                     [pre + "Dnn", pre + "Sn"], [pre + "tn%d" % sidx])
                            self.act(Pn[sidx][:], tn[sidx][:], AF.Exp, [pre + "tn%d" % sidx], [pre + "Pn%d" % sidx])
                            ok = pre + "O%d" % sidx
                            for kb in range(PB):
                                self.mm(Op_[sidx][0:16, 0:128], Ps[sidx][:, kb * 16:(kb + 1) * 16], Vcb[:, kb, :], kb == 0, False,
                                        [pre + "Ps%d" % sidx, pre + "Vcb"], [ok])
                            self.mm(Op_[sidx][0:16, 0:128], Pn[sidx][:], vsl[s][:, h * 128:(h + 1) * 128], False, True,
                                    [pre + "Pn%d" % sidx, pre + "vs"], [ok])
                            for kb in range(PB):
                                self.mm(Op_[sidx][0:16, 128:129], Ps[sidx][:, kb * 16:(kb + 1) * 16], self.ones_b[:, 0:1], kb == 0, False,
                                        [pre + "Ps%d" % sidx, "ones_b"], [ok])
                            self.mm(Op_[sidx][0:16, 128:129], Pn[sidx][:], self.ones_b[0:16, 0:1], False, True,
                                    [pre + "Pn%d" % sidx, "ones_b"], [ok])
                        self.P.op("dve", lambda e: e.reciprocal(r[:, 0:1], Op_[0][0:16, 128:129]), [pre + "O0"], [pre + "r"])
                        self.P.op("dve", lambda e: e.reciprocal(r[:, 1:2], Op_[1][0:16, 128:129]), [pre + "O1", pre + "r"], [pre + "r"])
                        self.ts("dve", r[:, 1:2], r[:, 1:2], self.nlam[0:16, 0:1], None, ALU.mult, None, [pre + "r", "nlam"], [pre + "r"])
                        self.ts("dve", o[:], Op_[0][0:16, 0:128], r[:, 0:1], None, ALU.mult, None, [pre + "O0", pre + "r"], [pre + "o"])
                        self.stt("dve", o[:], Op_[1][0:16, 0:128], r[:, 1:2], o[:], ALU.mult, ALU.add, [pre + "O1", pre + "r", pre + "o"], [pre + "o"])
                        self.rstd(o[:], 128, sq[:], ss[:], rs[:], pre + "o", pre)
                        self.ts("dve", rs[:], rs[:], 1.0 - LAM_INIT0, None, ALU.mult, None, [pre + "rs"], [pre + "rs"])
                        self.stt("dve", ob[:], o[:], rs[:, 0:1], gsr[0:16, :], ALU.mult, ALU.mult, [pre + "o", pre + "rs", pre + "gsr"], [pre + "ob"])
                        self.tr(pst[:, 0:16], ob[:], self.ident[0:16, 0:16], [pre + "ob", "ident"], [pre + "pst"])
                        self.copy("act", oTs[:], pst[:, 0:16], [pre + "pst"], [pre + "oTs"])
                        self.st(S["oT"][h, :, NP + c0:NP + c0 + 16], oTs[:], [pre + "oTs"], ["d_oT"], semkey=pre + "sto")
            else:
                for h in range(16):
                    self.st(S["oT1"][h, :, NP:NP + 128], zt[0:64, :], [pre + "zt"], ["d_oT1"], semkey=pre + "z")
                wk = sb("wk", [128, 3, 16, 96], BF16); wv = sb("wv", [128, 2, 1024], BF16)
                self.build_wk(wk, wv, pre)
                Cc = sb("Cc", [128, PB, 288]); Ccb = sb("Ccb", [128, PB, 288], BF16); CT = sb("CT", [128, 3, PAST], BF16)
                Vcb = sb("Vcb", [128, PB, 16, 65], BF16)
                qsT = sb("qsT", [96, 16, 128], BF16); ksT = sb("ksT", [96, 16, 128], BF16)
                KaTh = sb("KaTh", [96, PAST], BF16)
                Ps = sb("Ps", [128, 512], BF16); Pn = sb("Pn", [16, 16], BF16)
                r = sb("r", [16, 1]); ob = sb("ob", [16, 64], BF16); oTs = sb("oTs", [64, 16], BF16)
                self.ld(qsT[:], S["qaT"][:, :, NP:NP + 128].rearrange("h p t -> p h t"), ["d_qaT"], [pre + "qsT"])
                self.ld(ksT[:], S["kaTs"].rearrange("h p t -> p h t"), ["d_kaTs"], [pre + "ksT"])
                self.memset("pool", Vcb[:, :, :, 64:65], 1.0, [pre + "Vcb"])
                vsl = [sb("vs%d" % i, [16, D], BF16) for i in range(2)]
                vsx = [sb("vsx%d" % i, [16, 16, 65], BF16) for i in range(2)]
                for i in range(2):
                    self.ld(vsl[i][:], S["v1s"][i * 32:i * 32 + 16, :], ["d_v1s"], [pre + "vs"])
                    self.memset("pool", vsx[i][:, :, 64:65], 1.0, [pre + "vsx"])
                    self.copy("pool", vsx[i][:, :, 0:64], vsl[i][:].rearrange("p (h e) -> p h e", h=16), [pre + "vs", pre + "vsx"], [pre + "vsx"])
                for s in range(2):
                    c0 = s * 32
                    self.ld(Cc[:, :, 0:256], I["cckv"][s].rearrange("(b p) e -> p b e", p=128), [], [pre + "Cc"])
                    self.ld(Cc[:, :, 256:288], I["ckpe"][s].rearrange("(b p) e -> p b e", p=128), [], [pre + "Cc"])
                    self.copy("pool", Ccb[:], Cc[:], [pre + "Cc"], [pre + "Ccb"])
                    for ch in range(3):
                        wd = 128 if ch < 2 else 32
                        for g8 in range(PB // 8):
                            for j in range(8):
                                kb = g8 * 8 + j
                                self.tr(pst[0:wd, j * 128:(j + 1) * 128], Ccb[:, kb, ch * 128:ch * 128 + wd], self.ident[:],
                                        [pre + "Ccb", "ident"], [pre + "pst"])
                            self.copy("act", CT[0:wd, ch, g8 * 1024:(g8 + 1) * 1024], pst[0:wd, :], [pre + "pst"], [pre + "CT"])
                    for kb in range(PB):
                        for n in range(2):
                            pp = Sp[n]
                            for kc in range(2):
                                self.mm(pp[:], CT[:, kc, kb * 128:(kb + 1) * 128], wv[:, kc, n * 512:(n + 1) * 512], kc == 0, kc == 1,
                                        [pre + "CT", pre + "wv"], [pre + "S%d" % n])
                            self.copy("dve" if n else "act", Vcb[:, kb, n * 8:(n + 1) * 8, 0:64],
                                      pp[:].rearrange("p (h e) -> p h e", h=8), [pre + "S%d" % n], [pre + "Vcb"])
                    for h in range(16):
                        for kt in range(PAST // 512):
                            for ch in range(3):
                                wd = 128 if ch < 2 else 32
                                self.mm(Sn[0:96, :], wk[0:wd, ch, h, :], CT[0:wd, ch, kt * 512:(kt + 1) * 512], ch == 0, ch == 2,
                                        [pre + "wk", pre + "CT"], [pre + "Sn"])
                            self.copy("act", KaTh[:, kt * 512:(kt + 1) * 512], Sn[0:96, :], [pre + "Sn"], [pre + "KaTh"])
                        for kb in range(PB):
                            self.mm(Sp[0][:, kb * 16:(kb + 1) * 16], KaTh[:, kb * 128:(kb + 1) * 128], qsT[:, h, c0:c0 + 16], True, True,
                                    [pre + "KaTh", pre + "qsT"], [pre + "S0"])
                        self.act(Ps[:, 0:PB * 16], Sp[0][:, 0:PB * 16], AF.Exp, [pre + "S0"], [pre + "Ps"], scale=scale1)
                        self.mm(Sp[1][0:16, 0:16], ksT[:, h, c0:c0 + 16], qsT[:, h, c0:c0 + 16], True, True,
                                [pre + "ksT", pre + "qsT"], [pre + "S1"])
                        self.act(Pn[:], Sp[1][0:16, 0:16], AF.Exp, [pre + "S1"], [pre + "Pn"], scale=scale1)
                        for kb in range(PB):
                            self.mm(Op_[0][0:16, 0:65], Ps[:, kb * 16:(kb + 1) * 16], Vcb[:, kb, h, :], kb == 0, False,
                                    [pre + "Ps", pre + "Vcb"], [pre + "O0"])
                        self.mm(Op_[0][0:16, 0:65], Pn[:], vsx[s][:, h, :], False, True, [pre + "Pn", pre + "vsx"], [pre + "O0"])
                        self.P.op("dve", lambda e: e.reciprocal(r[:], Op_[0][0:16, 64:65]), [pre + "O0"], [pre + "r"])
                        self.ts("dve", ob[:], Op_[0][0:16, 0:64], r[:, 0:1], None, ALU.mult, None, [pre + "O0", pre + "r"], [pre + "ob"])
                        self.tr(pst[0:64, 0:16], ob[:], self.ident[0:16, 0:16], [pre + "ob", "ident"], [pre + "pst"])
                        self.copy("act", oTs[:], pst[0:64, 0:16], [pre + "pst"], [pre + "oTs"])
                        self.st(S["oT1"][h, :, NP + c0:NP + c0 + 16], oTs[:], [pre + "oTs"], ["d_oT1"], semkey=pre + "sto")

    def build_wk(self, wk, wv, pre):
        I = self.I
        self.memset("pool", wk[:], 0.0, [pre + "wk"])
        w = I["w_ukv"].rearrange("(kc p) (h x) -> p kc h x", p=128, h=16)
        for kc in range(2):
            self.ld(wk[:, kc, :, 0:64], w[:, kc, :, 0:64], [], [pre + "wk"], eng="pool")
            self.ld(wv[:, kc, :].rearrange("p (h e) -> p h e", h=16), w[:, kc, :, 64:128], [], [pre + "wv"], eng="pool")
        for h in range(16):
            self.copy("dve", wk[0:32, 2, h, 64:96], self.ident[0:32, 0:32], ["ident", pre + "wk"], [pre + "wk"])

    def phase_C1(self):
        nc, P, I, O, S = self.nc, self.P, self.I, self.O, self.S
        pre = "C1_"
        with contextlib.ExitStack() as st:
            sb = lambda n, s, d=F32: st.enter_context(nc.sbuf_tensor(pre + n, list(s), d))
            psf = lambda n: st.enter_context(nc.psum_tensor(pre + n, [128, 512], F32))
            wo = sb("wo", [128, 8, D], BF16); wgu = sb("wgu", [128, 8, 2 * FD], BF16); gb = sb("gb", [128, D])
            xt = [sb("xt%d" % i, [128, D]) for i in range(2)]
            T = {"sq": sb("sq", [128, D], BF16), "ss": sb("ss", [128, 1]), "rs": sb("rs", [128, 1]),
                 "hb": sb("hb", [128, D], BF16), "pst": st.enter_context(nc.psum_tensor(pre + "pst", [128, 1024], BF16))}
            oTt = [sb("oTt%d" % i, [128, 8, 512], BF16) for i in range(2)]
            hT = [sb("hT%d" % i, [128, 8, 512], BF16) for i in range(2)]
            sg = [sb("sg%d" % i, [128, 512]) for i in range(2)]
            aT = [sb("aT%d" % i, [128, 22, 512], BF16) for i in range(1)]
            ps = [psf("ps%d" % i) for i in range(6)]
            pi = 0
            self.ld(wo[:], I["w_o0"].rearrange("(kc p) n -> p kc n", p=128), [], [pre + "wo"], eng="pool")
            self.ld(wgu[:], I["w_gu0"].rearrange("(kc p) n -> p kc n", p=128), [], [pre + "wgu"], eng="pool")
            self.ld(gb[:], I["g_ffn0"], [], ["gains"])
            bi = 0
            for ti, (t0, nt) in enumerate(self.tiles):
                hs = ti % 2
                self.ld(oTt[hs][:, :, 0:nt], S["oT"][:, :, t0:t0 + nt].rearrange("h p t -> p h t"), ["d_oT"], [pre + "oTt%d" % hs])
                for b in range(nt // 128):
                    xs = bi % 2; bi += 1
                    r0 = t0 + b * 128
                    xk = pre + "xt%d" % xs
                    self.ld(xt[xs][:], I["xin"][r0:r0 + 128, :], [], [xk])
                    for n in range(2):
                        p_ = ps[pi % 6]; pk = pre + "ps%d" % (pi % 6); pi += 1
                        for h in range(8):
                            self.mm(p_[:], oTt[hs][:, h, b * 128:(b + 1) * 128], wo[:, h, n * 512:(n + 1) * 512], h == 0, h == 7,
                                    [pre + "oTt%d" % hs, pre + "wo"], [pk])
                        self.tt("dve", xt[xs][:, n * 512:(n + 1) * 512], xt[xs][:, n * 512:(n + 1) * 512], p_[:], ALU.add, [xk, pk], [xk])
                    self.st(S["x1"][r0:r0 + 128, :], xt[xs][:], [xk], ["d_x1"], semkey=pre + "stx%d" % xs)
                    self.norm_T(T, xt[xs][:], xk, gb[:], D, None, None, pre)
                    self.copy("act", hT[hs][:, :, b * 128:(b + 1) * 128], T["pst"][:].rearrange("p (k c) -> p k c", k=8),
                              [pre + "pst"], [pre + "hT%d" % hs])
                for j in range(22):
                    pg = ps[pi % 6]; pgk = pre + "ps%d" % (pi % 6); pi += 1
                    pu = ps[pi % 6]; puk = pre + "ps%d" % (pi % 6); pi += 1
                    for kc in range(8):
                        self.mm(pg[:, 0:nt], wgu[:, kc, j * 128:(j + 1) * 128], hT[hs][:, kc, 0:nt], kc == 0, kc == 7,
                                [pre + "wgu", pre + "hT%d" % hs], [pgk])
                    for kc in range(8):
                        self.mm(pu[:, 0:nt], wgu[:, kc, FD + j * 128:FD + (j + 1) * 128], hT[hs][:, kc, 0:nt], kc == 0, kc == 7,
                                [pre + "wgu", pre + "hT%d" % hs], [puk])
                    sgs = j % 2
                    self.act(sg[sgs][:, 0:nt], pg[:, 0:nt], AF.Silu, [pgk], [pre + "sg%d" % sgs])
                    self.tt("dve", aT[0][:, j, 0:nt], sg[sgs][:, 0:nt], pu[:, 0:nt], ALU.mult, [pre + "sg%d" % sgs, puk], [pre + "aT0"])
                self.st(S["actT"][:, :, t0:t0 + nt].rearrange("j p t -> p j t"), aT[0][:, :, 0:nt], [pre + "aT0"], ["d_actT"], semkey=pre + "sta")

    def phase_C2(self):
        nc, P, I, O, S = self.nc, self.P, self.I, self.O, self.S
        NP = self.NP
        pre = "C2_"
        with contextlib.ExitStack() as st:
            sb = lambda n, s, d=F32: st.enter_context(nc.sbuf_tensor(pre + n, list(s), d))
            psf = lambda n: st.enter_context(nc.psum_tensor(pre + n, [128, 512], F32))
            wdn = sb("wdn", [128, 22, D], BF16); wa = sb("wa", [128, 8, 1056], BF16)
            wuq = sb("wuq", [128, 6, 1536], BF16); wuqs = sb("wuqs", [128, 6, 1536], BF16)
            gb = sb("gb", [128, D]); gq = sb("gq", [128, 768]); gkv = sb("gkv", [128, 256])
            csf = sb("csf", [96, 2, 512]); cst = sb("cst", [128, 32])
            xt = [sb("xt%d" % i, [128, D]) for i in range(2)]
            T = {"sq": sb("sq", [128, D], BF16), "ss": sb("ss", [128, 1]), "rs": sb("rs", [128, 1]),
                 "hb": sb("hb", [128, D], BF16), "pst": st.enter_context(nc.psum_tensor(pre + "pst", [128, 1024], BF16))}
            aT = [sb("aT%d" % i, [128, 22, 512], BF16) for i in range(1)]
            hT = sb("hT", [128, 8, 128], BF16)
            af = sb("af", [128, 1056]); ckv = sb("ckv", [128, 256]); kpe = sb("kpe", [128, 32]); rt = sb("rt", [128, 64])
            cpb = sb("cpb", [128, 288], BF16)
            cqT = [sb("cqT%d" % i, [128, 6, 512], BF16) for i in range(2)]
            cpT = [sb("cpT%d" % i, [128, 3, 512], BF16) for i in range(2)]
            qa = [sb("qa%d" % i, [96, 512], BF16) for i in range(2)]
            t1 = sb("t1", [96, 512]); t2 = sb("t2", [96, 512])
            ps = [psf("ps%d" % i) for i in range(6)]
            pi = 0
            self.ld(wdn[:], I["w_dn0"].rearrange("(j p) n -> p j n", p=128), [], [pre + "wdn"], eng="pool")
            self.ld(wa[:], I["w_a"].rearrange("(kc p) n -> p kc n", p=128), [], [pre + "wa"], eng="pool")
            self.ld(wuq[:], I["w_uq"].rearrange("(kc p) n -> p kc n", p=128), [], [pre + "wuq"], eng="pool")
            self.ld(wuqs[:], I["w_uqs"].rearrange("(kc p) n -> p kc n", p=128), [], [pre + "wuqs"], eng="pool")
            self.ld(gb[:], I["g_mix1"], [], ["gains"])
            self.ld(gq[:], I["g_q"], [], [pre + "gq"])
            self.ld(gkv[:], I["g_kv"], [], [pre + "gkv"])
            bi = 0
            qi = 0
            for ti, (t0, nt) in enumerate(self.tiles):
                hs = ti % 2
                for c in range(2):
                    self.ld(csf[64:96, c, 0:nt], I["cs_fm"][c, :, t0:t0 + nt], [], [pre + "csf"])
                self.ld(aT[0][:, :, 0:nt], S["actT"][:, :, t0:t0 + nt].rearrange("j p t -> p j t"), ["d_actT"], [pre + "aT0"])
                for b in range(nt // 128):
                    xs = bi % 2; bi += 1
                    r0 = t0 + b * 128
                    xk = pre + "xt%d" % xs
                    self.ld(xt[xs][:], S["x1"][r0:r0 + 128, :], ["d_x1"], [xk])
                    self.ld(cst[:], I["cs_tm"][r0:r0 + 128, :], [], [pre + "cst"])
                    for n in range(2):
                        p_ = ps[pi % 6]; pk = pre + "ps%d" % (pi % 6); pi += 1
                        for j in range(22):
                            self.mm(p_[:], aT[0][:, j, b * 128:(b + 1) * 128], wdn[:, j, n * 512:(n + 1) * 512], j == 0, j == 21,
                                    [pre + "aT0", pre + "wdn"], [pk])
                        self.tt("dve", xt[xs][:, n * 512:(n + 1) * 512], xt[xs][:, n * 512:(n + 1) * 512], p_[:], ALU.add, [xk, pk], [xk])
                    self.st(S["x2"][r0:r0 + 128, :], xt[xs][:], [xk], ["d_x2"], semkey=pre + "stx%d" % xs)
                    self.norm_T(T, xt[xs][:], xk, gb[:], D, None, None, pre)
                    self.copy("act", hT[:], T["pst"][:].rearrange("p (k c) -> p k c", k=8), [pre + "pst"], [pre + "hT"])
                    for (c0, cw) in ((0, 512), (512, 512), (1024, 32)):
                        p_ = ps[pi % 6]; pk = pre + "ps%d" % (pi % 6); pi += 1
                        for kc in range(8):
                            self.mm(p_[:, 0:cw], hT[:, kc, :], wa[:, kc, c0:c0 + cw], kc == 0, kc == 7, [pre + "hT", pre + "wa"], [pk])
                        self.copy("act" if c0 == 512 else "dve", af[:, c0:c0 + cw], p_[:, 0:cw], [pk], [pre + "af"])
                    self.norm_T(T, af[:, 0:768], pre + "af", gq[:], 768, None, None, pre)
                    self.copy("act", cqT[hs][:, :, b * 128:(b + 1) * 128], T["pst"][:, 0:768].rearrange("p (k c) -> p k c", k=6),
                              [pre + "pst"], [pre + "cqT%d" % hs, pre + "pst"])
                    self.rstd(af[:, 768:1024], 256, T["sq"][:, 0:256], T["ss"][:, 0:1], T["rs"][:, 0:1], pre + "af", pre)
                    self.stt("dve", ckv[:], af[:, 768:1024], T["rs"][:, 0:1], gkv[:], ALU.mult, ALU.mult,
                             [pre + "af", pre + "rs", pre + "gkv"], [pre + "ckv"])
                    self.st(O["ckv"][r0:r0 + 128, :], ckv[:], [pre + "ckv"], [], final=True, semkey=pre + "stc")
                    x1_, x2_ = af[:, 1024:1040], af[:, 1040:1056]
                    co, si_ = cst[:, 0:16], cst[:, 16:32]
                    self.tt("dve", rt[:, 0:16], x1_, co, ALU.mult, [pre + "af", pre + "cst"], [pre + "rt"])
                    self.tt("dve", rt[:, 16:32], x2_, si_, ALU.mult, [pre + "af", pre + "cst", pre + "rt"], [pre + "rt"])
                    self.tt("dve", rt[:, 32:48], x2_, co, ALU.mult, [pre + "af", pre + "cst", pre + "rt"], [pre + "rt"])
                    self.tt("dve", rt[:, 48:64], x1_, si_, ALU.mult, [pre + "af", pre + "cst", pre + "rt"], [pre + "rt"])
                    self.tt("dve", kpe[:, 0:16], rt[:, 0:16], rt[:, 16:32], ALU.subtract, [pre + "rt"], [pre + "kpe"])
                    self.tt("dve", kpe[:, 16:32], rt[:, 32:48], rt[:, 48:64], ALU.add, [pre + "rt", pre + "kpe"], [pre + "kpe"])
                    self.st(O["kpe"][r0:r0 + 128, :], kpe[:], [pre + "kpe"], [], final=True, semkey=pre + "stp")
                    self.copy("pool", cpb[:, 0:256], ckv[:], [pre + "ckv"], [pre + "cpb"])
                    self.copy("pool", cpb[:, 256:288], kpe[:], [pre + "kpe", pre + "cpb"], [pre + "cpb"])
                    for ch in range(3):
                        wd = 128 if ch < 2 else 32
                        self.tr(T["pst"][0:wd, ch * 128:(ch + 1) * 128], cpb[:, ch * 128:ch * 128 + wd], self.ident[:],
                                [pre + "cpb", "ident", pre + "pst"], [pre + "pst"])
                    self.copy("act", cpT[hs][:, 0:2, b * 128:(b + 1) * 128], T["pst"][:, 0:256].rearrange("p (k c) -> p k c", k=2),
                              [pre + "pst"], [pre + "cpT%d" % hs, pre + "pst"])
                    self.copy("act", cpT[hs][0:32, 2, b * 128:(b + 1) * 128], T["pst"][0:32, 256:384],
                              [pre + "pst"], [pre + "cpT%d" % hs, pre + "pst"])
                if t0 < NP:
                    for ch in range(3):
                        wd = 128 if ch < 2 else 32
                        self.st(S["cpl"][ch][t0 // self.CW][0:wd, t0 % self.CW:t0 % self.CW + nt], cpT[hs][0:wd, ch, 0:nt], [pre + "cpT%d" % hs], ["d_cpl"], semkey=pre + "stcp%d" % hs)
                else:
                    self.st(S["cps"][0:2].rearrange("c p t -> p c t"), cpT[hs][:, 0:2, 0:nt], [pre + "cpT%d" % hs], ["d_cps"], semkey=pre + "stcp%d" % hs)
                    self.st(S["cps"][2, 0:32, :], cpT[hs][0:32, 2, 0:nt], [pre + "cpT%d" % hs], ["d_cps"], semkey=pre + "stcp%d" % hs)
                for h in range(16):
                    pa = ps[pi % 6]; pak = pre + "ps%d" % (pi % 6); pi += 1
                    pb_ = ps[pi % 6]; pbk = pre + "ps%d" % (pi % 6); pi += 1
                    for kc in range(6):
                        self.mm(pa[0:96, 0:nt], wuq[:, kc, h * 96:(h + 1) * 96], cqT[hs][:, kc, 0:nt], kc == 0, kc == 5,
                                [pre + "wuq", pre + "cqT%d" % hs], [pak])
                    for kc in range(6):
                        self.mm(pb_[0:96, 0:nt], wuqs[:, kc, h * 96:(h + 1) * 96], cqT[hs][:, kc, 0:nt], kc == 0, kc == 5,
                                [pre + "wuqs", pre + "cqT%d" % hs], [pbk])
                    qs = qi % 2; qi += 1
                    qk_ = pre + "qa%d" % qs
                    self.copy("act", qa[qs][0:64, 0:nt], pa[0:64, 0:nt], [pak], [qk_])
                    self.tt("dve", t1[64:96, 0:nt], pa[64:96, 0:nt], csf[64:96, 0, 0:nt], ALU.mult, [pak, pre + "csf"], [pre + "t1"])
                    self.tt("dve", t2[64:96, 0:nt], pb_[64:96, 0:nt], csf[64:96, 1, 0:nt], ALU.mult, [pbk, pre + "csf"], [pre + "t2"])
                    self.tt("pool", qa[qs][64:96, 0:nt], t1[64:96, 0:nt], t2[64:96, 0:nt], ALU.add, [pre + "t1", pre + "t2", qk_], [qk_])
                    self.st(S["qaT"][h, :, t0:t0 + nt], qa[qs][:, 0:nt], [qk_], ["d_qaT"], semkey=pre + "stq%d" % qs)

    def phase_D0(self):
        nc, P, I, O, S = self.nc, self.P, self.I, self.O, self.S
        NP = self.NP
        pre = "D0_"
        with contextlib.ExitStack() as st:
            sb = lambda n, s, d=F32: st.enter_context(nc.sbuf_tensor(pre + n, list(s), d))
            psf = lambda n: st.enter_context(nc.psum_tensor(pre + n, [128, 512], F32))
            wk = sb("wk", [128, 3, 16, 96], BF16); wv = sb("wv", [128, 2, 1024], BF16)
            self.build_wk(wk, wv, pre)
            cT = [sb("cT%d" % i, [128, 3, 512], BF16) for i in range(2)]
            ka = [sb("ka%d" % i, [96, 16, 512], BF16) for i in range(2)]
            vt = [sb("vt%d" % i, [128, D], BF16) for i in range(2)]
            ps = [psf("ps%d" % i) for i in range(6)]
            pi = 0
            jobs = []
            for rk in range(4):
                for t0 in range(0, NP, 512):
                    jobs.append(("g", rk, t0, 512))
            for t0 in range(0, NP, 512):
                jobs.append(("l", 0, t0, 512))
            jobs.append(("s", 0, 0, 128))
            vi = 0
            for ji, (kind, rk, t0, nt) in enumerate(jobs):
                cs = ji % 2
                ck = pre + "cT%d" % cs
                if kind == "g":
                    for ch in range(3):
                        wd = 128 if ch < 2 else 32
                        self.ld(cT[cs][0:wd, ch, :], S["cpg"][ch][t0 // self.CW][rk * 128:rk * 128 + wd, t0 % self.CW:t0 % self.CW + 512], ["d_cpg"], [ck])
                elif kind == "l":
                    for ch in range(3):
                        wd = 128 if ch < 2 else 32
                        self.ld(cT[cs][0:wd, ch, :], S["cpl"][ch][t0 // self.CW][0:wd, t0 % self.CW:t0 % self.CW + 512], ["d_cpl"], [ck])
                else:
                    self.ld(cT[cs][:, 0:2, 0:128], S["cps"][0:2].rearrange("c p t -> p c t"), ["d_cps"], [ck])
                    self.ld(cT[cs][0:32, 2, 0:128], S["cps"][2, 0:32, :], ["d_cps"], [ck])
                kk = pre + "ka%d" % cs
                for h in range(16):
                    p_ = ps[pi % 6]; pk = pre + "ps%d" % (pi % 6); pi += 1
                    for ch in range(3):
                        wd = 128 if ch < 2 else 32
                        self.mm(p_[0:96, 0:nt], wk[0:wd, ch, h, :], cT[cs][0:wd, ch, 0:nt], ch == 0, ch == 2, [pre + "wk", ck], [pk])
                    self.copy("act" if h % 2 else "dve", ka[cs][:, h, 0:nt], p_[0:96, 0:nt], [pk], [kk])
                if kind == "g":
                    self.st(S["kaT"][:, :, rk * NP + t0:rk * NP + t0 + 512].rearrange("h p t -> p h t"), ka[cs][:], [kk], ["d_kaT"], semkey=pre + "stk%d" % cs)
                elif kind == "l":
                    self.st(S["kaTl"][:, :, t0:t0 + 512].rearrange("h p t -> p h t"), ka[cs][:], [kk], ["d_kTl"], semkey=pre + "stk%d" % cs)
                else:
                    self.st(S["kaTs"].rearrange("h p t -> p h t"), ka[cs][:, :, 0:128], [kk], ["d_kaTs"], semkey=pre + "stk%d" % cs)
                for b in range(nt // 128):
                    vs_ = vi % 2; vi += 1
                    vk = pre + "vt%d" % vs_
                    for n in range(2):
                        p_ = ps[pi % 6]; pk = pre + "ps%d" % (pi % 6); pi += 1
                        for kc in range(2):
                            self.mm(p_[:], cT[cs][:, kc, b * 128:(b + 1) * 128], wv[:, kc, n * 512:(n + 1) * 512], kc == 0, kc == 1,
                                    [ck, pre + "wv"], [pk])
                        self.copy("act" if n else "dve", vt[vs_][:, n * 512:(n + 1) * 512], p_[:], [pk], [vk])
                    r0 = t0 + b * 128
                    if kind == "g":
                        self.st(S["v1"][rk * NP + r0:rk * NP + r0 + 128, :], vt[vs_][:], [vk], ["d_v1"], semkey=pre + "stv%d" % vs_)
                    elif kind == "l":
                        self.st(S["v1l"][r0:r0 + 128, :], vt[vs_][:], [vk], ["d_vl"], semkey=pre + "stv%d" % vs_)
                    else:
                        self.st(S["v1s"], vt[vs_][:], [vk], ["d_v1s"], semkey=pre + "stv%d" % vs_)

    def phase_E1(self):
        nc, P, I, O, S = self.nc, self.P, self.I, self.O, self.S
        pre = "E1_"
        with contextlib.ExitStack() as st:
            sb = lambda n, s, d=F32: st.enter_context(nc.sbuf_tensor(pre + n, list(s), d))
            psf = lambda n: st.enter_context(nc.psum_tensor(pre + n, [128, 512], F32))
            wo = sb("wo", [64, 16, D], BF16); wr = sb("wr", [128, 8, 8], BF16); gb = sb("gb", [128, D])
            xt = [sb("xt%d" % i, [128, D]) for i in range(2)]
            T = {"sq": sb("sq", [128, D], BF16), "ss": sb("ss", [128, 1]), "rs": sb("rs", [128, 1]),
                 "hb": sb("hb", [128, D], BF16), "pst": st.enter_context(nc.psum_tensor(pre + "pst", [128, 1024], BF16))}
            oTt = [sb("oTt%d" % i, [64, 16, 512], BF16) for i in range(2)]
            hT = [sb("hT%d" % i, [128, 8, 128], BF16) for i in range(2)]
            lg = sb("lg", [128, 8]); m1 = sb("m1", [128, 1]); m2 = sb("m2", [128, 1]); eq = sb("eq", [128, 8]); l2 = sb("l2", [128, 8])
            ex = sb("ex", [128, 8]); sm = sb("sm", [128, 1]); cb = [sb("cb%d" % i, [128, 8]) for i in range(2)]
            ps = [psf("ps%d" % i) for i in range(6)]
            pi = 0
            self.ld(wo[:], I["w_o1"].rearrange("(h p) n -> p h n", p=64), [], [pre + "wo"], eng="pool")
            self.ld(wr[:], I["w_r"].rearrange("(kc p) n -> p kc n", p=128), [], [pre + "wr"], eng="pool")
            self.ld(gb[:], I["g_ffn1"], [], ["gains"])
            bi = 0
            for ti, (t0, nt) in enumerate(self.tiles):
                hs = ti % 2
                self.ld(oTt[hs][:, :, 0:nt], S["oT1"][:, :, t0:t0 + nt].rearrange("h p t -> p h t"), ["d_oT1"], [pre + "oTt%d" % hs])
                for b in range(nt // 128):
                    xs = bi % 2; bi += 1
                    r0 = t0 + b * 128
                    xk = pre + "xt%d" % xs
                    self.ld(xt[xs][:], S["x2"][r0:r0 + 128, :], ["d_x2"], [xk])
                    for n in range(2):
                        p_ = ps[pi % 6]; pk = pre + "ps%d" % (pi % 6); pi += 1
                        for h in range(16):
                            self.mm(p_[:], oTt[hs][:, h, b * 128:(b + 1) * 128], wo[:, h, n * 512:(n + 1) * 512], h == 0, h == 15,
                                    [pre + "oTt%d" % hs, pre + "wo"], [pk])
                        self.tt("dve", xt[xs][:, n * 512:(n + 1) * 512], xt[xs][:, n * 512:(n + 1) * 512], p_[:], ALU.add, [xk, pk], [xk])
                    self.st(S["x3"][r0:r0 + 128, :], xt[xs][:], [xk], ["d_x3"], semkey=pre + "stx%d" % xs)
                    self.norm_T(T, xt[xs][:], xk, gb[:], D, None, None, pre)
                    hk = pre + "hT%d" % xs
                    self.copy("act", hT[xs][:], T["pst"][:].rearrange("p (k c) -> p k c", k=8), [pre + "pst"], [hk])
                    self.st(S["hT"][:, :, r0:r0 + 128].rearrange("k p t -> p k t"), hT[xs][:], [hk], ["d_hT"], semkey=pre + "sth%d" % xs)
                    p_ = ps[pi % 6]; pk = pre + "ps%d" % (pi % 6); pi += 1
                    for kc in range(8):
                        self.mm(p_[:, 0:8], hT[xs][:, kc, :], wr[:, kc, :], kc == 0, kc == 7, [hk, pre + "wr"], [pk])
                    self.copy("dve", lg[:], p_[:, 0:8], [pk], [pre + "lg"])
                    self.P.op("dve", lambda e: e.reduce_max(m1[:], lg[:], AX.X), [pre + "lg"], [pre + "m1"])
                    self.ts("dve", eq[:], lg[:], m1[:, 0:1], -1e30, ALU.is_equal, ALU.mult, [pre + "lg", pre + "m1"], [pre + "eq"])
                    self.tt("dve", l2[:], lg[:], eq[:], ALU.add, [pre + "lg", pre + "eq"], [pre + "l2"])
                    self.P.op("dve", lambda e: e.reduce_max(m2[:], l2[:], AX.X), [pre + "l2"], [pre + "m2"])
                    self.ts("dve", eq[:], lg[:], m2[:, 0:1], None, ALU.is_ge, None, [pre + "lg", pre + "m2", pre + "eq"], [pre + "eq"])
                    self.ts("dve", l2[:], lg[:], m1[:, 0:1], None, ALU.subtract, None, [pre + "lg", pre + "m1", pre + "l2"], [pre + "l2"])
                    self.act(ex[:], l2[:], AF.Exp, [pre + "l2"], [pre + "ex"])
                    self.tt("dve", ex[:], ex[:], eq[:], ALU.mult, [pre + "ex", pre + "eq"], [pre + "ex"])
                    self.P.op("dve", lambda e: e.reduce_sum(sm[:], ex[:], AX.X), [pre + "ex"], [pre + "sm"])
                    self.P.op("dve", lambda e: e.reciprocal(sm[:], sm[:]), [pre + "sm"], [pre + "sm"])
                    ck = pre + "cb%d" % xs
                    self.ts("dve", cb[xs][:], ex[:], sm[:, 0:1], None, ALU.mult, None, [pre + "ex", pre + "sm"], [ck])
                    self.st(S["comb"][r0:r0 + 128, :], cb[xs][:], [ck], ["d_comb"], semkey=pre + "stc%d" % xs)

    def phase_E2(self):
        nc, P, I, O, S = self.nc, self.P, self.I, self.O, self.S
        NBLK = self.NBLK
        pre = "E2_"
        GB = 11 if NBLK % 11 == 0 else NBLK
        NG = NBLK // GB
        GT = GB * 128
        widths = []
        o_ = 0
        while o_ < GT:
            w_ = min(512, GT - o_); widths.append((o_, w_)); o_ += w_
        QF = FE // 4
        NCH = QF // 128
        with contextlib.ExitStack() as st:
            sb = lambda n, s, d=F32: st.enter_context(nc.sbuf_tensor(pre + n, list(s), d))
            psf = lambda n: st.enter_context(nc.psum_tensor(pre + n, [128, 512], F32))
            hT = sb("hT", [128, 8, GT], BF16)
            yacc = sb("yacc", [128, GB, D])
            cb = sb("cb", [128, GB, 8])
            wgu = [sb("wgu%d" % i, [128, 8, 2, QF], BF16) for i in range(2)]
            wdn = [sb("wdn%d" % i, [128, NCH, D], BF16) for i in range(2)]
            sg = [sb("sg%d" % i, [128, 512]) for i in range(2)]
            aT = [sb("aT%d" % i, [128, NCH, 512], BF16) for i in range(2)]
            gb = sb("gb", [128, D]); sq = sb("sq", [128, D], BF16); ss = sb("ss", [128, 1]); rs = sb("rs", [128, 1])
            yo = [sb("yo%d" % i, [128, D]) for i in range(2)]
            ps = [psf("ps%d" % i) for i in range(7)]
            pi = 0
            self.ld(gb[:], I["g_fin"], [], ["gains"])
            ui = 0
            ai = 0
            for gi in range(NG):
                g0 = gi * GT
                self.ld(hT[:], S["hT"][:, :, g0:g0 + GT].rearrange("k p t -> p k t"), ["d_hT"], [pre + "hT"])
                self.ld(cb[:], S["comb"][g0:g0 + GT, :].rearrange("(b p) e -> p b e", p=128), ["d_comb"], [pre + "cb"])
                self.ld(yacc[:], S["x3"][g0:g0 + GT, :].rearrange("(b p) e -> p b e", p=128), ["d_x3"], [pre + "yacc%d" % b_ for b_ in range(GB)])
                for e_ in range(NE):
                    for q in range(4):
                        ws = ui % 2; ui += 1
                        wgk, wdk = pre + "wgu%d" % ws, pre + "wdn%d" % ws
                        src = I["w_gu1"][e_].rearrange("(kc p) (two f) -> p kc two f", p=128, two=2)
                        for two in range(2):
                            self.ld(wgu[ws][:, :, two, :], src[:, :, two, q * QF:(q + 1) * QF], [], [wgk], eng="pool")
                        self.ld(wdn[ws][:], I["w_dn1"][e_, q * QF:(q + 1) * QF, :].rearrange("(j p) n -> p j n", p=128), [], [wdk], eng="pool")
                        for (o0, wd) in widths:
                            as_ = ai % 2; ai += 1
                            ak = pre + "aT%d" % as_
                            for j in range(NCH):
                                pg = ps[pi % 7]; pgk = pre + "ps%d" % (pi % 7); pi += 1
                                pu = ps[pi % 7]; puk = pre + "ps%d" % (pi % 7); pi += 1
                                for kc in range(8):
                                    self.mm(pg[:, 0:wd], wgu[ws][:, kc, 0, j * 128:(j + 1) * 128], hT[:, kc, o0:o0 + wd], kc == 0, kc == 7,
                                            [wgk, pre + "hT"], [pgk])
                                for kc in range(8):
                                    self.mm(pu[:, 0:wd], wgu[ws][:, kc, 1, j * 128:(j + 1) * 128], hT[:, kc, o0:o0 + wd], kc == 0, kc == 7,
                                            [wgk, pre + "hT"], [puk])
                                sgs = j % 2
                                self.act(sg[sgs][:, 0:wd], pg[:, 0:wd], AF.Silu, [pgk], [pre + "sg%d" % sgs])
                                self.tt("dve", aT[as_][:, j, 0:wd], sg[sgs][:, 0:wd], pu[:, 0:wd], ALU.mult, [pre + "sg%d" % sgs, puk], [ak])
                            for b in range(wd // 128):
                                blk = o0 // 128 + b
                                for n in range(2):
                                    p_ = ps[pi % 7]; pk = pre + "ps%d" % (pi % 7); pi += 1
                                    for j in range(NCH):
                                        self.mm(p_[:], aT[as_][:, j, b * 128:(b + 1) * 128], wdn[ws][:, j, n * 512:(n + 1) * 512], j == 0, j == NCH - 1,
                                                [ak, wdk], [pk])
                                    self.stt("dve", yacc[:, blk, n * 512:(n + 1) * 512], p_[:], cb[:, blk, e_:e_ + 1],
                                             yacc[:, blk, n * 512:(n + 1) * 512], ALU.mult, ALU.add, [pk, pre + "cb", pre + "yacc%d" % blk], [pre + "yacc%d" % blk])
                for blk in range(GB):
                    ys = blk % 2
                    x = yacc[:, blk, :]
                    self.memset("pool", ss[:], 0.0, [pre + "ss"])
                    self.act(sq[:], x, AF.Square, [pre + "yacc%d" % blk, pre + "ss"], [pre + "sq", pre + "ss"], accum_out=ss[:])
                    self.act(rs[:], ss[:], AF.Ln, [pre + "ss"], [pre + "rs"], scale=1.0 / D, bias=EPS)
                    self.act(rs[:], rs[:], AF.Exp, [pre + "rs"], [pre + "rs"], scale=-0.5)
                    self.stt("dve", yo[ys][:], x, rs[:, 0:1], gb[:], ALU.mult, ALU.mult, [pre + "yacc%d" % blk, pre + "rs", "gains"], [pre + "yo%d" % ys])
                    r0 = g0 + blk * 128
                    self.st(O["y"][r0:r0 + 128, :], yo[ys][:], [pre + "yo%d" % ys], [], final=True, semkey=pre + "sty%d" % ys)


def host_tables(T, PAST, r):
    NB = T // 2048
    NKB = 16 * NB
    NP = NB * 512
    NBLKP = NB * 4
    f = np.float32
    k = np.arange(128)[:, None]
    q = np.arange(512)[None, :]
    T0 = (-(q - k)).astype(f)
    Dneg = np.zeros((128, 4, 512), f)
    Dmask = np.zeros((128, 4, 512), f)
    for i in range(4):
        kp = 128 * i + k
        vis = (kp // 64) <= (q // 64)
        Dneg[:, i, :] = np.where(vis, -np.abs(q - kp), -1e30)
        Dmask[:, i, :] = np.where(vis, 0.0, -1e30)
    ct = np.zeros((8, NB, NKB), f)
    mt = np.zeros((NB, NKB), f)
    for J in range(NB):
        gq = 4 * J + r
        for rk in range(4):
            for Jk in range(NB):
                gk = 4 * Jk + rk
                for i in range(4):
                    kbi = rk * NBLKP + Jk * 4 + i
                    if gk < gq:
                        dlt = (gq - gk) * 512 - 128 * i
                        for h in range(8):
                            ct[h, J, kbi] = -SLOPES[h] * dlt
                        mt[J, kbi] = 0.0
                    else:
                        ct[:, J, kbi] = -30000.0
                        mt[J, kbi] = -30000.0
    ctab0 = np.ascontiguousarray(np.broadcast_to(ct.reshape(1, -1), (128, 8 * NB * NKB)))
    mtab = np.ascontiguousarray(np.broadcast_to(mt.reshape(1, -1), (128, NB * NKB)))
    NTOK = (NBLKP + 1) * 128
    pos = np.zeros(NTOK, np.int64)
    for J in range(NB):
        pos[J * 512:(J + 1) * 512] = (4 * J + r) * 512 + np.arange(512)
    for s in range(2):
        pos[NP + s * 32:NP + s * 32 + 16] = PAST + np.arange(16)
    half = 16
    freqs = np.power(np.float32(10000.0), -np.arange(half, dtype=f) * np.float32(2.0) / np.float32(32)).astype(f)
    ang = pos.astype(f)[:, None] * freqs[None, :]
    co, si = np.cos(ang).astype(f), np.sin(ang).astype(f)
    cs_tm = np.concatenate([co, si], 1).astype(f)
    cs_fm = np.stack([np.concatenate([co.T, co.T], 0), np.concatenate([-si.T, si.T], 0)], 0).astype(f)
    PB = PAST // 128
    kb = np.arange(PB)[None, :, None]
    ii = np.arange(16)[None, None, :]
    Dnegs = (-(PAST + ii - 128 * kb - k[:, :, None])).astype(f).reshape(128, PB * 16)
    kn = np.arange(16)[:, None]
    Dnegn = (-np.abs(np.arange(16)[None, :] - kn)).astype(f)
    return dict(T0=T0, Dneg=Dneg, Dmask=Dmask, ctab0=ctab0, mtab=mtab, cs_tm=cs_tm, cs_fm=np.ascontiguousarray(cs_fm),
                Dnegs=np.ascontiguousarray(Dnegs), Dnegn=Dnegn, ident=np.eye(128, dtype=f))


_CACHE = {}


def run(inputs, T, PAST, dbg=()):
    f = np.float32
    A = {k_: np.asarray(v) for k_, v in inputs.items()}
    NB = T // 2048
    NP = NB * 512
    NTOK = (NB * 4 + 1) * 128
    key = (T, PAST, tuple(dbg))
    if key not in _CACHE:
        _CACHE[key] = Builder(T, PAST, dbg).build()
    nc = _CACHE[key]
    bc = lambda v, n=128: np.ascontiguousarray(np.broadcast_to(np.asarray(v, f).reshape(1, -1), (n, np.asarray(v).size)))
    w_uq = A["mla_w_uq"][0]
    wq4 = w_uq.reshape(768, 16, 96)
    w_uqs = np.concatenate([wq4[:, :, 0:64], wq4[:, :, 80:96], wq4[:, :, 64:80]], axis=2).reshape(768, 1536)
    common = dict(
        g_mix0=bc(A["norm_mix"][0]), g_mix1=bc(A["norm_mix"][1]), g_ffn0=bc(A["norm_ffn"][0]), g_ffn1=bc(A["norm_ffn"][1]),
        g_fin=bc(A["norm_final"]), lam=bc(A["diff_lambda"][0].reshape(-1)), gsub_col=np.ascontiguousarray(A["diff_subln"][0].reshape(128, 1)),
        gsub_row=bc(A["diff_subln"][0]), g_q=bc(A["mla_norm_q"][0]), g_kv=bc(A["mla_norm_kv"][0]),
        w_qkv=A["diff_w_qkv"][0], w_o0=A["diff_w_o"][0], w_gu0=A["ffn_w_gu"][0], w_dn0=A["ffn_w_down"][0],
        w_a=A["mla_w_a"][0], w_uq=w_uq, w_uqs=np.ascontiguousarray(w_uqs), w_ukv=A["mla_w_ukv"][0], w_o1=A["mla_w_o"][0],
        w_r=A["moe_router"][0], w_gu1=A["moe_w_gu"][0], w_dn1=A["moe_w_down"][0])
    in_maps = []
    for c in range(8):
        b, r = c // 4, c % 4
        xin = np.zeros((NTOK, D), f)
        xp = A["x_prompt"][b].reshape(T // 512, 512, D)
        xin[:NP] = xp[r::4].reshape(NP, D)
        for s in range(2):
            xin[NP + s * 32:NP + s * 32 + 16] = A["x_sample"][2 * c + s]
        m = dict(common)
        m.update(host_tables(T, PAST, r))
        m["xin"] = xin
        m["cdk"] = np.ascontiguousarray(A["cache_diff_k"][0, 2 * c:2 * c + 2].reshape(2, PAST, D))
        m["cdv"] = np.ascontiguousarray(A["cache_diff_v"][0, 2 * c:2 * c + 2].reshape(2, PAST, D))
        m["cckv"] = np.ascontiguousarray(A["cache_mla_ckv"][0, 2 * c:2 * c + 2])
        m["ckpe"] = np.ascontiguousarray(A["cache_mla_kpe"][0, 2 * c:2 * c + 2])
        in_maps.append(m)
    res = run_bass_kernel_spmd(nc, in_maps, core_ids=list(range(8)))
    B = 2
    outs = {}
    shapes = dict(y=D, dk=D, dv=D, ckv=256, kpe=32)
    for name, wdt in shapes.items():
        pr = np.zeros((B, T // 512, 512, wdt), f)
        sm = np.zeros((16, 16, wdt), f)
        for c in range(8):
            b, r = c // 4, c % 4
            o = res.results[c][name]
            pr[b, r::4] = o[:NP].reshape(NB, 512, wdt)
            for s in range(2):
                sm[2 * c + s] = o[NP + s * 32:NP + s * 32 + 16]
        outs[name] = (pr.reshape(B, T, wdt), sm)
    dbgout = {n: [res.results[c][n] for c in range(8)] for n in dbg}
    y_p, y_s = outs["y"]
    dk_p, dk_s = outs["dk"]
    dv_p, dv_s = outs["dv"]
    ck_p, ck_s = outs["ckv"]
    kp_p, kp_s = outs["kpe"]
    out = (y_p, y_s, dk_p.reshape(1, B, T, 8, 128), dv_p.reshape(1, B, T, 8, 128), ck_p.reshape(1, B, T, 256), kp_p.reshape(1, B, T, 32),
           dk_s.reshape(1, 16, 16, 8, 128), dv_s.reshape(1, 16, 16, 8, 128), ck_s.reshape(1, 16, 16, 256), kp_s.reshape(1, 16, 16, 32))
    if dbg:
        return out, dbgout
    return out


def kernel(**inputs):
    T = inputs["x_prompt"].shape[1]
    PAST = inputs["cache_diff_k"].shape[2]
    return run(inputs, T, PAST)
```
